# Optimizing a Trainium2 kernel written in Bass

```python
import math
import jax, jax.numpy as jnp
from jax import lax
import numpy as np

D_MODEL = 1024
BATCH = 8
SEQ = 2048
DEPTH = 1

PLE_DIM = 256
D_MIX = D_MODEL
D_LRU = D_MIX // 2
LRU_BLOCKS = 8
LRU_BLOCK_DIM = D_LRU // LRU_BLOCKS
CONV_WIDTH = 4
LRU_C = 8.0
D_ATTN = D_MIX - D_LRU
N_ATT_HEADS = 8
HEAD_DIM = D_ATTN // N_ATT_HEADS
DILATED_GROUPS = ((128, 1), (512, 4), (2048, 16))
ATT_BLOCK = 128
REL_BUCKETS = 32
REL_MAX_DIST = 2048
PEER_HEADS = 8
PEER_NKEYS = 128
PEER_EXPERTS = PEER_NKEYS * PEER_NKEYS
PEER_QDIM = 256
PEER_TOPK = 16
PEER_TOKEN_BLOCK = 128
NORM_EPS = 1e-6
D_IN_PROJ = 2 * D_LRU + 3 * D_ATTN

kernel_name = 'hybrid_rglru_dilated_attn_peer'


def rmsnorm(x, g):
    xf = x.astype(jnp.float32)
    y = xf * lax.rsqrt(jnp.mean(xf * xf, axis=-1, keepdims=True) + NORM_EPS)
    return (y * g.astype(jnp.float32)).astype(x.dtype)


def t5_causal_bucket(dist):
    max_exact = REL_BUCKETS // 2
    n = jnp.maximum(dist, 0)
    nf = jnp.maximum(n, 1).astype(jnp.float32)
    large = max_exact + (jnp.log(nf / max_exact) / math.log(REL_MAX_DIST / max_exact)
                         * (REL_BUCKETS - max_exact)).astype(jnp.int32)
    large = jnp.minimum(large, REL_BUCKETS - 1)
    return jnp.where(n < max_exact, n, large)


def causal_depthwise_conv(x, w, b):
    y = lax.conv_general_dilated(
        x, w[:, None, :], window_strides=(1,), padding=[(CONV_WIDTH - 1, 0)],
        dimension_numbers=('NWC', 'WIO', 'NWC'), feature_group_count=x.shape[-1])
    return y + b


def rg_lru(x, w_rg, b_rg, w_ig, b_ig, lam):
    B, T, C = x.shape
    xf = x.astype(jnp.float32)
    xb = xf.reshape(B, T, LRU_BLOCKS, LRU_BLOCK_DIM)
    r = jax.nn.sigmoid(jnp.einsum('btgi,gij->btgj', xb, w_rg.astype(jnp.float32)).reshape(B, T, C)
                       + b_rg.astype(jnp.float32))
    i = jax.nn.sigmoid(jnp.einsum('btgi,gij->btgj', xb, w_ig.astype(jnp.float32)).reshape(B, T, C)
                       + b_ig.astype(jnp.float32))
    log_a = -LRU_C * r * jax.nn.softplus(-lam.astype(jnp.float32))
    a = jnp.exp(log_a)
    bterm = jnp.sqrt(-jnp.expm1(2.0 * log_a)) * (i * xf)

    def combine(c1, c2):
        a1, b1 = c1
        a2, b2 = c2
        return a1 * a2, a2 * b1 + b2

    _, h = lax.associative_scan(combine, (a, bterm), axis=1)
    return h.astype(x.dtype)


def dilated_branch(q, k, v, rel_bias, window, dilation):
    B, T, H, Dh = q.shape
    L = T // dilation
    W = window // dilation
    nb = -(-L // ATT_BLOCK)
    Lp = nb * ATT_BLOCK
    n_prev = -(-W // ATT_BLOCK)
    band = (n_prev + 1) * ATT_BLOCK

    def to_sub(t):
        return t.reshape(B, L, dilation, H, Dh).transpose(0, 2, 3, 1, 4)

    qs = jnp.pad(to_sub(q), ((0, 0), (0, 0), (0, 0), (0, Lp - L), (0, 0)))
    qb = qs.reshape(B, dilation, H, nb, ATT_BLOCK, Dh)
    pad_kv = ((0, 0), (0, 0), (0, 0), (n_prev * ATT_BLOCK, Lp - L), (0, 0))

    def bands(t):
        tb = jnp.pad(to_sub(t), pad_kv).reshape(B, dilation, H, nb + n_prev, ATT_BLOCK, Dh)
        return jnp.concatenate([tb[:, :, :, j:j + nb] for j in range(n_prev + 1)], axis=4)

    kb = bands(k)
    vb = bands(v)
    qi = jnp.arange(ATT_BLOCK)[:, None]
    kj = jnp.arange(band)[None, :]
    dist_sub = qi + n_prev * ATT_BLOCK - kj
    key_sub = (jnp.arange(nb)[:, None, None] - n_prev) * ATT_BLOCK + kj[None]
    mask = (dist_sub >= 0) & (dist_sub <= W) & (key_sub >= 0)
    bias = rel_bias[t5_causal_bucket(dist_sub * dilation)].transpose(2, 0, 1)

    s = jnp.einsum('bdhnqc,bdhnkc->bdhnqk', qb, kb) + bias[None, None, :, None].astype(jnp.float32)
    s = jnp.where(mask[None, None, None], s, -jnp.inf)
    m = jnp.max(s, axis=-1, keepdims=True)
    e = jnp.exp(s - m)
    den = jnp.sum(e, axis=-1)
    o = jnp.einsum('bdhnqk,bdhnkc->bdhnqc', e, vb) / den[..., None]
    lse = m[..., 0] + jnp.log(den)
    o = o.reshape(B, dilation, H, Lp, Dh)[:, :, :, :L].transpose(0, 3, 1, 2, 4).reshape(B, T, H, Dh)
    lse = lse.reshape(B, dilation, H, Lp)[..., :L].transpose(0, 3, 1, 2).reshape(B, T, H)
    return o, lse


def dilated_attention(q, k, v, rel_bias):
    outs = []
    lses = []
    for window, dilation in DILATED_GROUPS:
        o, lse = dilated_branch(q, k, v, rel_bias, window, dilation)
        outs.append(o)
        lses.append(lse)
    wts = jax.nn.softmax(jnp.stack(lses, axis=0), axis=0)
    return jnp.sum(wts[..., None] * jnp.stack(outs, axis=0), axis=0)


def hybrid_mixer(u, w_in, conv_w, conv_b, w_rg, b_rg, w_ig, b_ig, lru_lambda,
                 g_lru_out, g_attn_out, w_out, rel_bias):
    B, T, _ = u.shape
    z = u @ w_in
    x_lru = z[..., :D_LRU]
    gate = z[..., D_LRU:2 * D_LRU]
    o0 = 2 * D_LRU
    q = z[..., o0:o0 + D_ATTN]
    k = z[..., o0 + D_ATTN:o0 + 2 * D_ATTN]
    v = z[..., o0 + 2 * D_ATTN:]
    xc = causal_depthwise_conv(x_lru, conv_w, conv_b)
    y_lru = rg_lru(xc, w_rg, b_rg, w_ig, b_ig, lru_lambda) * jax.nn.gelu(gate)
    qh = q.reshape(B, T, N_ATT_HEADS, HEAD_DIM).astype(jnp.float32) * (HEAD_DIM ** -0.5)
    kh = k.reshape(B, T, N_ATT_HEADS, HEAD_DIM).astype(jnp.float32)
    vh = v.reshape(B, T, N_ATT_HEADS, HEAD_DIM).astype(jnp.float32)
    y_att = dilated_attention(qh, kh, vh, rel_bias).reshape(B, T, D_ATTN).astype(u.dtype)
    y = jnp.concatenate([rmsnorm(y_lru, g_lru_out), rmsnorm(y_att, g_attn_out)], axis=-1)
    return y @ w_out


def peer_ffn(x, w_pq, peer_k1, peer_k2, peer_u, peer_v):
    B, T, D = x.shape
    N = B * T
    xt = x.reshape(N, D)
    half = PEER_QDIM // 2
    q = (xt @ w_pq).astype(jnp.float32).reshape(N, PEER_HEADS, PEER_QDIM)
    s1 = jnp.einsum('nhc,kc->nhk', q[..., :half], peer_k1.astype(jnp.float32))
    s2 = jnp.einsum('nhc,kc->nhk', q[..., half:], peer_k2.astype(jnp.float32))
    t1, i1 = lax.top_k(s1, PEER_TOPK)
    t2, i2 = lax.top_k(s2, PEER_TOPK)
    cand_s = (t1[..., :, None] + t2[..., None, :]).reshape(N, PEER_HEADS, PEER_TOPK * PEER_TOPK)
    cand_id = (i1[..., :, None] * PEER_NKEYS + i2[..., None, :]).reshape(N, PEER_HEADS, PEER_TOPK * PEER_TOPK)
    top_s, top_pos = lax.top_k(cand_s, PEER_TOPK)
    ids = jnp.take_along_axis(cand_id, top_pos, axis=-1)
    g = jax.nn.softmax(top_s, axis=-1).astype(x.dtype)
    nblk = N // PEER_TOKEN_BLOCK

    def expert_block(args):
        xb, idb, gb = args
        act = jax.nn.gelu(jnp.einsum('td,thkd->thk', xb, peer_u[idb])) * gb
        return jnp.einsum('thk,thkd->td', act, peer_v[idb])

    y = lax.map(expert_block, (xt.reshape(nblk, PEER_TOKEN_BLOCK, D),
                               ids.reshape(nblk, PEER_TOKEN_BLOCK, PEER_HEADS, PEER_TOPK),
                               g.reshape(nblk, PEER_TOKEN_BLOCK, PEER_HEADS, PEER_TOPK)))
    return y.reshape(B, T, D)


def setup_inputs(seed: int = 0) -> dict:
    key = jax.random.key(seed)
    ks = jax.random.split(key, 26)
    f32 = jnp.float32

    def nrm(k, shape, scale):
        return jax.random.normal(k, shape, f32) * scale

    def gain(k, shape):
        return 1.0 + 0.02 * jax.random.normal(k, shape, f32)

    u = jax.random.uniform(ks[10], (DEPTH, D_LRU), f32, 0.9, 0.999)
    a0 = u ** (1.0 / LRU_C)
    lru_lambda = jnp.log(a0) - jnp.log1p(-a0)
    return {
        'x': jax.random.normal(ks[0], (BATCH, SEQ, D_MODEL), f32),
        'p': jax.random.normal(ks[1], (DEPTH, BATCH, SEQ, PLE_DIM), f32),
        'g_mix': gain(ks[2], (DEPTH, D_MODEL)),
        'w_in': nrm(ks[3], (DEPTH, D_MODEL, D_IN_PROJ), D_MODEL ** -0.5),
        'conv_w': nrm(ks[4], (DEPTH, CONV_WIDTH, D_LRU), CONV_WIDTH ** -0.5),
        'conv_b': nrm(ks[5], (DEPTH, D_LRU), 0.02),
        'w_rg': nrm(ks[6], (DEPTH, LRU_BLOCKS, LRU_BLOCK_DIM, LRU_BLOCK_DIM), LRU_BLOCK_DIM ** -0.5),
        'b_rg': nrm(ks[7], (DEPTH, D_LRU), 0.02),
        'w_ig': nrm(ks[8], (DEPTH, LRU_BLOCKS, LRU_BLOCK_DIM, LRU_BLOCK_DIM), LRU_BLOCK_DIM ** -0.5),
        'b_ig': nrm(ks[9], (DEPTH, D_LRU), 0.02),
        'lru_lambda': lru_lambda,
        'g_lru_out': gain(ks[11], (DEPTH, D_LRU)),
        'g_attn_out': gain(ks[12], (DEPTH, D_ATTN)),
        'w_out': nrm(ks[13], (DEPTH, D_MIX, D_MODEL), D_MIX ** -0.5),
        'rel_bias': nrm(ks[14], (REL_BUCKETS, N_ATT_HEADS), 0.5),
        'g_ffn': gain(ks[15], (DEPTH, D_MODEL)),
        'peer_wq': nrm(ks[16], (DEPTH, D_MODEL, PEER_HEADS * PEER_QDIM), D_MODEL ** -0.5),
        'peer_k1': nrm(ks[17], (DEPTH, PEER_NKEYS, PEER_QDIM // 2), (PEER_QDIM // 2) ** -0.5),
        'peer_k2': nrm(ks[18], (DEPTH, PEER_NKEYS, PEER_QDIM // 2), (PEER_QDIM // 2) ** -0.5),
        'peer_u': nrm(ks[19], (DEPTH, PEER_EXPERTS, D_MODEL), D_MODEL ** -0.5),
        'peer_v': nrm(ks[20], (DEPTH, PEER_EXPERTS, D_MODEL), PEER_HEADS ** -0.5),
        'g_ple': gain(ks[21], (DEPTH, D_MODEL)),
        'w_ple_gate': nrm(ks[22], (DEPTH, D_MODEL, D_MODEL), D_MODEL ** -0.5),
        'w_ple_proj': nrm(ks[23], (DEPTH, PLE_DIM, D_MODEL), PLE_DIM ** -0.5),
        'g_final': gain(ks[24], (D_MODEL,)),
    }


def reference(x, p, g_mix, w_in, conv_w, conv_b, w_rg, b_rg, w_ig, b_ig, lru_lambda,
              g_lru_out, g_attn_out, w_out, rel_bias, g_ffn, peer_wq, peer_k1, peer_k2,
              peer_u, peer_v, g_ple, w_ple_gate, w_ple_proj, g_final):
    h = x
    for i in range(DEPTH):
        h = h + hybrid_mixer(rmsnorm(h, g_mix[i]), w_in[i], conv_w[i], conv_b[i], w_rg[i], b_rg[i],
                             w_ig[i], b_ig[i], lru_lambda[i], g_lru_out[i], g_attn_out[i], w_out[i],
                             rel_bias)
        h = h + peer_ffn(rmsnorm(h, g_ffn[i]), peer_wq[i], peer_k1[i], peer_k2[i], peer_u[i], peer_v[i])
        h = h + jax.nn.sigmoid(rmsnorm(h, g_ple[i]) @ w_ple_gate[i]) * (p[i] @ w_ple_proj[i])
    return rmsnorm(h, g_final)
```

```python
import numpy as np
import concourse.bass as bass
import concourse.mybir as mybir
from concourse.bass_utils import run_bass_kernel_spmd

F32 = mybir.dt.float32
BF16 = mybir.dt.bfloat16
U32 = mybir.dt.uint32
I32 = mybir.dt.int32
AF = mybir.ActivationFunctionType
ALU = mybir.AluOpType
AX = mybir.AxisListType

T = 2048
D = 1024
NT = 16
EPS = 1e-6
NEG = -30000.0


class Buf:
    __slots__ = ("name", "lastw", "reads", "dsem", "dcnt")

    def __init__(self, name):
        self.name = name
        self.lastw = None
        self.reads = {}
        self.dsem = None
        self.dcnt = 0


class Prog:
    ENG = ("pe", "dve", "act", "pool", "sp")

    def __init__(self, nc):
        self.nc = nc
        self.q = {e: [] for e in self.ENG}
        self.sem = {e: nc.alloc_semaphore(f"prog_{e}") for e in self.ENG}
        self.cnt = {e: 0 for e in self.ENG}
        self.waited = {e: {} for e in self.ENG}
        self.nbuf = 0
        self.dsems = []
        self.free_dsems = []

    def buf(self, name=None):
        self.nbuf += 1
        return Buf(name or f"b{self.nbuf}")

    def bufs(self, name, n):
        return [self.buf(f"{name}{i}") for i in range(n)]

    def _tokens(self, eng, reads, writes):
        need = {}

        def add(tok):
            if tok is None:
                return
            s, v = tok
            if need.get(id(s), (None, 0))[1] < v:
                need[id(s)] = (s, v)

        for b in reads:
            add(b.lastw)
        for b in writes:
            add(b.lastw)
            for tok in b.reads.values():
                add(tok)
        own = self.sem[eng]
        waits = []
        w = self.waited[eng]
        for s, v in need.values():
            if eng == "pe" and s is own:
                continue
            if w.get(id(s), 0) < v:
                w[id(s)] = v
                waits.append((s, v))
        return waits

    def _commit(self, tok, reads, writes):
        s, v = tok
        for b in reads:
            if b.reads.get(id(s), (None, 0))[1] < v:
                b.reads[id(s)] = (s, v)
        for b in writes:
            b.lastw = tok
            b.reads = {}

    def op(self, eng, fn, reads=(), writes=()):
        waits = self._tokens(eng, reads, writes)
        self.cnt[eng] += 1
        tok = (self.sem[eng], self.cnt[eng])
        self.q[eng].append((waits, fn, tok[0], 1))
        self._commit(tok, reads, writes)
        return tok

    def dma(self, eng, fn, sb, reads=(), writes=()):
        waits = self._tokens(eng, reads, writes)
        if sb.dsem is None:
            sb.dsem = self.nc.alloc_semaphore(f"dma_{sb.name}")
            self.dsems.append(sb)
        sb.dcnt += 16
        tok = (sb.dsem, sb.dcnt)
        self.q[eng].append((waits, fn, sb.dsem, 16))
        self._commit(tok, reads, writes)
        return tok

    def share_dsem(self, bufs):
        return bufs

    def barrier(self):
        toks = [(self.sem[e], self.cnt[e]) for e in self.ENG if self.cnt[e] > 0]
        toks += [(b.dsem, b.dcnt) for b in self.dsems]
        for e in self.ENG:
            waits = []
            w = self.waited[e]
            for s, v in toks:
                if e == "pe" and s is self.sem["pe"]:
                    continue
                if w.get(id(s), 0) < v:
                    w[id(s)] = v
                    waits.append((s, v))
            if waits:
                self.q[e].append((waits, None, None, 0))

    def emit(self):
        nc = self.nc
        with nc.Block() as block:
            @block.tensor
            def _(e):
                self._emit("pe", e)

            @block.vector
            def _(e):
                self._emit("dve", e)

            @block.scalar
            def _(e):
                self._emit("act", e)

            @block.gpsimd
            def _(e):
                self._emit("pool", e)

            @block.sync
            def _(e):
                self._emit("sp", e)

    def _emit(self, name, e):
        for waits, fn, sem, inc in self.q[name]:
            for s, v in waits:
                e.wait_ge(s, v)
            if fn is not None:
                ins = fn(e)
                ins.then_inc(sem, inc)


class Alloc:
    def __init__(self, nc):
        self.nc = nc
        self.lo = (int(nc.sbuf_base) + 63) // 64 * 64
        self.hi = int(nc.sbuf_top)
        self.cur = self.lo
        self.n = 0
        self.offs = {}

    def alloc(self, name, shape, dtype):
        size = int(np.prod(shape[1:])) * mybir.dt.size(dtype)
        off = (self.cur + 63) // 64 * 64
        assert off + size <= self.hi, f"SBUF overflow allocating {name}: {off}+{size} > {self.hi}"
        self.cur = off + size
        self.n += 1
        self.offs[name] = off
        return self.nc.alloc_sbuf_tensor_at(f"{name}_{self.n}", list(shape), dtype, offset=off)

    def alloc_at(self, name, shape, dtype, off):
        self.n += 1
        return self.nc.alloc_sbuf_tensor_at(f"{name}_{self.n}", list(shape), dtype, offset=off)

    def mark(self):
        return self.cur

    def release(self, m):
        self.cur = m


def bcast_rows(ap1d, n, parts=128):
    return bass.AP(ap1d.tensor, ap1d.offset, [[0, parts], [1, n]])


def peer_topk(P, A, sc, SC, iota16, IOTA, idx_out, gw_out, OUT):
    m = A.mark()
    m16 = A.alloc("tk_m16", [128, 16, 16], F32); M16 = P.buf("tk_m16")
    ix16 = A.alloc("tk_ix16", [128, 16, 16], U32); IX16 = P.buf("tk_ix16")
    scr = A.alloc("tk_scr", [128, 256], F32); SCR = P.buf("tk_scr")
    for j in range(16):
        P.op("dve", lambda e, j=j: e.max(out=m16[:, j, 0:8], in_=sc[:, j, :]), reads=[SC], writes=[M16])
        P.op("dve", lambda e, j=j: e.match_replace(out=scr[:, 0:128], in_to_replace=m16[:, j, 0:8],
                                                   in_values=sc[:, j, :], imm_value=-1e30),
             reads=[SC, M16], writes=[SCR])
        P.op("dve", lambda e, j=j: e.max(out=m16[:, j, 8:16], in_=scr[:, 0:128]), reads=[SCR], writes=[M16])
        P.op("dve", lambda e, j=j: e.max_index(out=ix16[:, j, 0:8], in_max=m16[:, j, 0:8], in_values=sc[:, j, :]),
             reads=[SC, M16], writes=[IX16])
        P.op("dve", lambda e, j=j: e.max_index(out=ix16[:, j, 8:16], in_max=m16[:, j, 8:16], in_values=sc[:, j, :]),
             reads=[SC, M16], writes=[IX16])
    cand = A.alloc("tk_cand", [128, 8, 256], F32); CAND = P.buf("tk_cand")
    t1 = bass.AP(m16.tensor if hasattr(m16, "tensor") else m16, m16[:].offset, [list(m16[:].ap[0]), [32, 8], [1, 16], [0, 16]])
    t2 = bass.AP(m16.tensor if hasattr(m16, "tensor") else m16, m16[:].offset + 16, [list(m16[:].ap[0]), [32, 8], [0, 16], [1, 16]])
    P.op("dve", lambda e: e.tensor_tensor(out=cand[:].rearrange("p h (a b) -> p h a b", a=16), in0=t1, in1=t2, op=ALU.add), reads=[M16], writes=[CAND])
    c16 = A.alloc("tk_c16", [128, 8, 16], F32); C16 = P.buf("tk_c16")
    pos = A.alloc("tk_pos", [128, 8, 16], U32); POS = P.buf("tk_pos")
    for h in range(8):
        P.op("dve", lambda e, h=h: e.max(out=c16[:, h, 0:8], in_=cand[:, h, :]), reads=[CAND], writes=[C16])
        P.op("dve", lambda e, h=h: e.match_replace(out=scr[:],
                                                   in_to_replace=c16[:, h, 0:8],
                                                   in_values=cand[:, h, :], imm_value=-1e30),
             reads=[CAND, C16], writes=[SCR])
        P.op("dve", lambda e, h=h: e.max(out=c16[:, h, 8:16], in_=scr[:]), reads=[SCR], writes=[C16])
        P.op("dve", lambda e, h=h: e.max_index(out=pos[:, h, 0:8], in_max=c16[:, h, 0:8], in_values=cand[:, h, :]),
             reads=[CAND, C16], writes=[POS])
        P.op("dve", lambda e, h=h: e.max_index(out=pos[:, h, 8:16], in_max=c16[:, h, 8:16], in_values=cand[:, h, :]),
             reads=[CAND, C16], writes=[POS])
    au = A.alloc("tk_au", [128, 8, 16], U32); AU = P.buf("tk_au")
    bu = A.alloc("tk_bu", [128, 8, 16], U32); BU = P.buf("tk_bu")
    P.op("dve", lambda e: e.tensor_single_scalar(out=au[:], in_=pos[:], scalar=4, op=ALU.logical_shift_right),
         reads=[POS], writes=[AU])
    P.op("dve", lambda e: e.tensor_single_scalar(out=bu[:], in_=pos[:], scalar=15, op=ALU.bitwise_and),
         reads=[POS], writes=[BU])
    af = A.alloc("tk_af", [128, 8, 16], F32); AFb = P.buf("tk_af")
    bf = A.alloc("tk_bf", [128, 8, 16], F32); BFb = P.buf("tk_bf")
    ixf = A.alloc("tk_ixf", [128, 16, 16], F32); IXF = P.buf("tk_ixf")
    P.op("dve", lambda e: e.tensor_copy(out=af[:], in_=au[:]), reads=[AU], writes=[AFb])
    P.op("dve", lambda e: e.tensor_copy(out=bf[:], in_=bu[:]), reads=[BU], writes=[BFb])
    P.op("dve", lambda e: e.tensor_copy(out=ixf[:], in_=ix16[:]), reads=[IX16], writes=[IXF])
    eq = A.alloc("tk_eq", [128, 8, 16, 16], F32); EQ = P.buf("tk_eq")
    idf = A.alloc("tk_idf", [128, 2, 8, 16], F32); IDF = P.buf("tk_idf")
    p0 = list(m16[:].ap[0])
    for s_, (vf, VF) in enumerate(((af, AFb), (bf, BFb))):
        vb_ = bass.AP(vf.tensor if hasattr(vf, "tensor") else vf, vf[:].offset, [list(vf[:].ap[0]), [16, 8], [1, 16], [0, 16]])
        io_ = bass.AP(iota16.tensor if hasattr(iota16, "tensor") else iota16, iota16[:].offset,
                      [list(iota16[:].ap[0]), [0, 8], [0, 16], [1, 16]])
        P.op("dve", lambda e, vb_=vb_, io_=io_: e.tensor_tensor(out=eq[:], in0=vb_, in1=io_, op=ALU.is_equal),
             reads=[VF, IOTA], writes=[EQ])
        tab = bass.AP(ixf.tensor if hasattr(ixf, "tensor") else ixf, ixf[:].offset + 16 * s_,
                      [p0, [32, 8], [0, 16], [1, 16]])
        P.op("dve", lambda e, tab=tab: e.tensor_tensor(out=eq[:], in0=eq[:], in1=tab, op=ALU.mult),
             reads=[EQ, IXF], writes=[EQ])
        P.op("dve", lambda e, s_=s_: e.tensor_reduce(out=idf[:, s_, :, :], in_=eq[:], axis=AX.X, op=ALU.add),
             reads=[EQ], writes=[IDF])
    P.op("dve", lambda e: e.scalar_tensor_tensor(out=idf[:, 0, :, :], in0=idf[:, 0, :, :], scalar=128.0,
                                                 in1=idf[:, 1, :, :], op0=ALU.mult, op1=ALU.add),
         reads=[IDF], writes=[IDF])
    P.op("dve", lambda e: e.tensor_copy(out=idx_out, in_=idf[:, 0, :, :].rearrange("p h k -> p (h k)")),
         reads=[IDF], writes=[OUT])
    cmax = bass.AP(c16.tensor if hasattr(c16, "tensor") else c16, c16[:].offset, [list(c16[:].ap[0]), [16, 8], [0, 16]])
    P.op("dve", lambda e: e.tensor_tensor(out=bf[:], in0=c16[:], in1=cmax, op=ALU.subtract),
         reads=[C16, BFb], writes=[BFb])
    P.op("act", lambda e: e.activation(out=bf[:], in_=bf[:], func=AF.Exp), reads=[BFb], writes=[BFb])
    ssum = A.alloc("tk_ssum", [128, 8], F32); SSUM = P.buf("tk_ssum")
    P.op("dve", lambda e: e.tensor_reduce(out=ssum[:], in_=bf[:], axis=AX.X, op=ALU.add), reads=[BFb], writes=[SSUM])
    P.op("dve", lambda e: e.reciprocal(out=ssum[:], in_=ssum[:]), reads=[SSUM], writes=[SSUM])
    sb_ = bass.AP(ssum.tensor if hasattr(ssum, "tensor") else ssum, ssum[:].offset, [list(ssum[:].ap[0]), [1, 8], [0, 16]])
    P.op("dve", lambda e: e.tensor_tensor(out=gw_out.rearrange("p (h k) -> p h k", h=8), in0=bf[:], in1=sb_, op=ALU.mult),
         reads=[BFb, SSUM], writes=[OUT])
    A.release(m)


def build(nc, stage="full", dumps=()):
    P = Prog(nc)
    A = Alloc(nc)
    dumps = set(dumps)
    dump_list = []

    def din(name, shape, dt=F32):
        return nc.dram_tensor(name, list(shape), dt, kind="ExternalInput").ap()

    x_d = din("x", [T, D])
    p_d = din("p", [T, 256])
    g_mix_d = din("g_mix", [D])
    w_in_d = din("w_in", [D, 2560])
    cv_d = din("cv", [128, 40])
    wrg_d = din("wrg_bd", [128, 4, 128])
    wig_d = din("wig_bd", [128, 4, 128])
    biasT_d = din("biasT", [128, 4, 3 * 2 * 256])
    w_out_d = din("w_out", [D, D])
    identb_d = din("ident_bf", [128, 128], BF16)
    g_ffn_d = din("g_ffn", [D])
    peer_wq_d = din("peer_wq", [D, 2048])
    k1T_d = din("k1T", [128, 128])
    k2T_d = din("k2T", [128, 128])
    iota_d = din("iota16", [128, 16])
    peer_u_d = din("peer_u", [16384, D])
    peer_v_d = din("peer_v", [16384, D])
    g_ple_d = din("g_ple", [D])
    w_gate_d = din("w_ple_gate", [D, D])
    w_proj_d = din("w_ple_proj", [256, D])
    g_final_d = din("g_final", [D])
    out_d = nc.dram_tensor("out", [T, D], F32, kind="ExternalOutput").ap()

    def dump(name, ap, bufs, shape, dt=F32):
        if name not in dumps:
            return
        d = nc.dram_tensor("dbg_" + name, list(shape), dt, kind="ExternalOutput").ap()
        DB = P.buf("dbg_" + name)
        P.dma("sp", lambda e: e.dma_start(out=d, in_=ap), DB, reads=list(bufs))
        dump_list.append(DB)

    psall = nc.alloc_psum_tensor("psall", [128, 8 * 512], F32)
    ps_t = [psall[:, i * 512:(i + 1) * 512] for i in range(8)]
    ps_b = [P.buf(f"psb{i}") for i in range(8)]
    ps_rr = [0]

    def psum(idx=None):
        if idx is None:
            idx = ps_rr[0]
            ps_rr[0] = (idx + 1) % 8
        return ps_t[idx], ps_b[idx]

    def psum_pair():
        idx = (ps_rr[0] + 1) // 2 * 2 % 8
        ps_rr[0] = (idx + 2) % 8
        return psall[:, idx * 512:(idx + 2) * 512].rearrange("p (a q) -> p a q", a=2), [ps_b[idx], ps_b[idx + 1]]

    identb = A.alloc("identb", [128, 128], BF16); IDB = P.buf("identb")
    P.dma("sp", lambda e: e.dma_start(out=identb[:], in_=identb_d), IDB, writes=[IDB])
    ones_f = A.alloc("ones_f", [128, 128], F32); ONF = P.buf("ones_f")
    P.op("pool", lambda e: e.memset(ones_f[:], 1.0), writes=[ONF])
    ones_b = A.alloc("ones_b", [128, 64], BF16); ONB = P.buf("ones_b")
    P.op("pool", lambda e: e.memset(ones_b[:], 1.0), writes=[ONB])
    cv = A.alloc("cv", [128, 40], F32); CV = P.buf("cv")
    P.dma("sp", lambda e: e.dma_start(out=cv[:], in_=cv_d), CV, writes=[CV])
    gB = A.alloc("gB", [128, D], F32); GB = P.buf("gB")
    rstd_all = A.alloc("rstd_all", [128, NT], F32); RSTD = P.buf("rstd_all")

    uT = A.alloc("uT", [128, 8, T], BF16); UT = P.bufs("uT", NT)
    yT = A.alloc("yT", [128, 8, T], BF16); YT = P.bufs("yT", 8)

    uv_d = nc.dram_tensor("uv16", [16384, 2 * D], BF16, kind="Internal").ap()
    RPP = 2
    pst = [A.alloc(f"pst{j}", [128, RPP, D], F32) for j in range(2)]; PST = P.bufs("pst", 2)
    psb_ = [A.alloc(f"ppb{j}", [128, RPP, D], BF16) for j in range(2)]; PSB = P.bufs("ppb", 2)
    UVD = P.buf("uv16")

    def prepass_gen():
        n = 0
        for t_, c0 in ((peer_u_d, 0), (peer_v_d, D)):
            for blk in range(16384 // (128 * RPP)):
                j = n % 2
                n += 1
                r0 = blk * 128 * RPP
                src = t_[r0:r0 + 128 * RPP, :].rearrange("(p r) d -> p r d", r=RPP)
                dst = uv_d[r0:r0 + 128 * RPP, c0:c0 + D].rearrange("(p r) d -> p r d", r=RPP)
                P.dma("sp", lambda e, j=j, src=src: e.dma_start(out=pst[j][:], in_=src), PST[j], writes=[PST[j]])
                P.op("pool", lambda e, j=j: e.tensor_copy(out=psb_[j][:], in_=pst[j][:]),
                     reads=[PST[j]], writes=[PSB[j]])
                P.dma("sp", lambda e, j=j, dst=dst: e.dma_start(out=dst, in_=psb_[j][:]), PSB[j],
                      reads=[PSB[j]], writes=[UVD])
                yield

    pp = prepass_gen()

    def pp_adv(n):
        for _ in range(n):
            next(pp, None)

    def norm_transpose(src_tiles, gvec_d, dstT, DST, hook=None):
        m = A.mark()
        P.dma("sp", lambda e: e.dma_start(out=gB[:], in_=bcast_rows(gvec_d, D)), GB, writes=[GB])
        sq = A.alloc("nt_sq", [128, D], BF16); SQ = P.buf("nt_sq")
        ss = A.alloc("nt_ss", [128, 2], F32); SS = P.buf("nt_ss")
        xs = [A.alloc(f"nt_xs{j}", [128, D], BF16) for j in range(2)]
        XS = P.bufs("nt_xs", 2)
        for i in range(NT):
            if hook is not None:
                hook()
            src, SRC = src_tiles(i)
            P.op("act", lambda e, src=src: e.activation(out=sq[:], in_=src, func=AF.Square,
                                                         accum_out=ss[:, 0:1]),
                 reads=SRC, writes=[SQ, SS])
            P.op("act", lambda e: e.activation(out=ss[:, 1:2], in_=ss[:, 0:1], func=AF.Sqrt,
                                               scale=1.0 / D, bias=EPS),
                 reads=[SS], writes=[SS])
            P.op("dve", lambda e, i=i: e.reciprocal(out=rstd_all[:, i:i + 1], in_=ss[:, 1:2]),
                 reads=[SS], writes=[RSTD])
            j = i % 2
            P.op("dve", lambda e, src=src, i=i, j=j: e.scalar_tensor_tensor(
                out=xs[j][:], in0=src, scalar=rstd_all[:, i:i + 1], in1=gB[:],
                op0=ALU.mult, op1=ALU.mult), reads=SRC + [RSTD, GB], writes=[XS[j]])
            ps, PS = psum()
            psb = ps[:].bitcast(BF16)
            for k in range(8):
                P.op("pe", lambda e, j=j, k=k, psb=psb: e.transpose(
                    out=psb[:, k * 128:(k + 1) * 128], in_=xs[j][:, k * 128:(k + 1) * 128],
                    identity=identb[:]), reads=[XS[j], IDB], writes=[PS])
            P.op("act", lambda e, i=i, psb=psb: e.copy(
                out=dstT[:, :, i * 128:(i + 1) * 128],
                in_=psb.rearrange("p (k t) -> p k t", k=8)), reads=[PS], writes=[DST[i]])
        P.barrier()
        A.release(m)

    wsl = [A.alloc(f"wsl{j}", [128, 8, 128], BF16) for j in range(3)]
    WSL = P.bufs("wsl", 3)
    wsl_rr = [0]

    def load_wchunk(w_d, col0):
        j = wsl_rr[0]
        wsl_rr[0] = (j + 1) % 3
        src = w_d.rearrange("(k p) c -> p k c", p=128)[:, :, col0:col0 + 128]
        P.dma("pool", lambda e: e.dma_start(out=wsl[j][:], in_=src), WSL[j], writes=[WSL[j]])
        return wsl[j], WSL[j]

    def proj_chunk(w_d, col0, srcT, SRC, evac):
        w, W = load_wchunk(w_d, col0)
        for tq in range(4):
            ps, PS = psum()
            for k in range(8):
                P.op("pe", lambda e, ps=ps, k=k, tq=tq, w=w: e.matmul(
                    ps[:], lhsT=w[:, k, :], rhs=srcT[:, k, tq * 512:(tq + 1) * 512],
                    start=(k == 0), stop=(k == 7)),
                    reads=[W] + SRC[tq * 4:(tq + 1) * 4], writes=[PS])
            evac(tq, ps, PS)

    def finalize(out_bufs):
        final = []
        w = P.waited["sp"]
        for b in list(out_bufs) + dump_list:
            if b.dsem is not None and w.get(id(b.dsem), 0) < b.dcnt:
                w[id(b.dsem)] = b.dcnt
                final.append((b.dsem, b.dcnt))
        P.q["sp"].append((final, None, None, 0))
        P.emit()
        return nc

    m_phase = A.mark()
    xin = [A.alloc(f"xin{j}", [128, D], F32) for j in range(2)]
    XIN = P.bufs("xin", 2)

    def x_src(i):
        j = i % 2
        P.dma("sp", lambda e: e.dma_start(out=xin[j][:], in_=x_d[i * 128:(i + 1) * 128, :]),
              XIN[j], writes=[XIN[j]])
        return xin[j][:], [XIN[j]]

    norm_transpose(x_src, g_mix_d, uT, UT, hook=lambda: pp_adv(1))
    dump("uT", uT[:], UT, [128, 8, T], BF16)
    P.barrier()
    A.release(m_phase)
    if stage == "p1":
        return finalize([])

    m_phase = A.mark()
    y32 = A.alloc("y32", [128, 4, T], F32); Y32 = P.bufs("y32", 4)
    wrg = A.alloc("wrg", [128, 4, 128], BF16); WRG = P.buf("wrg")
    wig = A.alloc("wig", [128, 4, 128], BF16); WIG = P.buf("wig")
    P.dma("pool", lambda e: e.dma_start(out=wrg[:], in_=wrg_d), WRG, writes=[WRG])
    P.dma("pool", lambda e: e.dma_start(out=wig[:], in_=wig_d), WIG, writes=[WIG])
    nsp = A.alloc("nsp", [128, 4], F32); NSP = P.buf("nsp")
    P.op("act", lambda e: e.activation(out=nsp[:], in_=cv[:, 28:32], func=AF.Exp, scale=-1.0),
         reads=[CV], writes=[NSP])
    P.op("act", lambda e: e.activation(out=nsp[:], in_=nsp[:], func=AF.Ln, scale=1.0, bias=1.0),
         reads=[NSP], writes=[NSP])
    P.op("dve", lambda e: e.tensor_scalar(out=nsp[:], in0=nsp[:], scalar1=-8.0, scalar2=None,
                                          op0=ALU.mult), reads=[NSP], writes=[NSP])

    def big(name, dt=F32):
        return A.alloc(name, [128, T], dt), P.buf(name)

    xl, XL = big("xl"); gt, GT = big("gt"); xc, XC = big("xc"); xcb, XCB = big("xcb", BF16)
    ra, RA = big("ra"); ig, IG = big("ig"); tmp, TMP = big("tmp")

    for c in range(4):
        def ev_xl(tq, ps, PS):
            P.op("act", lambda e, ps=ps, tq=tq: e.copy(out=xl[:, tq * 512:(tq + 1) * 512], in_=ps[:]),
                 reads=[PS], writes=[XL])

        def ev_gt(tq, ps, PS):
            P.op("act", lambda e, ps=ps, tq=tq: e.copy(out=gt[:, tq * 512:(tq + 1) * 512], in_=ps[:]),
                 reads=[PS], writes=[GT])

        proj_chunk(w_in_d, c * 128, uT, UT, ev_xl)
        pp_adv(2)
        proj_chunk(w_in_d, 512 + c * 128, uT, UT, ev_gt)
        pp_adv(2)
        P.op("dve", lambda e, c=c: e.tensor_scalar(
            out=xc[:], in0=xl[:], scalar1=cv[:, c * 4 + 3:c * 4 + 4], scalar2=cv[:, 16 + c:17 + c],
            op0=ALU.mult, op1=ALU.add), reads=[XL, CV], writes=[XC])
        for s in (1, 2, 3):
            P.op("dve", lambda e, c=c, s=s: e.scalar_tensor_tensor(
                out=xc[:, s:], in0=xl[:, 0:T - s], scalar=cv[:, c * 4 + 3 - s:c * 4 + 4 - s],
                in1=xc[:, s:], op0=ALU.mult, op1=ALU.add), reads=[XL, CV, XC], writes=[XC])
        P.op("act", lambda e: e.copy(out=xcb[:], in_=xc[:]), reads=[XC], writes=[XCB])
        for tq in range(4):
            ps, PS = psum()
            P.op("pe", lambda e, ps=ps, tq=tq, c=c: e.matmul(
                ps[:], lhsT=wrg[:, c, :], rhs=xcb[:, tq * 512:(tq + 1) * 512], start=True, stop=True),
                reads=[WRG, XCB], writes=[PS])
            P.op("act", lambda e, ps=ps, tq=tq, c=c: e.activation(
                out=ra[:, tq * 512:(tq + 1) * 512], in_=ps[:], func=AF.Sigmoid,
                bias=cv[:, 20 + c:21 + c], scale=1.0), reads=[PS, CV], writes=[RA])
        for tq in range(4):
            ps, PS = psum()
            P.op("pe", lambda e, ps=ps, tq=tq, c=c: e.matmul(
                ps[:], lhsT=wig[:, c, :], rhs=xcb[:, tq * 512:(tq + 1) * 512], start=True, stop=True),
                reads=[WIG, XCB], writes=[PS])
            P.op("act", lambda e, ps=ps, tq=tq, c=c: e.activation(
                out=ig[:, tq * 512:(tq + 1) * 512], in_=ps[:], func=AF.Sigmoid,
                bias=cv[:, 24 + c:25 + c], scale=1.0), reads=[PS, CV], writes=[IG])
        P.op("act", lambda e, c=c: e.activation(out=ra[:], in_=ra[:], func=AF.Exp,
                                                scale=nsp[:, c:c + 1]), reads=[RA, NSP], writes=[RA])
        P.op("dve", lambda e: e.tensor_tensor(out=tmp[:], in0=ra[:], in1=ra[:], op=ALU.mult),
             reads=[RA], writes=[TMP])
        P.op("act", lambda e: e.activation(out=tmp[:], in_=tmp[:], func=AF.Sqrt, scale=-1.0, bias=1.0),
             reads=[TMP], writes=[TMP])
        P.op("dve", lambda e: e.tensor_tensor(out=ig[:], in0=ig[:], in1=xc[:], op=ALU.mult),
             reads=[IG, XC], writes=[IG])
        P.op("dve", lambda e: e.tensor_tensor(out=ig[:], in0=ig[:], in1=tmp[:], op=ALU.mult),
             reads=[IG, TMP], writes=[IG])
        P.op("dve", lambda e: e.tensor_tensor_scan(out=tmp[:], data0=ra[:], data1=ig[:], initial=0.0,
                                                   op0=ALU.mult, op1=ALU.add),
             reads=[RA, IG], writes=[TMP])
        P.op("act", lambda e: e.activation(out=gt[:], in_=gt[:], func=AF.Gelu_apprx_tanh),
             reads=[GT], writes=[GT])
        P.op("dve", lambda e, c=c: e.tensor_tensor(out=y32[:, c, :], in0=tmp[:], in1=gt[:], op=ALU.mult),
             reads=[TMP, GT], writes=[Y32[c]])

    def group_rmsnorm(gcol0, ybase, y32, Y32, tmp, TMP, ra, RA):
        pss = [psum(i) for i in range(4)]
        for c in range(4):
            P.op("act", lambda e, c=c: e.activation(out=tmp[:], in_=y32[:, c, :], func=AF.Square),
                 reads=[Y32[c]], writes=[TMP])
            for tq in range(4):
                ps, PS = pss[tq]
                P.op("pe", lambda e, ps=ps, tq=tq, c=c: e.matmul(
                    ps[:], lhsT=ones_f[:], rhs=tmp[:, tq * 512:(tq + 1) * 512],
                    start=(c == 0), stop=(c == 3)), reads=[ONF, TMP], writes=[PS])
        for tq in range(4):
            ps, PS = pss[tq]
            P.op("act", lambda e, ps=ps, tq=tq: e.activation(
                out=ra[:, tq * 512:(tq + 1) * 512], in_=ps[:], func=AF.Sqrt, scale=1.0 / 512, bias=EPS),
                reads=[PS], writes=[RA])
        P.op("dve", lambda e: e.reciprocal(out=ra[:], in_=ra[:]), reads=[RA], writes=[RA])
        for c in range(4):
            P.op("dve", lambda e, c=c: e.scalar_tensor_tensor(
                out=yT[:, ybase + c, :], in0=y32[:, c, :], scalar=cv[:, gcol0 + c:gcol0 + c + 1],
                in1=ra[:], op0=ALU.mult, op1=ALU.mult), reads=[Y32[c], CV, RA], writes=[YT[ybase + c]])

    dump("y_lru", y32[:], Y32, [128, 4, T])
    if stage == "p2a":
        return finalize([])
    group_rmsnorm(32, 0, y32, Y32, tmp, TMP, ra, RA)
    if stage == "p2":
        dump("yT", yT[:], YT, [128, 8, T], BF16)
    P.barrier()
    A.release(m_phase)
    if stage == "p2":
        return finalize([])

    m_phase = A.mark()
    y3 = A.alloc("y32a", [128, 4, T], F32); Y3 = P.bufs("y32a", 4)
    ra3, RA3 = big("ra2"); tmp3, TMP3 = big("tmp2")
    qT, QT = big("qT", BF16); kT, KT = big("kT", BF16)
    dacc, DACC = big("dacc")
    bias_sb = A.alloc("bias_sb", [128, 3, 2, 256], F32); BIAS = P.buf("bias_sb")
    vb = A.alloc("vb", [128, 16, 128], BF16); VB = P.buf("vb")
    tS = [A.alloc(f"tS{j}", [128, 2, 256], F32) for j in range(2)]; TS = P.bufs("tS", 2)
    pT = [A.alloc(f"pT{j}", [128, 2, 256], BF16) for j in range(2)]; PT = P.bufs("pT", 2)
    it = 0
    for c in range(4):
        OACC = Y3[c]
        P.dma("sp", lambda e, c=c: e.dma_start(
            out=bias_sb[:].rearrange("p a b q -> p (a b q)"), in_=biasT_d[:, c, :]), BIAS, writes=[BIAS])

        def ev_q(tq, ps, PS):
            P.op("act", lambda e, ps=ps, tq=tq: e.activation(
                out=qT[:, tq * 512:(tq + 1) * 512], in_=ps[:], func=AF.Copy, scale=0.125),
                reads=[PS], writes=[QT])

        def ev_k(tq, ps, PS):
            P.op("act", lambda e, ps=ps, tq=tq: e.copy(out=kT[:, tq * 512:(tq + 1) * 512], in_=ps[:]),
                 reads=[PS], writes=[KT])

        proj_chunk(w_in_d, 1024 + c * 128, uT, UT, ev_q)
        proj_chunk(w_in_d, 1536 + c * 128, uT, UT, ev_k)
        wv, WV = load_wchunk(w_in_d, 2048 + c * 128)
        P.op("pool", lambda e, c=c: e.memset(y3[:, c, :], 0.0), writes=[OACC])
        P.op("pool", lambda e: e.memset(dacc[:], 0.0), writes=[DACC])
        for bi, d in enumerate((1, 4, 16)):
            nb = 16 // d
            for g4 in range(4):
                ps, PS = psum()
                for u4 in range(4):
                    ti = g4 * 4 + u4
                    r, m = ti // nb, ti % nb
                    t0 = r + d * 128 * m
                    for k in range(8):
                        P.op("pe", lambda e, ps=ps, u4=u4, k=k, t0=t0, d=d, wv=wv: e.matmul(
                            ps[:, u4 * 128:(u4 + 1) * 128],
                            lhsT=uT[:, k, t0:t0 + d * 127 + 1:d], rhs=wv[:, k, :],
                            start=(k == 0), stop=(k == 7)), reads=[WV] + UT, writes=[PS])
                P.op("act", lambda e, ps=ps, g4=g4: e.copy(
                    out=vb[:, g4 * 4:(g4 + 1) * 4, :], in_=ps[:].rearrange("p (a b) -> p a b", a=4)),
                    reads=[PS], writes=[VB])
            for r in range(d):
                for m in range(nb):
                    nq = 256 if m + 1 < nb else 128
                    t0 = r + d * 128 * m
                    kend = t0 + d * 127 + 1
                    qend = t0 + d * (nq - 1) + 1
                    j = it % 2
                    it += 1
                    if it % 3 == 0:
                        pp_adv(1)
                    ps, PSP = psum_pair()
                    for hh in range(2):
                        p0 = hh * 64
                        P.op("pe", lambda e, ps=ps, hh=hh, p0=p0, t0=t0, kend=kend, qend=qend, d=d, nq=nq: e.matmul(
                            ps[:, hh, 0:nq], lhsT=kT[p0:p0 + 64, t0:kend:d],
                            rhs=qT[p0:p0 + 64, t0:qend:d], start=True, stop=True),
                            reads=[KT, QT], writes=[PSP[hh]])
                    P.op("dve", lambda e, ps=ps, j=j, bi=bi, nq=nq: e.tensor_tensor(
                        out=tS[j][:, :, 0:nq], in0=ps[:, :, 0:nq],
                        in1=bias_sb[:, bi, :, 0:nq], op=ALU.add), reads=PSP + [BIAS], writes=[TS[j]])
                    P.op("act", lambda e, j=j, nq=nq: e.activation(
                        out=pT[j][:, :, 0:nq], in_=tS[j][:, :, 0:nq], func=AF.Exp),
                        reads=[TS[j]], writes=[PT[j]])
                    ps2, PS2 = psum()
                    ti = r * nb + m
                    for hh in range(2):
                        p0 = hh * 64
                        P.op("pe", lambda e, ps2=ps2, hh=hh, p0=p0, ti=ti, j=j, nq=nq: e.matmul(
                            ps2[p0:p0 + 64, 0:nq], lhsT=vb[:, ti, hh * 64:(hh + 1) * 64],
                            rhs=pT[j][:, hh, 0:nq], start=True, stop=True),
                            reads=[VB, PT[j]], writes=[PS2])
                        P.op("pe", lambda e, ps2=ps2, hh=hh, p0=p0, j=j, nq=nq: e.matmul(
                            ps2[p0:p0 + 64, 256:256 + nq], lhsT=ones_b[:, 0:64],
                            rhs=pT[j][:, hh, 0:nq], start=True, stop=True),
                            reads=[ONB, PT[j]], writes=[PS2])
                    P.op("dve", lambda e, ps2=ps2, c=c, t0=t0, qend=qend, d=d, nq=nq: e.tensor_tensor(
                        out=y3[:, c, t0:qend:d], in0=y3[:, c, t0:qend:d], in1=ps2[:, 0:nq], op=ALU.add),
                        reads=[PS2, OACC], writes=[OACC])
                    P.op("dve", lambda e, ps2=ps2, t0=t0, qend=qend, d=d, nq=nq: e.tensor_tensor(
                        out=dacc[:, t0:qend:d], in0=dacc[:, t0:qend:d], in1=ps2[:, 256:256 + nq], op=ALU.add),
                        reads=[PS2, DACC], writes=[DACC])
        P.op("dve", lambda e: e.reciprocal(out=dacc[:], in_=dacc[:]), reads=[DACC], writes=[DACC])
        P.op("dve", lambda e, c=c: e.tensor_tensor(out=y3[:, c, :], in0=y3[:, c, :], in1=dacc[:], op=ALU.mult),
             reads=[OACC, DACC], writes=[OACC])
    dump("y_att", y3[:], Y3, [128, 4, T])
    if stage == "p3a":
        return finalize([])
    group_rmsnorm(36, 4, y3, Y3, tmp3, TMP3, ra3, RA3)
    if stage != "p2":
        dump("yT", yT[:], YT, [128, 8, T], BF16)
    P.barrier()
    A.release(m_phase)

    xt = A.alloc("xt", [128, NT, D], F32); XT = P.bufs("xt", NT)
    m_phase = A.mark()
    wo = A.alloc("wo", [128, 8, D], BF16); WO = P.buf("wo")
    P.dma("pool", lambda e: e.dma_start(out=wo[:], in_=w_out_d.rearrange("(k p) c -> p k c", p=128)),
          WO, writes=[WO])
    for i in range(NT):
        P.dma("sp", lambda e, i=i: e.dma_start(out=xt[:, i, :], in_=x_d[i * 128:(i + 1) * 128, :]),
              XT[i], writes=[XT[i]])
    for i in range(NT):
        for hf in range(2):
            pp_adv(1)
            ps, PS = psum()
            for k in range(8):
                P.op("pe", lambda e, ps=ps, k=k, i=i, hf=hf: e.matmul(
                    ps[:], lhsT=yT[:, k, i * 128:(i + 1) * 128], rhs=wo[:, k, hf * 512:(hf + 1) * 512],
                    start=(k == 0), stop=(k == 7)), reads=[WO] + YT, writes=[PS])
            P.op("dve", lambda e, ps=ps, i=i, hf=hf: e.tensor_tensor(
                out=xt[:, i, hf * 512:(hf + 1) * 512], in0=xt[:, i, hf * 512:(hf + 1) * 512],
                in1=ps[:], op=ALU.add), reads=[PS, XT[i]], writes=[XT[i]])
    for _ in pp:
        pass
    P.barrier()
    A.release(m_phase)

    if stage == "mixer":
        for i in range(NT):
            P.dma("sp", lambda e, i=i: e.dma_start(out=out_d[i * 128:(i + 1) * 128, :], in_=xt[:, i, :]),
                  XT[i], reads=[XT[i]])
        return finalize(XT)

    def xt_src(i):
        return xt[:, i, :], [XT[i]]

    m_phase = A.mark()
    norm_transpose(xt_src, g_ffn_d, uT, UT)
    wq = yT
    WQ = P.buf("wq")
    wq_src = peer_wq_d.rearrange("(k p) c -> p k c", p=128)
    for hf in range(2):
        P.dma("pool", lambda e, hf=hf: e.dma_start(out=wq[:, :, hf * 1024:(hf + 1) * 1024],
                                                   in_=wq_src[:, :, hf * 1024:(hf + 1) * 1024]),
              WQ, writes=[WQ])
    kTs = A.alloc("kTs", [128, 2, 128], F32); KTS = P.buf("kTs")
    P.dma("sp", lambda e: e.dma_start(out=kTs[:, 0, :], in_=k1T_d), KTS, writes=[KTS])
    P.dma("sp", lambda e: e.dma_start(out=kTs[:, 1, :], in_=k2T_d), KTS, writes=[KTS])
    iota16 = A.alloc("iota16", [128, 16], F32); IOTA = P.buf("iota16")
    P.dma("sp", lambda e: e.dma_start(out=iota16[:], in_=iota_d), IOTA, writes=[IOTA])
    idx_all = A.alloc("idx_all", [128, NT, 128], I32); IDX = P.bufs("idx_all", NT)
    gw_all = A.alloc("gw_all", [128, NT, 128], F32)
    m_sub = A.mark()
    assert A.offs["ppb1"] == A.offs["ppb0"] + RPP * D * 2
    qTt = A.alloc_at("qTt", [128, 16, 128], F32, A.offs["pst0"]); QTT = P.buf("qTt")
    scs = [A.alloc_at("sc0", [128, 16, 128], F32, A.offs["pst1"]),
           A.alloc_at("sc1", [128, 16, 128], F32, A.offs["ppb0"])]
    SCS = P.bufs("sc", 2)
    for i in range(NT):
        for jg in range(4):
            ps, PS = psum()
            for u in range(4):
                j = jg * 4 + u
                for k in range(8):
                    P.op("pe", lambda e, ps=ps, u=u, j=j, k=k, i=i: e.matmul(
                        ps[:, u * 128:(u + 1) * 128], lhsT=wq[:, k, j * 128:(j + 1) * 128],
                        rhs=uT[:, k, i * 128:(i + 1) * 128], start=(k == 0), stop=(k == 7)),
                        reads=[WQ, UT[i]], writes=[PS])
            P.op("act", lambda e, ps=ps, jg=jg: e.copy(
                out=qTt[:, jg * 4:(jg + 1) * 4, :], in_=ps[:].rearrange("p (a b) -> p a b", a=4)),
                reads=[PS], writes=[QTT])
        sc, SC = scs[i % 2], SCS[i % 2]
        for jg in range(4):
            ps, PS = psum()
            for u in range(4):
                j = jg * 4 + u
                P.op("pe", lambda e, ps=ps, u=u, j=j: e.matmul(
                    ps[:, u * 128:(u + 1) * 128], lhsT=qTt[:, j, :], rhs=kTs[:, j % 2, :],
                    start=True, stop=True), reads=[QTT, KTS], writes=[PS])
            P.op("act", lambda e, ps=ps, jg=jg, sc=sc: e.copy(
                out=sc[:, jg * 4:(jg + 1) * 4, :], in_=ps[:].rearrange("p (a b) -> p a b", a=4)),
                reads=[PS], writes=[SC])
        if i == 0:
            dump("qTt0", qTt[:], [QTT], [128, 16, 128])
            dump("sc0", sc[:], [SC], [128, 16, 128])
        peer_topk(P, A, sc, SC, iota16, IOTA, idx_all[:, i, :], gw_all[:, i, :], IDX[i])
    dump("idx_all", idx_all[:], IDX, [128, NT, 128], I32)
    dump("gw_all", gw_all[:], IDX, [128, NT, 128])
    P.barrier()
    A.release(m_sub)
    NS = 16
    GE = 8
    base_off = A.offs["uT"]
    assert A.offs["yT"] == base_off + 8 * T * 2
    gs = [A.alloc_at(f"gs{j}", [128, 2 * D], BF16, base_off + j * 4096) for j in range(NS)]
    GS = P.bufs("gs", NS)
    xn32 = A.alloc("xn32", [128, D], F32); XN = P.buf("xn32")
    junk = A.alloc("junk", [128, D], BF16); JUNK = P.buf("junk")
    dots = A.alloc("dots", [128, 128], F32); DOTS = P.bufs("dots", 128)
    wvg = A.alloc("wvg", [128, 128], F32); WVG = P.bufs("wvg", 16)
    wv_t = A.alloc("wv_t", [128, 128], F32); WVT = P.bufs("wv_t", 16)
    NDG = 8
    dg = [A.alloc(f"dg{j}", [128, 128], BF16) for j in range(NDG)]; DG = P.bufs("dg", NDG)
    gi = 0
    di = 0
    for i in range(NT):
        P.op("dve", lambda e, i=i: e.scalar_tensor_tensor(
            out=xn32[:], in0=xt[:, i, :], scalar=rstd_all[:, i:i + 1], in1=gB[:],
            op0=ALU.mult, op1=ALU.mult), reads=[XT[i], RSTD, GB], writes=[XN])
        bank = 4 * (i % 2)
        psy = [psum(bank), psum(bank + 1)]
        for g in range(128 // GE):
            sls = []
            for u in range(GE):
                j = g * GE + u
                sl = gi % NS
                gi += 1
                sls.append(sl)
                P.dma("pool", lambda e, sl=sl, i=i, j=j: e.indirect_dma_start(
                    out=gs[sl][:], out_offset=None, in_=uv_d,
                    in_offset=bass.IndirectOffsetOnAxis(ap=idx_all[:, i, j:j + 1], axis=0)),
                    GS[sl], reads=[IDX[i]], writes=[GS[sl]])
            for u in range(GE):
                j = g * GE + u
                sl = sls[u]
                P.op("dve", lambda e, sl=sl, j=j: e.scalar_tensor_tensor(
                    out=junk[:], in0=gs[sl][:, 0:D], scalar=1.0, in1=xn32[:], op0=ALU.mult, op1=ALU.mult,
                    accum_out=dots[:, j:j + 1]), reads=[GS[sl], XN], writes=[JUNK, DOTS[j]])
            c0, c1 = g * GE, (g + 1) * GE
            P.op("act", lambda e, c0=c0, c1=c1: e.activation(out=wvg[:, c0:c1], in_=dots[:, c0:c1],
                                                            func=AF.Gelu_apprx_tanh),
                 reads=DOTS[c0:c1], writes=[WVG[g]])
            P.op("dve", lambda e, i=i, c0=c0, c1=c1: e.tensor_tensor(
                out=wv_t[:, c0:c1], in0=wvg[:, c0:c1], in1=gw_all[:, i, c0:c1], op=ALU.mult),
                reads=[WVG[g], IDX[i]], writes=[WVT[g]])
            for u in range(GE):
                j = g * GE + u
                sl = sls[u]
                dsl = di % NDG
                di += 1
                P.op("act", lambda e, dsl=dsl, j=j: e.activation(
                    out=dg[dsl][:], in_=identb[:], func=AF.Copy, scale=wv_t[:, j:j + 1]),
                    reads=[IDB, WVT[g]], writes=[DG[dsl]])
                for hf in range(2):
                    ps, PS = psy[hf]
                    P.op("pe", lambda e, ps=ps, dsl=dsl, sl=sl, hf=hf, j=j: e.matmul(
                        ps[:], lhsT=dg[dsl][:], rhs=gs[sl][:, D + hf * 512:D + (hf + 1) * 512],
                        start=(j == 0), stop=(j == 127)), reads=[DG[dsl], GS[sl]], writes=[PS])
        for hf in range(2):
            ps, PS = psy[hf]
            P.op("dve", lambda e, ps=ps, i=i, hf=hf: e.tensor_tensor(
                out=xt[:, i, hf * 512:(hf + 1) * 512], in0=xt[:, i, hf * 512:(hf + 1) * 512],
                in1=ps[:], op=ALU.add), reads=[PS, XT[i]], writes=[XT[i]])
    P.barrier()
    A.release(m_phase)
    if stage == "ffn":
        for i in range(NT):
            P.dma("sp", lambda e, i=i: e.dma_start(out=out_d[i * 128:(i + 1) * 128, :], in_=xt[:, i, :]),
                  XT[i], reads=[XT[i]])
        return finalize(XT)

    m_phase = A.mark()
    norm_transpose(xt_src, g_ple_d, uT, UT)
    wg = A.alloc("wg", [128, 8, D], BF16); WG = P.buf("wg")
    P.dma("pool", lambda e: e.dma_start(out=wg[:], in_=w_gate_d.rearrange("(k p) c -> p k c", p=128)),
          WG, writes=[WG])
    wpp = A.alloc("wpp", [128, 2, D], BF16); WPP = P.buf("wpp")
    P.dma("pool", lambda e: e.dma_start(out=wpp[:], in_=w_proj_d.rearrange("(k p) c -> p k c", p=128)),
          WPP, writes=[WPP])
    pb = [A.alloc(f"pb{j}", [128, 256], BF16) for j in range(2)]; PB = P.bufs("pb", 2)
    pTt = [A.alloc(f"pTt{j}", [128, 2, 128], BF16) for j in range(2)]; PTT = P.bufs("pTt", 2)
    sg = [A.alloc(f"sg{j}", [128, 512], F32) for j in range(2)]; SG = P.bufs("sg", 2)
    it = 0
    for i in range(NT):
        j = i % 2
        P.dma("pool", lambda e, i=i, j=j: e.dma_start(out=pb[j][:], in_=p_d[i * 128:(i + 1) * 128, :]),
              PB[j], writes=[PB[j]])
        ps, PS = psum()
        psb = ps[:].bitcast(BF16)
        for kk in range(2):
            P.op("pe", lambda e, j=j, kk=kk, psb=psb: e.transpose(
                out=psb[:, kk * 128:(kk + 1) * 128], in_=pb[j][:, kk * 128:(kk + 1) * 128],
                identity=identb[:]), reads=[PB[j], IDB], writes=[PS])
        P.op("act", lambda e, j=j, psb=psb: e.copy(
            out=pTt[j][:], in_=psb[:, 0:256].rearrange("p (k t) -> p k t", k=2)), reads=[PS], writes=[PTT[j]])
        for hf in range(2):
            jj = it % 2
            it += 1
            ps1, PS1 = psum()
            for k in range(8):
                P.op("pe", lambda e, ps1=ps1, k=k, i=i, hf=hf: e.matmul(
                    ps1[:], lhsT=uT[:, k, i * 128:(i + 1) * 128], rhs=wg[:, k, hf * 512:(hf + 1) * 512],
                    start=(k == 0), stop=(k == 7)), reads=[WG, UT[i]], writes=[PS1])
            P.op("act", lambda e, ps1=ps1, jj=jj: e.activation(out=sg[jj][:], in_=ps1[:], func=AF.Sigmoid),
                 reads=[PS1], writes=[SG[jj]])
            ps2, PS2 = psum()
            for kk in range(2):
                P.op("pe", lambda e, ps2=ps2, kk=kk, j=j, hf=hf: e.matmul(
                    ps2[:], lhsT=pTt[j][:, kk, :], rhs=wpp[:, kk, hf * 512:(hf + 1) * 512],
                    start=(kk == 0), stop=(kk == 1)), reads=[WPP, PTT[j]], writes=[PS2])
            P.op("dve", lambda e, ps2=ps2, jj=jj: e.tensor_tensor(out=sg[jj][:], in0=sg[jj][:], in1=ps2[:], op=ALU.mult),
                 reads=[PS2, SG[jj]], writes=[SG[jj]])
            P.op("dve", lambda e, jj=jj, i=i, hf=hf: e.tensor_tensor(
                out=xt[:, i, hf * 512:(hf + 1) * 512], in0=xt[:, i, hf * 512:(hf + 1) * 512],
                in1=sg[jj][:], op=ALU.add), reads=[SG[jj], XT[i]], writes=[XT[i]])
    P.barrier()
    A.release(m_phase)

    P.dma("sp", lambda e: e.dma_start(out=gB[:], in_=bcast_rows(g_final_d, D)), GB, writes=[GB])
    sqf = A.alloc("fn_sq", [128, D], BF16); SQF = P.buf("fn_sq")
    ssf = A.alloc("fn_ss", [128, 2], F32); SSF = P.buf("fn_ss")
    for i in range(NT):
        P.op("act", lambda e, i=i: e.activation(out=sqf[:], in_=xt[:, i, :], func=AF.Square,
                                                accum_out=ssf[:, 0:1]), reads=[XT[i]], writes=[SQF, SSF])
        P.op("act", lambda e: e.activation(out=ssf[:, 1:2], in_=ssf[:, 0:1], func=AF.Sqrt,
                                           scale=1.0 / D, bias=EPS), reads=[SSF], writes=[SSF])
        P.op("dve", lambda e, i=i: e.reciprocal(out=rstd_all[:, i:i + 1], in_=ssf[:, 1:2]),
             reads=[SSF], writes=[RSTD])
        P.op("dve", lambda e, i=i: e.scalar_tensor_tensor(
            out=xt[:, i, :], in0=xt[:, i, :], scalar=rstd_all[:, i:i + 1], in1=gB[:],
            op0=ALU.mult, op1=ALU.mult), reads=[XT[i], RSTD, GB], writes=[XT[i]])
        P.dma("sp", lambda e, i=i: e.dma_start(out=out_d[i * 128:(i + 1) * 128, :], in_=xt[:, i, :]),
              XT[i], reads=[XT[i]])
    return finalize(XT)


def _t5_bucket(dist):
    max_exact = 16
    n = np.maximum(dist, 0)
    nf = np.maximum(n, 1).astype(np.float32)
    large = max_exact + (np.log(nf / max_exact) / np.float32(np.log(2048 / max_exact))
                         * (32 - max_exact)).astype(np.int32)
    large = np.minimum(large, 31)
    return np.where(n < max_exact, n, large)


def _bucket_table():
    import jax, jax.numpy as jnp
    return None


def prep_shared(inp):
    import ml_dtypes
    f32 = np.float32
    sh = {}
    sh["g_mix"] = np.ascontiguousarray(inp["g_mix"][0], f32)
    sh["w_in"] = np.ascontiguousarray(inp["w_in"][0], f32)
    sh["w_out"] = np.ascontiguousarray(inp["w_out"][0], f32)
    cv = np.zeros((128, 40), f32)
    cw = inp["conv_w"][0]
    for c in range(4):
        for j in range(4):
            cv[:, c * 4 + j] = cw[j, c * 128:(c + 1) * 128]
    for n, key in enumerate(["conv_b", "b_rg", "b_ig", "lru_lambda", "g_lru_out", "g_attn_out"]):
        cv[:, 16 + 4 * n:20 + 4 * n] = inp[key][0].reshape(4, 128).T
    sh["cv"] = cv
    for name, key in (("wrg_bd", "w_rg"), ("wig_bd", "w_ig")):
        w = inp[key][0]
        bd = np.zeros((128, 4, 128), f32)
        for g in range(8):
            c, l = g // 2, g % 2
            bd[l * 64:(l + 1) * 64, c, l * 64:(l + 1) * 64] = w[g]
        sh[name] = bd
    rb = inp["rel_bias"].astype(f32)
    k = np.arange(128)[:, None]
    q = np.arange(256)[None, :]
    dist = q - k
    valid = (dist >= 0) & (dist <= 128)
    bt = np.full((128, 4, 3, 2, 256), NEG, f32)
    for bi, d in enumerate((1, 4, 16)):
        bucket = _t5_bucket(dist * d)
        for h in range(8):
            vals = rb[bucket, h]
            bt[:, h // 2, bi, h % 2, :] = np.where(valid, vals, f32(NEG))
    sh["biasT"] = bt.reshape(128, 4, 3 * 2 * 256)
    sh["ident_bf"] = np.eye(128, dtype=f32).astype(ml_dtypes.bfloat16)
    sh["g_ffn"] = np.ascontiguousarray(inp["g_ffn"][0], f32)
    sh["peer_wq"] = np.ascontiguousarray(inp["peer_wq"][0], f32)
    sh["k1T"] = np.ascontiguousarray(inp["peer_k1"][0].T, f32)
    sh["k2T"] = np.ascontiguousarray(inp["peer_k2"][0].T, f32)
    sh["iota16"] = np.tile(np.arange(16, dtype=f32)[None], (128, 1))
    sh["peer_u"] = np.ascontiguousarray(inp["peer_u"][0], f32)
    sh["peer_v"] = np.ascontiguousarray(inp["peer_v"][0], f32)
    sh["g_ple"] = np.ascontiguousarray(inp["g_ple"][0], f32)
    sh["w_ple_gate"] = np.ascontiguousarray(inp["w_ple_gate"][0], f32)
    sh["w_ple_proj"] = np.ascontiguousarray(inp["w_ple_proj"][0], f32)
    sh["g_final"] = np.ascontiguousarray(inp["g_final"], f32)
    return sh


_NC_CACHE = {}


def kernel(**inputs):
    inp = {k: np.asarray(v) for k, v in inputs.items()}
    sh = prep_shared(inp)
    if "nc" not in _NC_CACHE:
        nc = bass.Bass("TRN2", target_bir_lowering=False)
        build(nc)
        _NC_CACHE["nc"] = nc
    nc = _NC_CACHE["nc"]
    in_maps = []
    for b in range(8):
        m = dict(sh)
        m["x"] = np.ascontiguousarray(inp["x"][b], np.float32)
        m["p"] = np.ascontiguousarray(inp["p"][0, b], np.float32)
        in_maps.append(m)
    res = run_bass_kernel_spmd(nc, in_maps, core_ids=list(range(8)))
    return np.stack([np.asarray(r["out"]) for r in res.results], axis=0).astype(np.float32)
```

```python
import numpy as np
import concourse.bass as bass
import concourse.mybir as mybir
from concourse.bass_utils import run_bass_kernel_spmd

F32 = mybir.dt.float32
BF16 = mybir.dt.bfloat16
U32 = mybir.dt.uint32
I32 = mybir.dt.int32
AF = mybir.ActivationFunctionType
ALU = mybir.AluOpType
AX = mybir.AxisListType

T = 2048
D = 1024
NT = 16
EPS = 1e-6
NEG = -30000.0


class Buf:
    __slots__ = ("name", "lastw", "reads", "dsem", "dcnt")

    def __init__(self, name):
        self.name = name
        self.lastw = None
        self.reads = {}
        self.dsem = None
        self.dcnt = 0


class Prog:
    ENG = ("pe", "dve", "act", "pool", "sp")

    def __init__(self, nc):
        self.nc = nc
        self.q = {e: [] for e in self.ENG}
        self.sem = {e: nc.alloc_semaphore(f"prog_{e}") for e in self.ENG}
        self.cnt = {e: 0 for e in self.ENG}
        self.waited = {e: {} for e in self.ENG}
        self.nbuf = 0
        self.dsems = []
        self.free_dsems = []

    def buf(self, name=None):
        self.nbuf += 1
        return Buf(name or f"b{self.nbuf}")

    def bufs(self, name, n):
        return [self.buf(f"{name}{i}") for i in range(n)]

    def _tokens(self, eng, reads, writes):
        need = {}

        def add(tok):
            if tok is None:
                return
            s, v = tok
            if need.get(id(s), (None, 0))[1] < v:
                need[id(s)] = (s, v)

        for b in reads:
            add(b.lastw)
        for b in writes:
            add(b.lastw)
            for tok in b.reads.values():
                add(tok)
        own = self.sem[eng]
        waits = []
        w = self.waited[eng]
        for s, v in need.values():
            if eng == "pe" and s is own:
                continue
            if w.get(id(s), 0) < v:
                w[id(s)] = v
                waits.append((s, v))
        return waits

    def _commit(self, tok, reads, writes):
        s, v = tok
        for b in reads:
            if b.reads.get(id(s), (None, 0))[1] < v:
                b.reads[id(s)] = (s, v)
        for b in writes:
            b.lastw = tok
            b.reads = {}

    def op(self, eng, fn, reads=(), writes=()):
        waits = self._tokens(eng, reads, writes)
        self.cnt[eng] += 1
        tok = (self.sem[eng], self.cnt[eng])
        self.q[eng].append((waits, fn, tok[0], 1))
        self._commit(tok, reads, writes)
        return tok

    def dma(self, eng, fn, sb, reads=(), writes=()):
        waits = self._tokens(eng, reads, writes)
        if sb.dsem is None:
            sb.dsem = self.nc.alloc_semaphore(f"dma_{sb.name}")
            self.dsems.append(sb)
        sb.dcnt += 16
        tok = (sb.dsem, sb.dcnt)
        self.q[eng].append((waits, fn, sb.dsem, 16))
        self._commit(tok, reads, writes)
        return tok

    def share_dsem(self, bufs):
        return bufs

    def barrier(self):
        toks = [(self.sem[e], self.cnt[e]) for e in self.ENG if self.cnt[e] > 0]
        toks += [(b.dsem, b.dcnt) for b in self.dsems]
        for e in self.ENG:
            waits = []
            w = self.waited[e]
            for s, v in toks:
                if e == "pe" and s is self.sem["pe"]:
                    continue
                if w.get(id(s), 0) < v:
                    w[id(s)] = v
                    waits.append((s, v))
            if waits:
                self.q[e].append((waits, None, None, 0))

    def emit(self):
        nc = self.nc
        with nc.Block() as block:
            @block.tensor
            def _(e):
                self._emit("pe", e)

            @block.vector
            def _(e):
                self._emit("dve", e)

            @block.scalar
            def _(e):
                self._emit("act", e)

            @block.gpsimd
            def _(e):
                self._emit("pool", e)

            @block.sync
            def _(e):
                self._emit("sp", e)

    def _emit(self, name, e):
        for waits, fn, sem, inc in self.q[name]:
            for s, v in waits:
                e.wait_ge(s, v)
            if fn is not None:
                ins = fn(e)
                ins.then_inc(sem, inc)


class Alloc:
    def __init__(self, nc):
        self.nc = nc
        self.lo = (int(nc.sbuf_base) + 63) // 64 * 64
        self.hi = int(nc.sbuf_top)
        self.cur = self.lo
        self.n = 0
        self.offs = {}

    def alloc(self, name, shape, dtype):
        size = int(np.prod(shape[1:])) * mybir.dt.size(dtype)
        off = (self.cur + 63) // 64 * 64
        assert off + size <= self.hi, f"SBUF overflow allocating {name}: {off}+{size} > {self.hi}"
        self.cur = off + size
        self.n += 1
        self.offs[name] = off
        return self.nc.alloc_sbuf_tensor_at(f"{name}_{self.n}", list(shape), dtype, offset=off)

    def alloc_at(self, name, shape, dtype, off):
        self.n += 1
        return self.nc.alloc_sbuf_tensor_at(f"{name}_{self.n}", list(shape), dtype, offset=off)

    def mark(self):
        return self.cur

    def release(self, m):
        self.cur = m


def bcast_rows(ap1d, n, parts=128):
    return bass.AP(ap1d.tensor, ap1d.offset, [[0, parts], [1, n]])


def peer_topk(P, A, sc, SC, iota16, IOTA, idx_out, gw_out, OUT):
    m = A.mark()
    m16 = A.alloc("tk_m16", [128, 16, 16], F32)
    M16A = P.bufs("tk_m16a", 16); M16B = P.bufs("tk_m16b", 16)
    ix16 = A.alloc("tk_ix16", [128, 16, 16], U32)
    IXA = P.bufs("tk_ixa", 16); IXB = P.bufs("tk_ixb", 16)
    cand = A.alloc("tk_cand", [128, 8, 256], F32); SCR = P.bufs("tk_scr", 16)
    scr = cand[:].rearrange("p h (s k) -> p (h s) k", s=2)
    for j in range(16):
        P.op("dve", lambda e, j=j: e.max(out=m16[:, j, 0:8], in_=sc[:, j, :]), reads=[SC], writes=[M16A[j]])
    for j in range(16):
        P.op("dve", lambda e, j=j: e.match_replace(out=scr[:, j, :], in_to_replace=m16[:, j, 0:8],
                                                   in_values=sc[:, j, :], imm_value=-1e30),
             reads=[SC, M16A[j]], writes=[SCR[j]])
    for j in range(16):
        P.op("dve", lambda e, j=j: e.max(out=m16[:, j, 8:16], in_=scr[:, j, :]), reads=[SCR[j]], writes=[M16B[j]])
    for j in range(16):
        P.op("dve", lambda e, j=j: e.max_index(out=ix16[:, j, 0:8], in_max=m16[:, j, 0:8], in_values=sc[:, j, :]),
             reads=[SC, M16A[j]], writes=[IXA[j]])
    for j in range(16):
        P.op("dve", lambda e, j=j: e.max_index(out=ix16[:, j, 8:16], in_max=m16[:, j, 8:16], in_values=sc[:, j, :]),
             reads=[SC, M16B[j]], writes=[IXB[j]])
    M16 = M16A + M16B
    IX16 = IXA + IXB
    pm = list(m16[:].ap[0])
    t1 = bass.AP(m16[:].tensor, m16[:].offset, [pm, [32, 8], [1, 16], [0, 16]])
    t2 = bass.AP(m16[:].tensor, m16[:].offset + 16, [pm, [32, 8], [0, 16], [1, 16]])
    P.op("dve", lambda e: e.tensor_tensor(out=cand[:].rearrange("p h (a b) -> p h a b", a=16), in0=t1, in1=t2,
                                          op=ALU.add), reads=M16, writes=SCR)
    c16 = A.alloc("tk_c16", [128, 8, 16], F32); C16A = P.bufs("tk_c16a", 8); C16B = P.bufs("tk_c16b", 8)
    pos = A.alloc("tk_pos", [128, 8, 16], U32); POSA = P.bufs("tk_posa", 8); POSB = P.bufs("tk_posb", 8)
    eq = A.alloc("tk_eq", [128, 8, 256], F32); EQS = P.bufs("tk_eq", 8)
    for h in range(8):
        P.op("dve", lambda e, h=h: e.max(out=c16[:, h, 0:8], in_=cand[:, h, :]),
             reads=SCR[2 * h:2 * h + 2], writes=[C16A[h]])
    for h in range(8):
        P.op("dve", lambda e, h=h: e.match_replace(out=eq[:, h, :], in_to_replace=c16[:, h, 0:8],
                                                   in_values=cand[:, h, :], imm_value=-1e30),
             reads=SCR[2 * h:2 * h + 2] + [C16A[h]], writes=[EQS[h]])
    for h in range(8):
        P.op("dve", lambda e, h=h: e.max(out=c16[:, h, 8:16], in_=eq[:, h, :]), reads=[EQS[h]], writes=[C16B[h]])
    for h in range(8):
        P.op("dve", lambda e, h=h: e.max_index(out=pos[:, h, 0:8], in_max=c16[:, h, 0:8], in_values=cand[:, h, :]),
             reads=SCR[2 * h:2 * h + 2] + [C16A[h]], writes=[POSA[h]])
    for h in range(8):
        P.op("dve", lambda e, h=h: e.max_index(out=pos[:, h, 8:16], in_max=c16[:, h, 8:16], in_values=cand[:, h, :]),
             reads=SCR[2 * h:2 * h + 2] + [C16B[h]], writes=[POSB[h]])
    C16 = P.buf("tk_c16all"); POS = P.buf("tk_posall"); EQ = P.buf("tk_eqall")
    for Bn, lst in ((C16, C16A + C16B), (POS, POSA + POSB), (EQ, EQS)):
        for b_ in lst:
            tok = b_.lastw
            if tok is not None and (Bn.lastw is None or True):
                s_, v_ = tok
                if Bn.reads.get(id(s_), (None, 0))[1] < v_:
                    Bn.reads[id(s_)] = (s_, v_)
            for tk in b_.reads.values():
                s_, v_ = tk
                if Bn.reads.get(id(s_), (None, 0))[1] < v_:
                    Bn.reads[id(s_)] = (s_, v_)
    eq4 = eq[:].rearrange("p h (a b) -> p h a b", a=16)
    au = A.alloc("tk_au", [128, 8, 16], U32); AU = P.buf("tk_au")
    bu = A.alloc("tk_bu", [128, 8, 16], U32); BU = P.buf("tk_bu")
    P.op("dve", lambda e: e.tensor_single_scalar(out=au[:], in_=pos[:], scalar=4, op=ALU.logical_shift_right),
         reads=[], writes=[AU, POS])
    P.op("dve", lambda e: e.tensor_single_scalar(out=bu[:], in_=pos[:], scalar=15, op=ALU.bitwise_and),
         reads=[POS], writes=[BU])
    af = A.alloc("tk_af", [128, 8, 16], F32); AFb = P.buf("tk_af")
    bf = A.alloc("tk_bf", [128, 8, 16], F32); BFb = P.buf("tk_bf")
    ixf = A.alloc("tk_ixf", [128, 16, 16], F32); IXF = P.buf("tk_ixf")
    P.op("dve", lambda e: e.tensor_copy(out=af[:], in_=au[:]), reads=[AU], writes=[AFb])
    P.op("dve", lambda e: e.tensor_copy(out=bf[:], in_=bu[:]), reads=[BU], writes=[BFb])
    P.op("dve", lambda e: e.tensor_copy(out=ixf[:], in_=ix16[:]), reads=IX16, writes=[IXF])
    idf = A.alloc("tk_idf", [128, 2, 8, 16], F32); IDF = P.buf("tk_idf")
    p0 = list(m16[:].ap[0])
    for s_, (vf, VF) in enumerate(((af, AFb), (bf, BFb))):
        vb_ = bass.AP(vf.tensor if hasattr(vf, "tensor") else vf, vf[:].offset, [list(vf[:].ap[0]), [16, 8], [1, 16], [0, 16]])
        io_ = bass.AP(iota16.tensor if hasattr(iota16, "tensor") else iota16, iota16[:].offset,
                      [list(iota16[:].ap[0]), [0, 8], [0, 16], [1, 16]])
        P.op("dve", lambda e, vb_=vb_, io_=io_: e.tensor_tensor(out=eq4, in0=vb_, in1=io_, op=ALU.is_equal),
             reads=[VF, IOTA], writes=[EQ])
        tab = bass.AP(ixf.tensor if hasattr(ixf, "tensor") else ixf, ixf[:].offset + 16 * s_,
                      [p0, [32, 8], [0, 16], [1, 16]])
        P.op("dve", lambda e, tab=tab: e.tensor_tensor(out=eq4, in0=eq4, in1=tab, op=ALU.mult),
             reads=[EQ, IXF], writes=[EQ])
        P.op("dve", lambda e, s_=s_: e.tensor_reduce(out=idf[:, s_, :, :], in_=eq4, axis=AX.X, op=ALU.add),
             reads=[EQ], writes=[IDF])
    P.op("dve", lambda e: e.scalar_tensor_tensor(out=idf[:, 0, :, :], in0=idf[:, 0, :, :], scalar=128.0,
                                                 in1=idf[:, 1, :, :], op0=ALU.mult, op1=ALU.add),
         reads=[IDF], writes=[IDF])
    P.op("dve", lambda e: e.tensor_copy(out=idx_out, in_=idf[:, 0, :, :].rearrange("p h k -> p (h k)")),
         reads=[IDF], writes=[OUT])
    cmax = bass.AP(c16.tensor if hasattr(c16, "tensor") else c16, c16[:].offset, [list(c16[:].ap[0]), [16, 8], [0, 16]])
    P.op("dve", lambda e: e.tensor_tensor(out=bf[:], in0=c16[:], in1=cmax, op=ALU.subtract),
         reads=[BFb], writes=[BFb, C16])
    P.op("act", lambda e: e.activation(out=bf[:], in_=bf[:], func=AF.Exp), reads=[BFb], writes=[BFb])
    ssum = A.alloc("tk_ssum", [128, 8], F32); SSUM = P.buf("tk_ssum")
    P.op("dve", lambda e: e.tensor_reduce(out=ssum[:], in_=bf[:], axis=AX.X, op=ALU.add), reads=[BFb], writes=[SSUM])
    P.op("dve", lambda e: e.reciprocal(out=ssum[:], in_=ssum[:]), reads=[SSUM], writes=[SSUM])
    sb_ = bass.AP(ssum.tensor if hasattr(ssum, "tensor") else ssum, ssum[:].offset, [list(ssum[:].ap[0]), [1, 8], [0, 16]])
    P.op("dve", lambda e: e.tensor_tensor(out=gw_out.rearrange("p (h k) -> p h k", h=8), in0=bf[:], in1=sb_, op=ALU.mult),
         reads=[BFb, SSUM], writes=[OUT])
    A.release(m)


def build(nc, stage="full", dumps=()):
    P = Prog(nc)
    A = Alloc(nc)
    dumps = set(dumps)
    dump_list = []

    def din(name, shape, dt=F32):
        return nc.dram_tensor(name, list(shape), dt, kind="ExternalInput").ap()

    x_d = din("x", [T, D])
    p_d = din("p", [T, 256])
    g_mix_d = din("g_mix", [D])
    w_in_d = din("w_in", [D, 2560])
    cv_d = din("cv", [128, 40])
    wrg_d = din("wrg_bd", [128, 4, 128])
    wig_d = din("wig_bd", [128, 4, 128])
    biasT_d = din("biasT", [128, 4, 3 * 2 * 256])
    w_out_d = din("w_out", [D, D])
    identb_d = din("ident_bf", [128, 128], BF16)
    g_ffn_d = din("g_ffn", [D])
    peer_wq_d = din("peer_wq", [D, 2048])
    k1T_d = din("k1T", [128, 128])
    k2T_d = din("k2T", [128, 128])
    iota_d = din("iota16", [128, 16])
    peer_u_d = din("peer_u", [16384, D])
    peer_v_d = din("peer_v", [16384, D])
    g_ple_d = din("g_ple", [D])
    w_gate_d = din("w_ple_gate", [D, D])
    w_proj_d = din("w_ple_proj", [256, D])
    g_final_d = din("g_final", [D])
    out_d = nc.dram_tensor("out", [T, D], F32, kind="ExternalOutput").ap()

    def dump(name, ap, bufs, shape, dt=F32):
        if name not in dumps:
            return
        d = nc.dram_tensor("dbg_" + name, list(shape), dt, kind="ExternalOutput").ap()
        DB = P.buf("dbg_" + name)
        P.dma("sp", lambda e: e.dma_start(out=d, in_=ap), DB, reads=list(bufs))
        dump_list.append(DB)

    psall = nc.alloc_psum_tensor("psall", [128, 8 * 512], F32)
    ps_t = [psall[:, i * 512:(i + 1) * 512] for i in range(8)]
    ps_b = [P.buf(f"psb{i}") for i in range(8)]
    ps_rr = [0]

    def psum(idx=None):
        if idx is None:
            idx = ps_rr[0]
            ps_rr[0] = (idx + 1) % 8
        return ps_t[idx], ps_b[idx]

    def psum_pair():
        idx = (ps_rr[0] + 1) // 2 * 2 % 8
        ps_rr[0] = (idx + 2) % 8
        return psall[:, idx * 512:(idx + 2) * 512].rearrange("p (a q) -> p a q", a=2), [ps_b[idx], ps_b[idx + 1]]

    identb = A.alloc("identb", [128, 128], BF16); IDB = P.buf("identb")
    P.dma("sp", lambda e: e.dma_start(out=identb[:], in_=identb_d), IDB, writes=[IDB])
    ones_f = A.alloc("ones_f", [128, 128], F32); ONF = P.buf("ones_f")
    P.op("pool", lambda e: e.memset(ones_f[:], 1.0), writes=[ONF])
    ones_b = A.alloc("ones_b", [128, 64], BF16); ONB = P.buf("ones_b")
    P.op("pool", lambda e: e.memset(ones_b[:], 1.0), writes=[ONB])
    cv = A.alloc("cv", [128, 40], F32); CV = P.buf("cv")
    P.dma("sp", lambda e: e.dma_start(out=cv[:], in_=cv_d), CV, writes=[CV])
    gB = A.alloc("gB", [128, D], F32); GB = P.buf("gB")
    rstd_all = A.alloc("rstd_all", [128, NT], F32); RSTD = P.buf("rstd_all")

    uT = A.alloc("uT", [128, 8, T], BF16); UT = P.bufs("uT", NT)
    yT = A.alloc("yT", [128, 8, T], BF16); YT = P.bufs("yT", 8)

    uv_d = nc.dram_tensor("uv16", [16384, 2 * D], BF16, kind="Internal").ap()
    RPP = 2
    pst = [A.alloc(f"pst{j}", [128, RPP, D], F32) for j in range(2)]; PST = P.bufs("pst", 2)
    psb_ = [A.alloc(f"ppb{j}", [128, RPP, D], BF16) for j in range(2)]; PSB = P.bufs("ppb", 2)
    UVD = P.buf("uv16")

    def prepass_gen():
        steps = []
        for t_, c0 in ((peer_u_d, 0), (peer_v_d, D)):
            for blk in range(16384 // (128 * RPP)):
                r0 = blk * 128 * RPP
                src = t_[r0:r0 + 128 * RPP, :].rearrange("(p r) d -> p r d", r=RPP)
                dst = uv_d[r0:r0 + 128 * RPP, c0:c0 + D].rearrange("(p r) d -> p r d", r=RPP)
                steps.append((src, dst))

        def load(n):
            j = n % 2
            src = steps[n][0]
            P.dma("sp", lambda e, j=j, src=src: e.dma_start(out=pst[j][:], in_=src), PST[j], writes=[PST[j]])

        load(0)
        for n in range(len(steps)):
            j = n % 2
            dst = steps[n][1]
            P.op("pool", lambda e, j=j: e.tensor_copy(out=psb_[j][:], in_=pst[j][:]),
                 reads=[PST[j]], writes=[PSB[j]])
            if n + 1 < len(steps):
                load(n + 1)
            P.dma("sp", lambda e, j=j, dst=dst: e.dma_start(out=dst, in_=psb_[j][:]), PSB[j],
                  reads=[PSB[j]], writes=[UVD])
            yield

    pp = prepass_gen()

    def pp_adv(n):
        for _ in range(n):
            next(pp, None)

    def norm_transpose(src_tiles, gvec_d, dstT, DST, hook=None):
        m = A.mark()
        P.dma("sp", lambda e: e.dma_start(out=gB[:], in_=bcast_rows(gvec_d, D)), GB, writes=[GB])
        sq = A.alloc("nt_sq", [128, D], BF16); SQ = P.buf("nt_sq")
        ss = A.alloc("nt_ss", [128, 2], F32); SS = P.buf("nt_ss")
        xs = [A.alloc(f"nt_xs{j}", [128, D], BF16) for j in range(2)]
        XS = P.bufs("nt_xs", 2)
        for i in range(NT):
            if hook is not None:
                hook()
            src, SRC = src_tiles(i)
            P.op("act", lambda e, src=src: e.activation(out=sq[:], in_=src, func=AF.Square,
                                                         accum_out=ss[:, 0:1]),
                 reads=SRC, writes=[SQ, SS])
            P.op("act", lambda e: e.activation(out=ss[:, 1:2], in_=ss[:, 0:1], func=AF.Sqrt,
                                               scale=1.0 / D, bias=EPS),
                 reads=[SS], writes=[SS])
            P.op("dve", lambda e, i=i: e.reciprocal(out=rstd_all[:, i:i + 1], in_=ss[:, 1:2]),
                 reads=[SS], writes=[RSTD])
            j = i % 2
            P.op("dve", lambda e, src=src, i=i, j=j: e.scalar_tensor_tensor(
                out=xs[j][:], in0=src, scalar=rstd_all[:, i:i + 1], in1=gB[:],
                op0=ALU.mult, op1=ALU.mult), reads=SRC + [RSTD, GB], writes=[XS[j]])
            ps, PS = psum()
            psb = ps[:].bitcast(BF16)
            for k in range(8):
                P.op("pe", lambda e, j=j, k=k, psb=psb: e.transpose(
                    out=psb[:, k * 128:(k + 1) * 128], in_=xs[j][:, k * 128:(k + 1) * 128],
                    identity=identb[:]), reads=[XS[j], IDB], writes=[PS])
            P.op("act", lambda e, i=i, psb=psb: e.copy(
                out=dstT[:, :, i * 128:(i + 1) * 128],
                in_=psb.rearrange("p (k t) -> p k t", k=8)), reads=[PS], writes=[DST[i]])
        P.barrier()
        A.release(m)

    wsl = [A.alloc(f"wsl{j}", [128, 8, 128], BF16) for j in range(3)]
    WSL = P.bufs("wsl", 3)
    wsl_rr = [0]

    def load_wchunk(w_d, col0):
        j = wsl_rr[0]
        wsl_rr[0] = (j + 1) % 3
        src = w_d.rearrange("(k p) c -> p k c", p=128)[:, :, col0:col0 + 128]
        P.dma("pool", lambda e: e.dma_start(out=wsl[j][:], in_=src), WSL[j], writes=[WSL[j]])
        return wsl[j], WSL[j]

    def proj_chunk(w_d, col0, srcT, SRC, evac):
        w, W = load_wchunk(w_d, col0)
        for tq in range(4):
            ps, PS = psum()
            for k in range(8):
                P.op("pe", lambda e, ps=ps, k=k, tq=tq, w=w: e.matmul(
                    ps[:], lhsT=w[:, k, :], rhs=srcT[:, k, tq * 512:(tq + 1) * 512],
                    start=(k == 0), stop=(k == 7)),
                    reads=[W] + SRC[tq * 4:(tq + 1) * 4], writes=[PS])
            evac(tq, ps, PS)

    def finalize(out_bufs):
        final = []
        w = P.waited["sp"]
        for b in list(out_bufs) + dump_list:
            if b.dsem is not None and w.get(id(b.dsem), 0) < b.dcnt:
                w[id(b.dsem)] = b.dcnt
                final.append((b.dsem, b.dcnt))
        P.q["sp"].append((final, None, None, 0))
        P.emit()
        return nc

    m_phase = A.mark()
    xin = [A.alloc(f"xin{j}", [128, D], F32) for j in range(2)]
    XIN = P.bufs("xin", 2)

    def x_src(i):
        j = i % 2
        P.dma("sp", lambda e: e.dma_start(out=xin[j][:], in_=x_d[i * 128:(i + 1) * 128, :]),
              XIN[j], writes=[XIN[j]])
        return xin[j][:], [XIN[j]]

    norm_transpose(x_src, g_mix_d, uT, UT, hook=lambda: pp_adv(1))
    dump("uT", uT[:], UT, [128, 8, T], BF16)
    P.barrier()
    A.release(m_phase)
    if stage == "p1":
        return finalize([])

    m_phase = A.mark()
    y32 = A.alloc("y32", [128, 4, T], F32); Y32 = P.bufs("y32", 4)
    wrg = A.alloc("wrg", [128, 4, 128], BF16); WRG = P.buf("wrg")
    wig = A.alloc("wig", [128, 4, 128], BF16); WIG = P.buf("wig")
    P.dma("pool", lambda e: e.dma_start(out=wrg[:], in_=wrg_d), WRG, writes=[WRG])
    P.dma("pool", lambda e: e.dma_start(out=wig[:], in_=wig_d), WIG, writes=[WIG])
    nsp = A.alloc("nsp", [128, 4], F32); NSP = P.buf("nsp")
    P.op("act", lambda e: e.activation(out=nsp[:], in_=cv[:, 28:32], func=AF.Exp, scale=-1.0),
         reads=[CV], writes=[NSP])
    P.op("act", lambda e: e.activation(out=nsp[:], in_=nsp[:], func=AF.Ln, scale=1.0, bias=1.0),
         reads=[NSP], writes=[NSP])
    P.op("dve", lambda e: e.tensor_scalar(out=nsp[:], in0=nsp[:], scalar1=-8.0, scalar2=None,
                                          op0=ALU.mult), reads=[NSP], writes=[NSP])

    def big(name, dt=F32):
        return A.alloc(name, [128, T], dt), P.buf(name)

    xl, XL = big("xl"); gt, GT = big("gt"); xc, XC = big("xc"); xcb, XCB = big("xcb", BF16)
    ra, RA = big("ra"); ig, IG = big("ig"); tmp, TMP = big("tmp")

    for c in range(4):
        def ev_xl(tq, ps, PS):
            P.op("act", lambda e, ps=ps, tq=tq: e.copy(out=xl[:, tq * 512:(tq + 1) * 512], in_=ps[:]),
                 reads=[PS], writes=[XL])

        def ev_gt(tq, ps, PS):
            P.op("act", lambda e, ps=ps, tq=tq: e.copy(out=gt[:, tq * 512:(tq + 1) * 512], in_=ps[:]),
                 reads=[PS], writes=[GT])

        proj_chunk(w_in_d, c * 128, uT, UT, ev_xl)
        pp_adv(2)
        proj_chunk(w_in_d, 512 + c * 128, uT, UT, ev_gt)
        pp_adv(2)
        P.op("dve", lambda e, c=c: e.tensor_scalar(
            out=xc[:], in0=xl[:], scalar1=cv[:, c * 4 + 3:c * 4 + 4], scalar2=cv[:, 16 + c:17 + c],
            op0=ALU.mult, op1=ALU.add), reads=[XL, CV], writes=[XC])
        for s in (1, 2, 3):
            P.op("dve", lambda e, c=c, s=s: e.scalar_tensor_tensor(
                out=xc[:, s:], in0=xl[:, 0:T - s], scalar=cv[:, c * 4 + 3 - s:c * 4 + 4 - s],
                in1=xc[:, s:], op0=ALU.mult, op1=ALU.add), reads=[XL, CV, XC], writes=[XC])
        P.op("act", lambda e: e.copy(out=xcb[:], in_=xc[:]), reads=[XC], writes=[XCB])
        for tq in range(4):
            ps, PS = psum()
            P.op("pe", lambda e, ps=ps, tq=tq, c=c: e.matmul(
                ps[:], lhsT=wrg[:, c, :], rhs=xcb[:, tq * 512:(tq + 1) * 512], start=True, stop=True),
                reads=[WRG, XCB], writes=[PS])
            P.op("act", lambda e, ps=ps, tq=tq, c=c: e.activation(
                out=ra[:, tq * 512:(tq + 1) * 512], in_=ps[:], func=AF.Sigmoid,
                bias=cv[:, 20 + c:21 + c], scale=1.0), reads=[PS, CV], writes=[RA])
        for tq in range(4):
            ps, PS = psum()
            P.op("pe", lambda e, ps=ps, tq=tq, c=c: e.matmul(
                ps[:], lhsT=wig[:, c, :], rhs=xcb[:, tq * 512:(tq + 1) * 512], start=True, stop=True),
                reads=[WIG, XCB], writes=[PS])
            P.op("act", lambda e, ps=ps, tq=tq, c=c: e.activation(
                out=ig[:, tq * 512:(tq + 1) * 512], in_=ps[:], func=AF.Sigmoid,
                bias=cv[:, 24 + c:25 + c], scale=1.0), reads=[PS, CV], writes=[IG])
        P.op("act", lambda e, c=c: e.activation(out=ra[:], in_=ra[:], func=AF.Exp,
                                                scale=nsp[:, c:c + 1]), reads=[RA, NSP], writes=[RA])
        P.op("dve", lambda e: e.tensor_tensor(out=tmp[:], in0=ra[:], in1=ra[:], op=ALU.mult),
             reads=[RA], writes=[TMP])
        P.op("act", lambda e: e.activation(out=tmp[:], in_=tmp[:], func=AF.Sqrt, scale=-1.0, bias=1.0),
             reads=[TMP], writes=[TMP])
        P.op("dve", lambda e: e.tensor_tensor(out=ig[:], in0=ig[:], in1=xc[:], op=ALU.mult),
             reads=[IG, XC], writes=[IG])
        P.op("dve", lambda e: e.tensor_tensor(out=ig[:], in0=ig[:], in1=tmp[:], op=ALU.mult),
             reads=[IG, TMP], writes=[IG])
        P.op("dve", lambda e: e.tensor_tensor_scan(out=tmp[:], data0=ra[:], data1=ig[:], initial=0.0,
                                                   op0=ALU.mult, op1=ALU.add),
             reads=[RA, IG], writes=[TMP])
        P.op("act", lambda e: e.activation(out=gt[:], in_=gt[:], func=AF.Gelu_apprx_tanh),
             reads=[GT], writes=[GT])
        P.op("dve", lambda e, c=c: e.tensor_tensor(out=y32[:, c, :], in0=tmp[:], in1=gt[:], op=ALU.mult),
             reads=[TMP, GT], writes=[Y32[c]])

    def group_rmsnorm(gcol0, ybase, y32, Y32, tmp, TMP, ra, RA):
        pss = [psum(i) for i in range(4)]
        for c in range(4):
            P.op("act", lambda e, c=c: e.activation(out=tmp[:], in_=y32[:, c, :], func=AF.Square),
                 reads=[Y32[c]], writes=[TMP])
            for tq in range(4):
                ps, PS = pss[tq]
                P.op("pe", lambda e, ps=ps, tq=tq, c=c: e.matmul(
                    ps[:], lhsT=ones_f[:], rhs=tmp[:, tq * 512:(tq + 1) * 512],
                    start=(c == 0), stop=(c == 3)), reads=[ONF, TMP], writes=[PS])
        for tq in range(4):
            ps, PS = pss[tq]
            P.op("act", lambda e, ps=ps, tq=tq: e.activation(
                out=ra[:, tq * 512:(tq + 1) * 512], in_=ps[:], func=AF.Sqrt, scale=1.0 / 512, bias=EPS),
                reads=[PS], writes=[RA])
        P.op("dve", lambda e: e.reciprocal(out=ra[:], in_=ra[:]), reads=[RA], writes=[RA])
        for c in range(4):
            P.op("dve", lambda e, c=c: e.scalar_tensor_tensor(
                out=yT[:, ybase + c, :], in0=y32[:, c, :], scalar=cv[:, gcol0 + c:gcol0 + c + 1],
                in1=ra[:], op0=ALU.mult, op1=ALU.mult), reads=[Y32[c], CV, RA], writes=[YT[ybase + c]])

    dump("y_lru", y32[:], Y32, [128, 4, T])
    if stage == "p2a":
        return finalize([])
    group_rmsnorm(32, 0, y32, Y32, tmp, TMP, ra, RA)
    if stage == "p2":
        dump("yT", yT[:], YT, [128, 8, T], BF16)
    P.barrier()
    A.release(m_phase)
    if stage == "p2":
        return finalize([])

    m_phase = A.mark()
    y3 = A.alloc("y32a", [128, 4, T], F32); Y3 = P.bufs("y32a", 4)
    ra3, RA3 = big("ra2"); tmp3, TMP3 = big("tmp2")
    qT, QT = big("qT", BF16); kT, KT = big("kT", BF16)
    dacc, DACC = big("dacc")
    bias_sb = A.alloc("bias_sb", [128, 3, 2, 256], F32); BIAS = P.buf("bias_sb")
    vb = A.alloc("vb", [128, 16, 128], BF16); VB = P.buf("vb")
    tS = [A.alloc(f"tS{j}", [128, 2, 256], F32) for j in range(2)]; TS = P.bufs("tS", 2)
    pT = [A.alloc(f"pT{j}", [128, 2, 256], BF16) for j in range(2)]; PT = P.bufs("pT", 2)
    it = 0
    for c in range(4):
        OACC = Y3[c]
        P.dma("sp", lambda e, c=c: e.dma_start(
            out=bias_sb[:].rearrange("p a b q -> p (a b q)"), in_=biasT_d[:, c, :]), BIAS, writes=[BIAS])

        def ev_q(tq, ps, PS):
            P.op("act", lambda e, ps=ps, tq=tq: e.activation(
                out=qT[:, tq * 512:(tq + 1) * 512], in_=ps[:], func=AF.Copy, scale=0.125),
                reads=[PS], writes=[QT])

        def ev_k(tq, ps, PS):
            P.op("act", lambda e, ps=ps, tq=tq: e.copy(out=kT[:, tq * 512:(tq + 1) * 512], in_=ps[:]),
                 reads=[PS], writes=[KT])

        proj_chunk(w_in_d, 1024 + c * 128, uT, UT, ev_q)
        proj_chunk(w_in_d, 1536 + c * 128, uT, UT, ev_k)
        wv, WV = load_wchunk(w_in_d, 2048 + c * 128)
        P.op("pool", lambda e, c=c: e.memset(y3[:, c, :], 0.0), writes=[OACC])
        P.op("pool", lambda e: e.memset(dacc[:], 0.0), writes=[DACC])
        for bi, d in enumerate((1, 4, 16)):
            nb = 16 // d
            for g4 in range(4):
                ps, PS = psum()
                for u4 in range(4):
                    ti = g4 * 4 + u4
                    r, m = ti // nb, ti % nb
                    t0 = r + d * 128 * m
                    for k in range(8):
                        P.op("pe", lambda e, ps=ps, u4=u4, k=k, t0=t0, d=d, wv=wv: e.matmul(
                            ps[:, u4 * 128:(u4 + 1) * 128],
                            lhsT=uT[:, k, t0:t0 + d * 127 + 1:d], rhs=wv[:, k, :],
                            start=(k == 0), stop=(k == 7)), reads=[WV] + UT, writes=[PS])
                P.op("act", lambda e, ps=ps, g4=g4: e.copy(
                    out=vb[:, g4 * 4:(g4 + 1) * 4, :], in_=ps[:].rearrange("p (a b) -> p a b", a=4)),
                    reads=[PS], writes=[VB])
            for r in range(d):
                for m in range(nb):
                    nq = 256 if m + 1 < nb else 128
                    t0 = r + d * 128 * m
                    kend = t0 + d * 127 + 1
                    qend = t0 + d * (nq - 1) + 1
                    j = it % 2
                    it += 1
                    if it % 3 == 0:
                        pp_adv(1)
                    ps, PSP = psum_pair()
                    for hh in range(2):
                        p0 = hh * 64
                        P.op("pe", lambda e, ps=ps, hh=hh, p0=p0, t0=t0, kend=kend, qend=qend, d=d, nq=nq: e.matmul(
                            ps[:, hh, 0:nq], lhsT=kT[p0:p0 + 64, t0:kend:d],
                            rhs=qT[p0:p0 + 64, t0:qend:d], start=True, stop=True),
                            reads=[KT, QT], writes=[PSP[hh]])
                    P.op("dve", lambda e, ps=ps, j=j, bi=bi, nq=nq: e.tensor_tensor(
                        out=tS[j][:, :, 0:nq], in0=ps[:, :, 0:nq],
                        in1=bias_sb[:, bi, :, 0:nq], op=ALU.add), reads=PSP + [BIAS], writes=[TS[j]])
                    P.op("act", lambda e, j=j, nq=nq: e.activation(
                        out=pT[j][:, :, 0:nq], in_=tS[j][:, :, 0:nq], func=AF.Exp),
                        reads=[TS[j]], writes=[PT[j]])
                    ps2, PS2 = psum()
                    ti = r * nb + m
                    for hh in range(2):
                        p0 = hh * 64
                        P.op("pe", lambda e, ps2=ps2, hh=hh, p0=p0, ti=ti, j=j, nq=nq: e.matmul(
                            ps2[p0:p0 + 64, 0:nq], lhsT=vb[:, ti, hh * 64:(hh + 1) * 64],
                            rhs=pT[j][:, hh, 0:nq], start=True, stop=True),
                            reads=[VB, PT[j]], writes=[PS2])
                        P.op("pe", lambda e, ps2=ps2, hh=hh, p0=p0, j=j, nq=nq: e.matmul(
                            ps2[p0:p0 + 64, 256:256 + nq], lhsT=ones_b[:, 0:64],
                            rhs=pT[j][:, hh, 0:nq], start=True, stop=True),
                            reads=[ONB, PT[j]], writes=[PS2])
                    P.op("dve", lambda e, ps2=ps2, c=c, t0=t0, qend=qend, d=d, nq=nq: e.tensor_tensor(
                        out=y3[:, c, t0:qend:d], in0=y3[:, c, t0:qend:d], in1=ps2[:, 0:nq], op=ALU.add),
                        reads=[PS2, OACC], writes=[OACC])
                    P.op("dve", lambda e, ps2=ps2, t0=t0, qend=qend, d=d, nq=nq: e.tensor_tensor(
                        out=dacc[:, t0:qend:d], in0=dacc[:, t0:qend:d], in1=ps2[:, 256:256 + nq], op=ALU.add),
                        reads=[PS2, DACC], writes=[DACC])
        P.op("dve", lambda e: e.reciprocal(out=dacc[:], in_=dacc[:]), reads=[DACC], writes=[DACC])
        P.op("dve", lambda e, c=c: e.tensor_tensor(out=y3[:, c, :], in0=y3[:, c, :], in1=dacc[:], op=ALU.mult),
             reads=[OACC, DACC], writes=[OACC])
    dump("y_att", y3[:], Y3, [128, 4, T])
    if stage == "p3a":
        return finalize([])
    group_rmsnorm(36, 4, y3, Y3, tmp3, TMP3, ra3, RA3)
    if stage != "p2":
        dump("yT", yT[:], YT, [128, 8, T], BF16)
    P.barrier()
    A.release(m_phase)

    xt = A.alloc("xt", [128, NT, D], F32); XT = P.bufs("xt", NT)
    m_phase = A.mark()
    wo = A.alloc("wo", [128, 8, D], BF16); WO = P.buf("wo")
    P.dma("pool", lambda e: e.dma_start(out=wo[:], in_=w_out_d.rearrange("(k p) c -> p k c", p=128)),
          WO, writes=[WO])
    for i in range(NT):
        P.dma("sp", lambda e, i=i: e.dma_start(out=xt[:, i, :], in_=x_d[i * 128:(i + 1) * 128, :]),
              XT[i], writes=[XT[i]])
    for i in range(NT):
        for hf in range(2):
            pp_adv(1)
            ps, PS = psum()
            for k in range(8):
                P.op("pe", lambda e, ps=ps, k=k, i=i, hf=hf: e.matmul(
                    ps[:], lhsT=yT[:, k, i * 128:(i + 1) * 128], rhs=wo[:, k, hf * 512:(hf + 1) * 512],
                    start=(k == 0), stop=(k == 7)), reads=[WO] + YT, writes=[PS])
            P.op("dve", lambda e, ps=ps, i=i, hf=hf: e.tensor_tensor(
                out=xt[:, i, hf * 512:(hf + 1) * 512], in0=xt[:, i, hf * 512:(hf + 1) * 512],
                in1=ps[:], op=ALU.add), reads=[PS, XT[i]], writes=[XT[i]])
    for _ in pp:
        pass
    P.barrier()
    A.release(m_phase)

    if stage == "mixer":
        for i in range(NT):
            P.dma("sp", lambda e, i=i: e.dma_start(out=out_d[i * 128:(i + 1) * 128, :], in_=xt[:, i, :]),
                  XT[i], reads=[XT[i]])
        return finalize(XT)

    def xt_src(i):
        return xt[:, i, :], [XT[i]]

    m_phase = A.mark()
    norm_transpose(xt_src, g_ffn_d, uT, UT)
    wq = yT
    WQ = P.buf("wq")
    wq_src = peer_wq_d.rearrange("(k p) c -> p k c", p=128)
    for hf in range(2):
        P.dma("pool", lambda e, hf=hf: e.dma_start(out=wq[:, :, hf * 1024:(hf + 1) * 1024],
                                                   in_=wq_src[:, :, hf * 1024:(hf + 1) * 1024]),
              WQ, writes=[WQ])
    kTs = A.alloc("kTs", [128, 2, 128], F32); KTS = P.buf("kTs")
    P.dma("sp", lambda e: e.dma_start(out=kTs[:, 0, :], in_=k1T_d), KTS, writes=[KTS])
    P.dma("sp", lambda e: e.dma_start(out=kTs[:, 1, :], in_=k2T_d), KTS, writes=[KTS])
    iota16 = A.alloc("iota16", [128, 16], F32); IOTA = P.buf("iota16")
    P.dma("sp", lambda e: e.dma_start(out=iota16[:], in_=iota_d), IOTA, writes=[IOTA])
    idx_all = A.alloc("idx_all", [128, NT, 128], I32); IDX = P.bufs("idx_all", NT)
    gw_all = A.alloc("gw_all", [128, NT, 128], F32)
    m_sub = A.mark()
    assert A.offs["ppb1"] == A.offs["ppb0"] + RPP * D * 2
    qTt = A.alloc_at("qTt", [128, 16, 128], F32, A.offs["pst0"]); QTT = P.buf("qTt")
    scs = [A.alloc_at("sc0", [128, 16, 128], F32, A.offs["pst1"]),
           A.alloc_at("sc1", [128, 16, 128], F32, A.offs["ppb0"])]
    SCS = P.bufs("sc", 2)
    for i in range(NT):
        for jg in range(4):
            ps, PS = psum()
            for u in range(4):
                j = jg * 4 + u
                for k in range(8):
                    P.op("pe", lambda e, ps=ps, u=u, j=j, k=k, i=i: e.matmul(
                        ps[:, u * 128:(u + 1) * 128], lhsT=wq[:, k, j * 128:(j + 1) * 128],
                        rhs=uT[:, k, i * 128:(i + 1) * 128], start=(k == 0), stop=(k == 7)),
                        reads=[WQ, UT[i]], writes=[PS])
            P.op("act", lambda e, ps=ps, jg=jg: e.copy(
                out=qTt[:, jg * 4:(jg + 1) * 4, :], in_=ps[:].rearrange("p (a b) -> p a b", a=4)),
                reads=[PS], writes=[QTT])
        sc, SC = scs[i % 2], SCS[i % 2]
        for jg in range(4):
            ps, PS = psum()
            for u in range(4):
                j = jg * 4 + u
                P.op("pe", lambda e, ps=ps, u=u, j=j: e.matmul(
                    ps[:, u * 128:(u + 1) * 128], lhsT=qTt[:, j, :], rhs=kTs[:, j % 2, :],
                    start=True, stop=True), reads=[QTT, KTS], writes=[PS])
            P.op("act", lambda e, ps=ps, jg=jg, sc=sc: e.copy(
                out=sc[:, jg * 4:(jg + 1) * 4, :], in_=ps[:].rearrange("p (a b) -> p a b", a=4)),
                reads=[PS], writes=[SC])
        if i == 0:
            dump("qTt0", qTt[:], [QTT], [128, 16, 128])
            dump("sc0", sc[:], [SC], [128, 16, 128])
        peer_topk(P, A, sc, SC, iota16, IOTA, idx_all[:, i, :], gw_all[:, i, :], IDX[i])
    dump("idx_all", idx_all[:], IDX, [128, NT, 128], I32)
    dump("gw_all", gw_all[:], IDX, [128, NT, 128])
    P.barrier()
    A.release(m_sub)
    if stage == "topk":
        return finalize([])
    NS = 16
    GE = 8
    base_off = A.offs["uT"]
    assert A.offs["yT"] == base_off + 8 * T * 2
    gs = [A.alloc_at(f"gs{j}", [128, 2 * D], BF16, base_off + j * 4096) for j in range(NS)]
    GS = P.bufs("gs", NS)
    xn32 = A.alloc("xn32", [128, D], F32); XN = P.buf("xn32")
    junk = A.alloc("junk", [128, D], BF16); JUNK = P.buf("junk")
    dots = A.alloc("dots", [128, 128], F32); DOTS = P.bufs("dots", 128)
    wvg = A.alloc("wvg", [128, 128], F32); WVG = P.bufs("wvg", 16)
    wv_t = A.alloc("wv_t", [128, 128], F32); WVT = P.bufs("wv_t", 16)
    NDG = 8
    dg = [A.alloc(f"dg{j}", [128, 128], BF16) for j in range(NDG)]; DG = P.bufs("dg", NDG)
    gi = 0
    di = 0
    for i in range(NT):
        P.op("dve", lambda e, i=i: e.scalar_tensor_tensor(
            out=xn32[:], in0=xt[:, i, :], scalar=rstd_all[:, i:i + 1], in1=gB[:],
            op0=ALU.mult, op1=ALU.mult), reads=[XT[i], RSTD, GB], writes=[XN])
        bank = 4 * (i % 2)
        psy = [psum(bank), psum(bank + 1)]
        for g in range(128 // GE):
            sls = []
            for u in range(GE):
                j = g * GE + u
                sl = gi % NS
                gi += 1
                sls.append(sl)
                P.dma("pool", lambda e, sl=sl, i=i, j=j: e.indirect_dma_start(
                    out=gs[sl][:], out_offset=None, in_=uv_d,
                    in_offset=bass.IndirectOffsetOnAxis(ap=idx_all[:, i, j:j + 1], axis=0)),
                    GS[sl], reads=[IDX[i]], writes=[GS[sl]])
            for u in range(GE):
                j = g * GE + u
                sl = sls[u]
                P.op("dve", lambda e, sl=sl, j=j: e.scalar_tensor_tensor(
                    out=junk[:], in0=gs[sl][:, 0:D], scalar=1.0, in1=xn32[:], op0=ALU.mult, op1=ALU.mult,
                    accum_out=dots[:, j:j + 1]), reads=[GS[sl], XN], writes=[JUNK, DOTS[j]])
            c0, c1 = g * GE, (g + 1) * GE
            P.op("act", lambda e, c0=c0, c1=c1: e.activation(out=wvg[:, c0:c1], in_=dots[:, c0:c1],
                                                            func=AF.Gelu_apprx_tanh),
                 reads=DOTS[c0:c1], writes=[WVG[g]])
            P.op("dve", lambda e, i=i, c0=c0, c1=c1: e.tensor_tensor(
                out=wv_t[:, c0:c1], in0=wvg[:, c0:c1], in1=gw_all[:, i, c0:c1], op=ALU.mult),
                reads=[WVG[g], IDX[i]], writes=[WVT[g]])
            for u in range(GE):
                j = g * GE + u
                sl = sls[u]
                dsl = di % NDG
                di += 1
                P.op("act", lambda e, dsl=dsl, j=j: e.activation(
                    out=dg[dsl][:], in_=identb[:], func=AF.Copy, scale=wv_t[:, j:j + 1]),
                    reads=[IDB, WVT[g]], writes=[DG[dsl]])
                for hf in range(2):
                    ps, PS = psy[hf]
                    P.op("pe", lambda e, ps=ps, dsl=dsl, sl=sl, hf=hf, j=j: e.matmul(
                        ps[:], lhsT=dg[dsl][:], rhs=gs[sl][:, D + hf * 512:D + (hf + 1) * 512],
                        start=(j == 0), stop=(j == 127)), reads=[DG[dsl], GS[sl]], writes=[PS])
        for hf in range(2):
            ps, PS = psy[hf]
            P.op("dve", lambda e, ps=ps, i=i, hf=hf: e.tensor_tensor(
                out=xt[:, i, hf * 512:(hf + 1) * 512], in0=xt[:, i, hf * 512:(hf + 1) * 512],
                in1=ps[:], op=ALU.add), reads=[PS, XT[i]], writes=[XT[i]])
    P.barrier()
    A.release(m_phase)
    if stage == "ffn":
        for i in range(NT):
            P.dma("sp", lambda e, i=i: e.dma_start(out=out_d[i * 128:(i + 1) * 128, :], in_=xt[:, i, :]),
                  XT[i], reads=[XT[i]])
        return finalize(XT)

    m_phase = A.mark()
    norm_transpose(xt_src, g_ple_d, uT, UT)
    wg = A.alloc("wg", [128, 8, D], BF16); WG = P.buf("wg")
    P.dma("pool", lambda e: e.dma_start(out=wg[:], in_=w_gate_d.rearrange("(k p) c -> p k c", p=128)),
          WG, writes=[WG])
    wpp = A.alloc("wpp", [128, 2, D], BF16); WPP = P.buf("wpp")
    P.dma("pool", lambda e: e.dma_start(out=wpp[:], in_=w_proj_d.rearrange("(k p) c -> p k c", p=128)),
          WPP, writes=[WPP])
    pb = [A.alloc(f"pb{j}", [128, 256], BF16) for j in range(2)]; PB = P.bufs("pb", 2)
    pTt = [A.alloc(f"pTt{j}", [128, 2, 128], BF16) for j in range(2)]; PTT = P.bufs("pTt", 2)
    sg = [A.alloc(f"sg{j}", [128, 512], F32) for j in range(2)]; SG = P.bufs("sg", 2)
    it = 0
    for i in range(NT):
        j = i % 2
        P.dma("pool", lambda e, i=i, j=j: e.dma_start(out=pb[j][:], in_=p_d[i * 128:(i + 1) * 128, :]),
              PB[j], writes=[PB[j]])
        ps, PS = psum()
        psb = ps[:].bitcast(BF16)
        for kk in range(2):
            P.op("pe", lambda e, j=j, kk=kk, psb=psb: e.transpose(
                out=psb[:, kk * 128:(kk + 1) * 128], in_=pb[j][:, kk * 128:(kk + 1) * 128],
                identity=identb[:]), reads=[PB[j], IDB], writes=[PS])
        P.op("act", lambda e, j=j, psb=psb: e.copy(
            out=pTt[j][:], in_=psb[:, 0:256].rearrange("p (k t) -> p k t", k=2)), reads=[PS], writes=[PTT[j]])
        for hf in range(2):
            jj = it % 2
            it += 1
            ps1, PS1 = psum()
            for k in range(8):
                P.op("pe", lambda e, ps1=ps1, k=k, i=i, hf=hf: e.matmul(
                    ps1[:], lhsT=uT[:, k, i * 128:(i + 1) * 128], rhs=wg[:, k, hf * 512:(hf + 1) * 512],
                    start=(k == 0), stop=(k == 7)), reads=[WG, UT[i]], writes=[PS1])
            P.op("act", lambda e, ps1=ps1, jj=jj: e.activation(out=sg[jj][:], in_=ps1[:], func=AF.Sigmoid),
                 reads=[PS1], writes=[SG[jj]])
            ps2, PS2 = psum()
            for kk in range(2):
                P.op("pe", lambda e, ps2=ps2, kk=kk, j=j, hf=hf: e.matmul(
                    ps2[:], lhsT=pTt[j][:, kk, :], rhs=wpp[:, kk, hf * 512:(hf + 1) * 512],
                    start=(kk == 0), stop=(kk == 1)), reads=[WPP, PTT[j]], writes=[PS2])
            P.op("dve", lambda e, ps2=ps2, jj=jj: e.tensor_tensor(out=sg[jj][:], in0=sg[jj][:], in1=ps2[:], op=ALU.mult),
                 reads=[PS2, SG[jj]], writes=[SG[jj]])
            P.op("dve", lambda e, jj=jj, i=i, hf=hf: e.tensor_tensor(
                out=xt[:, i, hf * 512:(hf + 1) * 512], in0=xt[:, i, hf * 512:(hf + 1) * 512],
                in1=sg[jj][:], op=ALU.add), reads=[SG[jj], XT[i]], writes=[XT[i]])
    P.barrier()
    A.release(m_phase)

    P.dma("sp", lambda e: e.dma_start(out=gB[:], in_=bcast_rows(g_final_d, D)), GB, writes=[GB])
    sqf = A.alloc("fn_sq", [128, D], BF16); SQF = P.buf("fn_sq")
    ssf = A.alloc("fn_ss", [128, 2], F32); SSF = P.buf("fn_ss")
    for i in range(NT):
        P.op("act", lambda e, i=i: e.activation(out=sqf[:], in_=xt[:, i, :], func=AF.Square,
                                                accum_out=ssf[:, 0:1]), reads=[XT[i]], writes=[SQF, SSF])
        P.op("act", lambda e: e.activation(out=ssf[:, 1:2], in_=ssf[:, 0:1], func=AF.Sqrt,
                                           scale=1.0 / D, bias=EPS), reads=[SSF], writes=[SSF])
        P.op("dve", lambda e, i=i: e.reciprocal(out=rstd_all[:, i:i + 1], in_=ssf[:, 1:2]),
             reads=[SSF], writes=[RSTD])
        P.op("dve", lambda e, i=i: e.scalar_tensor_tensor(
            out=xt[:, i, :], in0=xt[:, i, :], scalar=rstd_all[:, i:i + 1], in1=gB[:],
            op0=ALU.mult, op1=ALU.mult), reads=[XT[i], RSTD, GB], writes=[XT[i]])
        P.dma("sp", lambda e, i=i: e.dma_start(out=out_d[i * 128:(i + 1) * 128, :], in_=xt[:, i, :]),
              XT[i], reads=[XT[i]])
    return finalize(XT)


def _t5_bucket(dist):
    max_exact = 16
    n = np.maximum(dist, 0)
    nf = np.maximum(n, 1).astype(np.float32)
    large = max_exact + (np.log(nf / max_exact) / np.float32(np.log(2048 / max_exact))
                         * (32 - max_exact)).astype(np.int32)
    large = np.minimum(large, 31)
    return np.where(n < max_exact, n, large)


def _bucket_table():
    import jax, jax.numpy as jnp
    return None


def prep_shared(inp):
    import ml_dtypes
    f32 = np.float32
    sh = {}
    sh["g_mix"] = np.ascontiguousarray(inp["g_mix"][0], f32)
    sh["w_in"] = np.ascontiguousarray(inp["w_in"][0], f32)
    sh["w_out"] = np.ascontiguousarray(inp["w_out"][0], f32)
    cv = np.zeros((128, 40), f32)
    cw = inp["conv_w"][0]
    for c in range(4):
        for j in range(4):
            cv[:, c * 4 + j] = cw[j, c * 128:(c + 1) * 128]
    for n, key in enumerate(["conv_b", "b_rg", "b_ig", "lru_lambda", "g_lru_out", "g_attn_out"]):
        cv[:, 16 + 4 * n:20 + 4 * n] = inp[key][0].reshape(4, 128).T
    sh["cv"] = cv
    for name, key in (("wrg_bd", "w_rg"), ("wig_bd", "w_ig")):
        w = inp[key][0]
        bd = np.zeros((128, 4, 128), f32)
        for g in range(8):
            c, l = g // 2, g % 2
            bd[l * 64:(l + 1) * 64, c, l * 64:(l + 1) * 64] = w[g]
        sh[name] = bd
    rb = inp["rel_bias"].astype(f32)
    k = np.arange(128)[:, None]
    q = np.arange(256)[None, :]
    dist = q - k
    valid = (dist >= 0) & (dist <= 128)
    bt = np.full((128, 4, 3, 2, 256), NEG, f32)
    for bi, d in enumerate((1, 4, 16)):
        bucket = _t5_bucket(dist * d)
        for h in range(8):
            vals = rb[bucket, h]
            bt[:, h // 2, bi, h % 2, :] = np.where(valid, vals, f32(NEG))
    sh["biasT"] = bt.reshape(128, 4, 3 * 2 * 256)
    sh["ident_bf"] = np.eye(128, dtype=f32).astype(ml_dtypes.bfloat16)
    sh["g_ffn"] = np.ascontiguousarray(inp["g_ffn"][0], f32)
    sh["peer_wq"] = np.ascontiguousarray(inp["peer_wq"][0], f32)
    sh["k1T"] = np.ascontiguousarray(inp["peer_k1"][0].T, f32)
    sh["k2T"] = np.ascontiguousarray(inp["peer_k2"][0].T, f32)
    sh["iota16"] = np.tile(np.arange(16, dtype=f32)[None], (128, 1))
    sh["peer_u"] = np.ascontiguousarray(inp["peer_u"][0], f32)
    sh["peer_v"] = np.ascontiguousarray(inp["peer_v"][0], f32)
    sh["g_ple"] = np.ascontiguousarray(inp["g_ple"][0], f32)
    sh["w_ple_gate"] = np.ascontiguousarray(inp["w_ple_gate"][0], f32)
    sh["w_ple_proj"] = np.ascontiguousarray(inp["w_ple_proj"][0], f32)
    sh["g_final"] = np.ascontiguousarray(inp["g_final"], f32)
    return sh


_NC_CACHE = {}


def kernel(**inputs):
    inp = {k: np.asarray(v) for k, v in inputs.items()}
    sh = prep_shared(inp)
    if "nc" not in _NC_CACHE:
        nc = bass.Bass("TRN2", target_bir_lowering=False)
        build(nc)
        _NC_CACHE["nc"] = nc
    nc = _NC_CACHE["nc"]
    in_maps = []
    for b in range(8):
        m = dict(sh)
        m["x"] = np.ascontiguousarray(inp["x"][b], np.float32)
        m["p"] = np.ascontiguousarray(inp["p"][0, b], np.float32)
        in_maps.append(m)
    res = run_bass_kernel_spmd(nc, in_maps, core_ids=list(range(8)))
    return np.stack([np.asarray(r["out"]) for r in res.results], axis=0).astype(np.float32)
```

```python
import numpy as np
import concourse.bass as bass
import concourse.mybir as mybir
from concourse.bass_utils import run_bass_kernel_spmd

F32 = mybir.dt.float32
BF16 = mybir.dt.bfloat16
U32 = mybir.dt.uint32
I32 = mybir.dt.int32
AF = mybir.ActivationFunctionType
ALU = mybir.AluOpType
AX = mybir.AxisListType

T = 2048
D = 1024
NT = 16
EPS = 1e-6
NEG = -30000.0


class Buf:
    __slots__ = ("name", "lastw", "reads", "dsem", "dcnt")

    def __init__(self, name):
        self.name = name
        self.lastw = None
        self.reads = {}
        self.dsem = None
        self.dcnt = 0


class Prog:
    ENG = ("pe", "dve", "act", "pool", "sp")

    def __init__(self, nc):
        self.nc = nc
        self.q = {e: [] for e in self.ENG}
        self.sem = {e: nc.alloc_semaphore(f"prog_{e}") for e in self.ENG}
        self.cnt = {e: 0 for e in self.ENG}
        self.waited = {e: {} for e in self.ENG}
        self.nbuf = 0
        self.dsems = []
        self.free_dsems = []

    def buf(self, name=None):
        self.nbuf += 1
        return Buf(name or f"b{self.nbuf}")

    def bufs(self, name, n):
        return [self.buf(f"{name}{i}") for i in range(n)]

    def _tokens(self, eng, reads, writes):
        need = {}

        def add(tok):
            if tok is None:
                return
            s, v = tok
            if need.get(id(s), (None, 0))[1] < v:
                need[id(s)] = (s, v)

        for b in reads:
            add(b.lastw)
        for b in writes:
            add(b.lastw)
            for tok in b.reads.values():
                add(tok)
        own = self.sem[eng]
        waits = []
        w = self.waited[eng]
        for s, v in need.values():
            if eng == "pe" and s is own:
                continue
            if w.get(id(s), 0) < v:
                w[id(s)] = v
                waits.append((s, v))
        return waits

    def _commit(self, tok, reads, writes):
        s, v = tok
        for b in reads:
            if b.reads.get(id(s), (None, 0))[1] < v:
                b.reads[id(s)] = (s, v)
        for b in writes:
            b.lastw = tok
            b.reads = {}

    def op(self, eng, fn, reads=(), writes=()):
        waits = self._tokens(eng, reads, writes)
        self.cnt[eng] += 1
        tok = (self.sem[eng], self.cnt[eng])
        self.q[eng].append((waits, fn, tok[0], 1))
        self._commit(tok, reads, writes)
        return tok

    def dma(self, eng, fn, sb, reads=(), writes=()):
        waits = self._tokens(eng, reads, writes)
        if sb.dsem is None:
            sb.dsem = self.nc.alloc_semaphore(f"dma_{sb.name}")
            self.dsems.append(sb)
        sb.dcnt += 16
        tok = (sb.dsem, sb.dcnt)
        self.q[eng].append((waits, fn, sb.dsem, 16))
        self._commit(tok, reads, writes)
        return tok

    def share_dsem(self, bufs):
        return bufs

    def barrier(self):
        toks = [(self.sem[e], self.cnt[e]) for e in self.ENG if self.cnt[e] > 0]
        toks += [(b.dsem, b.dcnt) for b in self.dsems]
        for e in self.ENG:
            waits = []
            w = self.waited[e]
            for s, v in toks:
                if e == "pe" and s is self.sem["pe"]:
                    continue
                if w.get(id(s), 0) < v:
                    w[id(s)] = v
                    waits.append((s, v))
            if waits:
                self.q[e].append((waits, None, None, 0))

    def emit(self):
        nc = self.nc
        with nc.Block() as block:
            @block.tensor
            def _(e):
                self._emit("pe", e)

            @block.vector
            def _(e):
                self._emit("dve", e)

            @block.scalar
            def _(e):
                self._emit("act", e)

            @block.gpsimd
            def _(e):
                self._emit("pool", e)

            @block.sync
            def _(e):
                self._emit("sp", e)

    def _emit(self, name, e):
        for waits, fn, sem, inc in self.q[name]:
            for s, v in waits:
                e.wait_ge(s, v)
            if fn is not None:
                ins = fn(e)
                ins.then_inc(sem, inc)


class Alloc:
    def __init__(self, nc):
        self.nc = nc
        self.lo = (int(nc.sbuf_base) + 63) // 64 * 64
        self.hi = int(nc.sbuf_top)
        self.cur = self.lo
        self.n = 0
        self.offs = {}

    def alloc(self, name, shape, dtype):
        size = int(np.prod(shape[1:])) * mybir.dt.size(dtype)
        off = (self.cur + 63) // 64 * 64
        assert off + size <= self.hi, f"SBUF overflow allocating {name}: {off}+{size} > {self.hi}"
        self.cur = off + size
        self.n += 1
        self.offs[name] = off
        return self.nc.alloc_sbuf_tensor_at(f"{name}_{self.n}", list(shape), dtype, offset=off)

    def alloc_at(self, name, shape, dtype, off):
        self.n += 1
        return self.nc.alloc_sbuf_tensor_at(f"{name}_{self.n}", list(shape), dtype, offset=off)

    def mark(self):
        return self.cur

    def release(self, m):
        self.cur = m


def bcast_rows(ap1d, n, parts=128):
    return bass.AP(ap1d.tensor, ap1d.offset, [[0, parts], [1, n]])


def peer_topk(P, A, sc, SC, iota16, IOTA, idx_out, gw_out, OUT):
    m = A.mark()
    m16 = A.alloc("tk_m16", [128, 16, 16], F32)
    M16A = P.bufs("tk_m16a", 16); M16B = P.bufs("tk_m16b", 16)
    ix16 = A.alloc("tk_ix16", [128, 16, 16], U32)
    IXA = P.bufs("tk_ixa", 16); IXB = P.bufs("tk_ixb", 16)
    cand = A.alloc("tk_cand", [128, 8, 256], F32); SCR = P.bufs("tk_scr", 16)
    scr = cand[:].rearrange("p h (s k) -> p (h s) k", s=2)
    for j in range(16):
        P.op("dve", lambda e, j=j: e.max(out=m16[:, j, 0:8], in_=sc[:, j, :]), reads=[SC], writes=[M16A[j]])
    for j in range(16):
        P.op("dve", lambda e, j=j: e.match_replace(out=scr[:, j, :], in_to_replace=m16[:, j, 0:8],
                                                   in_values=sc[:, j, :], imm_value=-1e30),
             reads=[SC, M16A[j]], writes=[SCR[j]])
    for j in range(16):
        P.op("dve", lambda e, j=j: e.max(out=m16[:, j, 8:16], in_=scr[:, j, :]), reads=[SCR[j]], writes=[M16B[j]])
    for j in range(16):
        P.op("dve", lambda e, j=j: e.max_index(out=ix16[:, j, 0:8], in_max=m16[:, j, 0:8], in_values=sc[:, j, :]),
             reads=[SC, M16A[j]], writes=[IXA[j]])
    for j in range(16):
        P.op("dve", lambda e, j=j: e.max_index(out=ix16[:, j, 8:16], in_max=m16[:, j, 8:16], in_values=sc[:, j, :]),
             reads=[SC, M16B[j]], writes=[IXB[j]])
    M16 = M16A + M16B
    IX16 = IXA + IXB
    pm = list(m16[:].ap[0])
    t1 = bass.AP(m16[:].tensor, m16[:].offset, [pm, [32, 8], [1, 16], [0, 16]])
    t2 = bass.AP(m16[:].tensor, m16[:].offset + 16, [pm, [32, 8], [0, 16], [1, 16]])
    P.op("dve", lambda e: e.tensor_tensor(out=cand[:].rearrange("p h (a b) -> p h a b", a=16), in0=t1, in1=t2,
                                          op=ALU.add), reads=M16, writes=SCR)
    c16 = A.alloc("tk_c16", [128, 8, 16], F32); C16A = P.bufs("tk_c16a", 8); C16B = P.bufs("tk_c16b", 8)
    pos = A.alloc("tk_pos", [128, 8, 16], U32); POSA = P.bufs("tk_posa", 8); POSB = P.bufs("tk_posb", 8)
    eq = A.alloc("tk_eq", [128, 8, 256], F32); EQS = P.bufs("tk_eq", 8)
    for h in range(8):
        P.op("dve", lambda e, h=h: e.max(out=c16[:, h, 0:8], in_=cand[:, h, :]),
             reads=SCR[2 * h:2 * h + 2], writes=[C16A[h]])
    for h in range(8):
        P.op("dve", lambda e, h=h: e.match_replace(out=eq[:, h, :], in_to_replace=c16[:, h, 0:8],
                                                   in_values=cand[:, h, :], imm_value=-1e30),
             reads=SCR[2 * h:2 * h + 2] + [C16A[h]], writes=[EQS[h]])
    for h in range(8):
        P.op("dve", lambda e, h=h: e.max(out=c16[:, h, 8:16], in_=eq[:, h, :]), reads=[EQS[h]], writes=[C16B[h]])
    for h in range(8):
        P.op("dve", lambda e, h=h: e.max_index(out=pos[:, h, 0:8], in_max=c16[:, h, 0:8], in_values=cand[:, h, :]),
             reads=SCR[2 * h:2 * h + 2] + [C16A[h]], writes=[POSA[h]])
    for h in range(8):
        P.op("dve", lambda e, h=h: e.max_index(out=pos[:, h, 8:16], in_max=c16[:, h, 8:16], in_values=cand[:, h, :]),
             reads=SCR[2 * h:2 * h + 2] + [C16B[h]], writes=[POSB[h]])
    C16 = P.buf("tk_c16all"); POS = P.buf("tk_posall"); EQ = P.buf("tk_eqall")
    for Bn, lst in ((C16, C16A + C16B), (POS, POSA + POSB), (EQ, EQS)):
        for b_ in lst:
            tok = b_.lastw
            if tok is not None and (Bn.lastw is None or True):
                s_, v_ = tok
                if Bn.reads.get(id(s_), (None, 0))[1] < v_:
                    Bn.reads[id(s_)] = (s_, v_)
            for tk in b_.reads.values():
                s_, v_ = tk
                if Bn.reads.get(id(s_), (None, 0))[1] < v_:
                    Bn.reads[id(s_)] = (s_, v_)
    eq4 = eq[:].rearrange("p h (a b) -> p h a b", a=16)
    au = A.alloc("tk_au", [128, 8, 16], U32); AU = P.buf("tk_au")
    bu = A.alloc("tk_bu", [128, 8, 16], U32); BU = P.buf("tk_bu")
    P.op("dve", lambda e: e.tensor_single_scalar(out=au[:], in_=pos[:], scalar=4, op=ALU.logical_shift_right),
         reads=[], writes=[AU, POS])
    P.op("dve", lambda e: e.tensor_single_scalar(out=bu[:], in_=pos[:], scalar=15, op=ALU.bitwise_and),
         reads=[POS], writes=[BU])
    af = A.alloc("tk_af", [128, 8, 16], F32); AFb = P.buf("tk_af")
    bf = A.alloc("tk_bf", [128, 8, 16], F32); BFb = P.buf("tk_bf")
    ixf = A.alloc("tk_ixf", [128, 16, 16], F32); IXF = P.buf("tk_ixf")
    P.op("dve", lambda e: e.tensor_copy(out=af[:], in_=au[:]), reads=[AU], writes=[AFb])
    P.op("dve", lambda e: e.tensor_copy(out=bf[:], in_=bu[:]), reads=[BU], writes=[BFb])
    P.op("dve", lambda e: e.tensor_copy(out=ixf[:], in_=ix16[:]), reads=IX16, writes=[IXF])
    idf = A.alloc("tk_idf", [128, 2, 8, 16], F32); IDF = P.buf("tk_idf")
    p0 = list(m16[:].ap[0])
    for s_, (vf, VF) in enumerate(((af, AFb), (bf, BFb))):
        vb_ = bass.AP(vf.tensor if hasattr(vf, "tensor") else vf, vf[:].offset, [list(vf[:].ap[0]), [16, 8], [1, 16], [0, 16]])
        io_ = bass.AP(iota16.tensor if hasattr(iota16, "tensor") else iota16, iota16[:].offset,
                      [list(iota16[:].ap[0]), [0, 8], [0, 16], [1, 16]])
        P.op("dve", lambda e, vb_=vb_, io_=io_: e.tensor_tensor(out=eq4, in0=vb_, in1=io_, op=ALU.is_equal),
             reads=[VF, IOTA], writes=[EQ])
        tab = bass.AP(ixf.tensor if hasattr(ixf, "tensor") else ixf, ixf[:].offset + 16 * s_,
                      [p0, [32, 8], [0, 16], [1, 16]])
        P.op("dve", lambda e, tab=tab: e.tensor_tensor(out=eq4, in0=eq4, in1=tab, op=ALU.mult),
             reads=[EQ, IXF], writes=[EQ])
        P.op("dve", lambda e, s_=s_: e.tensor_reduce(out=idf[:, s_, :, :], in_=eq4, axis=AX.X, op=ALU.add),
             reads=[EQ], writes=[IDF])
    P.op("dve", lambda e: e.scalar_tensor_tensor(out=idf[:, 0, :, :], in0=idf[:, 0, :, :], scalar=128.0,
                                                 in1=idf[:, 1, :, :], op0=ALU.mult, op1=ALU.add),
         reads=[IDF], writes=[IDF])
    P.op("dve", lambda e: e.tensor_copy(out=idx_out, in_=idf[:, 0, :, :].rearrange("p h k -> p (h k)")),
         reads=[IDF], writes=[OUT])
    cmax = bass.AP(c16.tensor if hasattr(c16, "tensor") else c16, c16[:].offset, [list(c16[:].ap[0]), [16, 8], [0, 16]])
    P.op("dve", lambda e: e.tensor_tensor(out=bf[:], in0=c16[:], in1=cmax, op=ALU.subtract),
         reads=[BFb], writes=[BFb, C16])
    P.op("act", lambda e: e.activation(out=bf[:], in_=bf[:], func=AF.Exp), reads=[BFb], writes=[BFb])
    ssum = A.alloc("tk_ssum", [128, 8], F32); SSUM = P.buf("tk_ssum")
    P.op("dve", lambda e: e.tensor_reduce(out=ssum[:], in_=bf[:], axis=AX.X, op=ALU.add), reads=[BFb], writes=[SSUM])
    P.op("dve", lambda e: e.reciprocal(out=ssum[:], in_=ssum[:]), reads=[SSUM], writes=[SSUM])
    sb_ = bass.AP(ssum.tensor if hasattr(ssum, "tensor") else ssum, ssum[:].offset, [list(ssum[:].ap[0]), [1, 8], [0, 16]])
    P.op("dve", lambda e: e.tensor_tensor(out=gw_out.rearrange("p (h k) -> p h k", h=8), in0=bf[:], in1=sb_, op=ALU.mult),
         reads=[BFb, SSUM], writes=[OUT])
    A.release(m)


def build(nc, stage="full", dumps=()):
    P = Prog(nc)
    A = Alloc(nc)
    dumps = set(dumps)
    dump_list = []

    def din(name, shape, dt=F32):
        return nc.dram_tensor(name, list(shape), dt, kind="ExternalInput").ap()

    x_d = din("x", [T, D])
    p_d = din("p", [T, 256])
    g_mix_d = din("g_mix", [D])
    w_in_d = din("w_in", [D, 2560])
    cv_d = din("cv", [128, 40])
    wrg_d = din("wrg_bd", [128, 4, 128])
    wig_d = din("wig_bd", [128, 4, 128])
    biasT_d = din("biasT", [128, 4, 3 * 2 * 256])
    w_out_d = din("w_out", [D, D])
    identb_d = din("ident_bf", [128, 128], BF16)
    g_ffn_d = din("g_ffn", [D])
    peer_wq_d = din("peer_wq", [D, 2048])
    k1T_d = din("k1T", [128, 128])
    k2T_d = din("k2T", [128, 128])
    iota_d = din("iota16", [128, 16])
    peer_u_d = din("peer_u", [16384, D])
    peer_v_d = din("peer_v", [16384, D])
    g_ple_d = din("g_ple", [D])
    w_gate_d = din("w_ple_gate", [D, D])
    w_proj_d = din("w_ple_proj", [256, D])
    g_final_d = din("g_final", [D])
    out_d = nc.dram_tensor("out", [T, D], F32, kind="ExternalOutput").ap()

    def dump(name, ap, bufs, shape, dt=F32):
        if name not in dumps:
            return
        d = nc.dram_tensor("dbg_" + name, list(shape), dt, kind="ExternalOutput").ap()
        DB = P.buf("dbg_" + name)
        P.dma("sp", lambda e: e.dma_start(out=d, in_=ap), DB, reads=list(bufs))
        dump_list.append(DB)

    psall = nc.alloc_psum_tensor("psall", [128, 8 * 512], F32)
    ps_t = [psall[:, i * 512:(i + 1) * 512] for i in range(8)]
    ps_b = [P.buf(f"psb{i}") for i in range(8)]
    ps_rr = [0]

    def psum(idx=None):
        if idx is None:
            idx = ps_rr[0]
            ps_rr[0] = (idx + 1) % 8
        return ps_t[idx], ps_b[idx]

    def psum_pair():
        idx = (ps_rr[0] + 1) // 2 * 2 % 8
        ps_rr[0] = (idx + 2) % 8
        return psall[:, idx * 512:(idx + 2) * 512].rearrange("p (a q) -> p a q", a=2), [ps_b[idx], ps_b[idx + 1]]

    identb = A.alloc("identb", [128, 128], BF16); IDB = P.buf("identb")
    P.dma("sp", lambda e: e.dma_start(out=identb[:], in_=identb_d), IDB, writes=[IDB])
    ones_f = A.alloc("ones_f", [128, 128], F32); ONF = P.buf("ones_f")
    P.op("pool", lambda e: e.memset(ones_f[:], 1.0), writes=[ONF])
    ones_b = A.alloc("ones_b", [128, 64], BF16); ONB = P.buf("ones_b")
    P.op("pool", lambda e: e.memset(ones_b[:], 1.0), writes=[ONB])
    cv = A.alloc("cv", [128, 40], F32); CV = P.buf("cv")
    P.dma("sp", lambda e: e.dma_start(out=cv[:], in_=cv_d), CV, writes=[CV])
    gB = A.alloc("gB", [128, D], F32); GB = P.buf("gB")
    rstd_all = A.alloc("rstd_all", [128, NT], F32); RSTD = P.buf("rstd_all")

    uT = A.alloc("uT", [128, 8, T], BF16); UT = P.bufs("uT", NT)
    yT = A.alloc("yT", [128, 8, T], BF16); YT = P.bufs("yT", 8)

    uv_d = nc.dram_tensor("uv16", [16384, 2 * D], BF16, kind="Internal").ap()
    RPP = 2
    pst = [A.alloc(f"pst{j}", [128, RPP, D], F32) for j in range(2)]; PST = P.bufs("pst", 2)
    psb_ = [A.alloc(f"ppb{j}", [128, RPP, D], BF16) for j in range(2)]; PSB = P.bufs("ppb", 2)
    UVD = P.buf("uv16")

    def prepass_gen():
        steps = []
        for t_, c0 in ((peer_u_d, 0), (peer_v_d, D)):
            for blk in range(16384 // (128 * RPP)):
                r0 = blk * 128 * RPP
                src = t_[r0:r0 + 128 * RPP, :].rearrange("(p r) d -> p r d", r=RPP)
                dst = uv_d[r0:r0 + 128 * RPP, c0:c0 + D].rearrange("(p r) d -> p r d", r=RPP)
                steps.append((src, dst))

        def load(n):
            j = n % 2
            src = steps[n][0]
            P.dma("sp", lambda e, j=j, src=src: e.dma_start(out=pst[j][:], in_=src), PST[j], writes=[PST[j]])

        load(0)
        for n in range(len(steps)):
            j = n % 2
            dst = steps[n][1]
            P.op("pool", lambda e, j=j: e.tensor_copy(out=psb_[j][:], in_=pst[j][:]),
                 reads=[PST[j]], writes=[PSB[j]])
            if n + 1 < len(steps):
                load(n + 1)
            P.dma("sp", lambda e, j=j, dst=dst: e.dma_start(out=dst, in_=psb_[j][:]), PSB[j],
                  reads=[PSB[j]], writes=[UVD])
            yield

    pp = prepass_gen()

    def pp_adv(n):
        for _ in range(n):
            next(pp, None)

    def norm_transpose(src_tiles, gvec_d, dstT, DST, hook=None):
        m = A.mark()
        P.dma("sp", lambda e: e.dma_start(out=gB[:], in_=bcast_rows(gvec_d, D)), GB, writes=[GB])
        sq = A.alloc("nt_sq", [128, D], BF16); SQ = P.buf("nt_sq")
        ss = A.alloc("nt_ss", [128, 2], F32); SS = P.buf("nt_ss")
        xs = [A.alloc(f"nt_xs{j}", [128, D], BF16) for j in range(2)]
        XS = P.bufs("nt_xs", 2)
        for i in range(NT):
            if hook is not None:
                hook()
            src, SRC = src_tiles(i)
            P.op("act", lambda e, src=src: e.activation(out=sq[:], in_=src, func=AF.Square,
                                                         accum_out=ss[:, 0:1]),
                 reads=SRC, writes=[SQ, SS])
            P.op("act", lambda e: e.activation(out=ss[:, 1:2], in_=ss[:, 0:1], func=AF.Sqrt,
                                               scale=1.0 / D, bias=EPS),
                 reads=[SS], writes=[SS])
            P.op("dve", lambda e, i=i: e.reciprocal(out=rstd_all[:, i:i + 1], in_=ss[:, 1:2]),
                 reads=[SS], writes=[RSTD])
            j = i % 2
            P.op("dve", lambda e, src=src, i=i, j=j: e.scalar_tensor_tensor(
                out=xs[j][:], in0=src, scalar=rstd_all[:, i:i + 1], in1=gB[:],
                op0=ALU.mult, op1=ALU.mult), reads=SRC + [RSTD, GB], writes=[XS[j]])
            ps, PS = psum()
            psb = ps[:].bitcast(BF16)
            for k in range(8):
                P.op("pe", lambda e, j=j, k=k, psb=psb: e.transpose(
                    out=psb[:, k * 128:(k + 1) * 128], in_=xs[j][:, k * 128:(k + 1) * 128],
                    identity=identb[:]), reads=[XS[j], IDB], writes=[PS])
            P.op("act", lambda e, i=i, psb=psb: e.copy(
                out=dstT[:, :, i * 128:(i + 1) * 128],
                in_=psb.rearrange("p (k t) -> p k t", k=8)), reads=[PS], writes=[DST[i]])
        P.barrier()
        A.release(m)

    wsl = [A.alloc(f"wsl{j}", [128, 8, 128], BF16) for j in range(3)]
    WSL = P.bufs("wsl", 3)
    wsl_rr = [0]

    def load_wchunk(w_d, col0):
        j = wsl_rr[0]
        wsl_rr[0] = (j + 1) % 3
        src = w_d.rearrange("(k p) c -> p k c", p=128)[:, :, col0:col0 + 128]
        P.dma("pool", lambda e: e.dma_start(out=wsl[j][:], in_=src), WSL[j], writes=[WSL[j]])
        return wsl[j], WSL[j]

    def proj_chunk(w_d, col0, srcT, SRC, evac):
        w, W = load_wchunk(w_d, col0)
        for tq in range(4):
            ps, PS = psum()
            for k in range(8):
                P.op("pe", lambda e, ps=ps, k=k, tq=tq, w=w: e.matmul(
                    ps[:], lhsT=w[:, k, :], rhs=srcT[:, k, tq * 512:(tq + 1) * 512],
                    start=(k == 0), stop=(k == 7)),
                    reads=[W] + SRC[tq * 4:(tq + 1) * 4], writes=[PS])
            evac(tq, ps, PS)

    def finalize(out_bufs):
        final = []
        w = P.waited["sp"]
        for b in list(out_bufs) + dump_list:
            if b.dsem is not None and w.get(id(b.dsem), 0) < b.dcnt:
                w[id(b.dsem)] = b.dcnt
                final.append((b.dsem, b.dcnt))
        P.q["sp"].append((final, None, None, 0))
        P.emit()
        return nc

    m_phase = A.mark()
    xin = [A.alloc(f"xin{j}", [128, D], F32) for j in range(2)]
    XIN = P.bufs("xin", 2)

    def x_src(i):
        j = i % 2
        P.dma("sp", lambda e: e.dma_start(out=xin[j][:], in_=x_d[i * 128:(i + 1) * 128, :]),
              XIN[j], writes=[XIN[j]])
        return xin[j][:], [XIN[j]]

    norm_transpose(x_src, g_mix_d, uT, UT, hook=lambda: pp_adv(1))
    dump("uT", uT[:], UT, [128, 8, T], BF16)
    P.barrier()
    A.release(m_phase)
    if stage == "p1":
        return finalize([])

    m_phase = A.mark()
    y32 = A.alloc("y32", [128, 4, T], F32); Y32 = P.bufs("y32", 4)
    wrg = A.alloc("wrg", [128, 4, 128], BF16); WRG = P.buf("wrg")
    wig = A.alloc("wig", [128, 4, 128], BF16); WIG = P.buf("wig")
    P.dma("pool", lambda e: e.dma_start(out=wrg[:], in_=wrg_d), WRG, writes=[WRG])
    P.dma("pool", lambda e: e.dma_start(out=wig[:], in_=wig_d), WIG, writes=[WIG])
    nsp = A.alloc("nsp", [128, 4], F32); NSP = P.buf("nsp")
    P.op("act", lambda e: e.activation(out=nsp[:], in_=cv[:, 28:32], func=AF.Exp, scale=-1.0),
         reads=[CV], writes=[NSP])
    P.op("act", lambda e: e.activation(out=nsp[:], in_=nsp[:], func=AF.Ln, scale=1.0, bias=1.0),
         reads=[NSP], writes=[NSP])
    P.op("dve", lambda e: e.tensor_scalar(out=nsp[:], in0=nsp[:], scalar1=-8.0, scalar2=None,
                                          op0=ALU.mult), reads=[NSP], writes=[NSP])

    def big(name, dt=F32):
        return A.alloc(name, [128, T], dt), P.buf(name)

    xl, XL = big("xl"); gt, GT = big("gt"); xc, XC = big("xc"); xcb, XCB = big("xcb", BF16)
    ra, RA = big("ra"); ig, IG = big("ig"); tmp, TMP = big("tmp")

    for c in range(4):
        def ev_xl(tq, ps, PS):
            P.op("act", lambda e, ps=ps, tq=tq: e.copy(out=xl[:, tq * 512:(tq + 1) * 512], in_=ps[:]),
                 reads=[PS], writes=[XL])

        def ev_gt(tq, ps, PS):
            P.op("act", lambda e, ps=ps, tq=tq: e.copy(out=gt[:, tq * 512:(tq + 1) * 512], in_=ps[:]),
                 reads=[PS], writes=[GT])

        proj_chunk(w_in_d, c * 128, uT, UT, ev_xl)
        pp_adv(2)
        proj_chunk(w_in_d, 512 + c * 128, uT, UT, ev_gt)
        pp_adv(2)
        P.op("dve", lambda e, c=c: e.tensor_scalar(
            out=xc[:], in0=xl[:], scalar1=cv[:, c * 4 + 3:c * 4 + 4], scalar2=cv[:, 16 + c:17 + c],
            op0=ALU.mult, op1=ALU.add), reads=[XL, CV], writes=[XC])
        for s in (1, 2, 3):
            P.op("dve", lambda e, c=c, s=s: e.scalar_tensor_tensor(
                out=xc[:, s:], in0=xl[:, 0:T - s], scalar=cv[:, c * 4 + 3 - s:c * 4 + 4 - s],
                in1=xc[:, s:], op0=ALU.mult, op1=ALU.add), reads=[XL, CV, XC], writes=[XC])
        P.op("act", lambda e: e.copy(out=xcb[:], in_=xc[:]), reads=[XC], writes=[XCB])
        for tq in range(4):
            ps, PS = psum()
            P.op("pe", lambda e, ps=ps, tq=tq, c=c: e.matmul(
                ps[:], lhsT=wrg[:, c, :], rhs=xcb[:, tq * 512:(tq + 1) * 512], start=True, stop=True),
                reads=[WRG, XCB], writes=[PS])
            P.op("act", lambda e, ps=ps, tq=tq, c=c: e.activation(
                out=ra[:, tq * 512:(tq + 1) * 512], in_=ps[:], func=AF.Sigmoid,
                bias=cv[:, 20 + c:21 + c], scale=1.0), reads=[PS, CV], writes=[RA])
        for tq in range(4):
            ps, PS = psum()
            P.op("pe", lambda e, ps=ps, tq=tq, c=c: e.matmul(
                ps[:], lhsT=wig[:, c, :], rhs=xcb[:, tq * 512:(tq + 1) * 512], start=True, stop=True),
                reads=[WIG, XCB], writes=[PS])
            P.op("act", lambda e, ps=ps, tq=tq, c=c: e.activation(
                out=ig[:, tq * 512:(tq + 1) * 512], in_=ps[:], func=AF.Sigmoid,
                bias=cv[:, 24 + c:25 + c], scale=1.0), reads=[PS, CV], writes=[IG])
        P.op("act", lambda e, c=c: e.activation(out=ra[:], in_=ra[:], func=AF.Exp,
                                                scale=nsp[:, c:c + 1]), reads=[RA, NSP], writes=[RA])
        P.op("dve", lambda e: e.tensor_tensor(out=tmp[:], in0=ra[:], in1=ra[:], op=ALU.mult),
             reads=[RA], writes=[TMP])
        P.op("act", lambda e: e.activation(out=tmp[:], in_=tmp[:], func=AF.Sqrt, scale=-1.0, bias=1.0),
             reads=[TMP], writes=[TMP])
        P.op("dve", lambda e: e.tensor_tensor(out=ig[:], in0=ig[:], in1=xc[:], op=ALU.mult),
             reads=[IG, XC], writes=[IG])
        P.op("dve", lambda e: e.tensor_tensor(out=ig[:], in0=ig[:], in1=tmp[:], op=ALU.mult),
             reads=[IG, TMP], writes=[IG])
        P.op("dve", lambda e: e.tensor_tensor_scan(out=tmp[:], data0=ra[:], data1=ig[:], initial=0.0,
                                                   op0=ALU.mult, op1=ALU.add),
             reads=[RA, IG], writes=[TMP])
        P.op("act", lambda e: e.activation(out=gt[:], in_=gt[:], func=AF.Gelu_apprx_tanh),
             reads=[GT], writes=[GT])
        P.op("dve", lambda e, c=c: e.tensor_tensor(out=y32[:, c, :], in0=tmp[:], in1=gt[:], op=ALU.mult),
             reads=[TMP, GT], writes=[Y32[c]])

    def group_rmsnorm(gcol0, ybase, y32, Y32, tmp, TMP, ra, RA):
        pss = [psum(i) for i in range(4)]
        for c in range(4):
            P.op("act", lambda e, c=c: e.activation(out=tmp[:], in_=y32[:, c, :], func=AF.Square),
                 reads=[Y32[c]], writes=[TMP])
            for tq in range(4):
                ps, PS = pss[tq]
                P.op("pe", lambda e, ps=ps, tq=tq, c=c: e.matmul(
                    ps[:], lhsT=ones_f[:], rhs=tmp[:, tq * 512:(tq + 1) * 512],
                    start=(c == 0), stop=(c == 3)), reads=[ONF, TMP], writes=[PS])
        for tq in range(4):
            ps, PS = pss[tq]
            P.op("act", lambda e, ps=ps, tq=tq: e.activation(
                out=ra[:, tq * 512:(tq + 1) * 512], in_=ps[:], func=AF.Sqrt, scale=1.0 / 512, bias=EPS),
                reads=[PS], writes=[RA])
        P.op("dve", lambda e: e.reciprocal(out=ra[:], in_=ra[:]), reads=[RA], writes=[RA])
        for c in range(4):
            P.op("dve", lambda e, c=c: e.scalar_tensor_tensor(
                out=yT[:, ybase + c, :], in0=y32[:, c, :], scalar=cv[:, gcol0 + c:gcol0 + c + 1],
                in1=ra[:], op0=ALU.mult, op1=ALU.mult), reads=[Y32[c], CV, RA], writes=[YT[ybase + c]])

    dump("y_lru", y32[:], Y32, [128, 4, T])
    if stage == "p2a":
        return finalize([])
    group_rmsnorm(32, 0, y32, Y32, tmp, TMP, ra, RA)
    if stage == "p2":
        dump("yT", yT[:], YT, [128, 8, T], BF16)
    P.barrier()
    A.release(m_phase)
    if stage == "p2":
        return finalize([])

    m_phase = A.mark()
    y3 = A.alloc("y32a", [128, 4, T], F32); Y3 = P.bufs("y32a", 4)
    ra3, RA3 = big("ra2"); tmp3, TMP3 = big("tmp2")
    qT, QT = big("qT", BF16); kT, KT = big("kT", BF16)
    dacc, DACC = big("dacc")
    bias_sb = A.alloc("bias_sb", [128, 3, 2, 256], F32); BIAS = P.buf("bias_sb")
    vb = A.alloc("vb", [128, 16, 128], BF16); VB = P.buf("vb")
    tS = [A.alloc(f"tS{j}", [128, 2, 256], F32) for j in range(2)]; TS = P.bufs("tS", 2)
    pT = [A.alloc(f"pT{j}", [128, 2, 256], BF16) for j in range(2)]; PT = P.bufs("pT", 2)
    it = 0
    for c in range(4):
        OACC = Y3[c]
        P.dma("sp", lambda e, c=c: e.dma_start(
            out=bias_sb[:].rearrange("p a b q -> p (a b q)"), in_=biasT_d[:, c, :]), BIAS, writes=[BIAS])

        def ev_q(tq, ps, PS):
            P.op("act", lambda e, ps=ps, tq=tq: e.activation(
                out=qT[:, tq * 512:(tq + 1) * 512], in_=ps[:], func=AF.Copy, scale=0.125),
                reads=[PS], writes=[QT])

        def ev_k(tq, ps, PS):
            P.op("act", lambda e, ps=ps, tq=tq: e.copy(out=kT[:, tq * 512:(tq + 1) * 512], in_=ps[:]),
                 reads=[PS], writes=[KT])

        proj_chunk(w_in_d, 1024 + c * 128, uT, UT, ev_q)
        proj_chunk(w_in_d, 1536 + c * 128, uT, UT, ev_k)
        wv, WV = load_wchunk(w_in_d, 2048 + c * 128)
        P.op("pool", lambda e, c=c: e.memset(y3[:, c, :], 0.0), writes=[OACC])
        P.op("pool", lambda e: e.memset(dacc[:], 0.0), writes=[DACC])
        for bi, d in enumerate((1, 4, 16)):
            nb = 16 // d
            for g4 in range(4):
                ps, PS = psum()
                for u4 in range(4):
                    ti = g4 * 4 + u4
                    r, m = ti // nb, ti % nb
                    t0 = r + d * 128 * m
                    for k in range(8):
                        P.op("pe", lambda e, ps=ps, u4=u4, k=k, t0=t0, d=d, wv=wv: e.matmul(
                            ps[:, u4 * 128:(u4 + 1) * 128],
                            lhsT=uT[:, k, t0:t0 + d * 127 + 1:d], rhs=wv[:, k, :],
                            start=(k == 0), stop=(k == 7)), reads=[WV] + UT, writes=[PS])
                P.op("act", lambda e, ps=ps, g4=g4: e.copy(
                    out=vb[:, g4 * 4:(g4 + 1) * 4, :], in_=ps[:].rearrange("p (a b) -> p a b", a=4)),
                    reads=[PS], writes=[VB])
            for r in range(d):
                for m in range(nb):
                    nq = 256 if m + 1 < nb else 128
                    t0 = r + d * 128 * m
                    kend = t0 + d * 127 + 1
                    qend = t0 + d * (nq - 1) + 1
                    j = it % 2
                    it += 1
                    if it % 3 == 0:
                        pp_adv(1)
                    ps, PSP = psum_pair()
                    for hh in range(2):
                        p0 = hh * 64
                        P.op("pe", lambda e, ps=ps, hh=hh, p0=p0, t0=t0, kend=kend, qend=qend, d=d, nq=nq: e.matmul(
                            ps[:, hh, 0:nq], lhsT=kT[p0:p0 + 64, t0:kend:d],
                            rhs=qT[p0:p0 + 64, t0:qend:d], start=True, stop=True),
                            reads=[KT, QT], writes=[PSP[hh]])
                    P.op("dve", lambda e, ps=ps, j=j, bi=bi, nq=nq: e.tensor_tensor(
                        out=tS[j][:, :, 0:nq], in0=ps[:, :, 0:nq],
                        in1=bias_sb[:, bi, :, 0:nq], op=ALU.add), reads=PSP + [BIAS], writes=[TS[j]])
                    P.op("act", lambda e, j=j, nq=nq: e.activation(
                        out=pT[j][:, :, 0:nq], in_=tS[j][:, :, 0:nq], func=AF.Exp),
                        reads=[TS[j]], writes=[PT[j]])
                    ps2, PS2 = psum()
                    ti = r * nb + m
                    for hh in range(2):
                        p0 = hh * 64
                        P.op("pe", lambda e, ps2=ps2, hh=hh, p0=p0, ti=ti, j=j, nq=nq: e.matmul(
                            ps2[p0:p0 + 64, 0:nq], lhsT=vb[:, ti, hh * 64:(hh + 1) * 64],
                            rhs=pT[j][:, hh, 0:nq], start=True, stop=True),
                            reads=[VB, PT[j]], writes=[PS2])
                        P.op("pe", lambda e, ps2=ps2, hh=hh, p0=p0, j=j, nq=nq: e.matmul(
                            ps2[p0:p0 + 64, 256:256 + nq], lhsT=ones_b[:, 0:64],
                            rhs=pT[j][:, hh, 0:nq], start=True, stop=True),
                            reads=[ONB, PT[j]], writes=[PS2])
                    P.op("dve", lambda e, ps2=ps2, c=c, t0=t0, qend=qend, d=d, nq=nq: e.tensor_tensor(
                        out=y3[:, c, t0:qend:d], in0=y3[:, c, t0:qend:d], in1=ps2[:, 0:nq], op=ALU.add),
                        reads=[PS2, OACC], writes=[OACC])
                    P.op("dve", lambda e, ps2=ps2, t0=t0, qend=qend, d=d, nq=nq: e.tensor_tensor(
                        out=dacc[:, t0:qend:d], in0=dacc[:, t0:qend:d], in1=ps2[:, 256:256 + nq], op=ALU.add),
                        reads=[PS2, DACC], writes=[DACC])
        P.op("dve", lambda e: e.reciprocal(out=dacc[:], in_=dacc[:]), reads=[DACC], writes=[DACC])
        P.op("dve", lambda e, c=c: e.tensor_tensor(out=y3[:, c, :], in0=y3[:, c, :], in1=dacc[:], op=ALU.mult),
             reads=[OACC, DACC], writes=[OACC])
    dump("y_att", y3[:], Y3, [128, 4, T])
    if stage == "p3a":
        return finalize([])
    group_rmsnorm(36, 4, y3, Y3, tmp3, TMP3, ra3, RA3)
    if stage != "p2":
        dump("yT", yT[:], YT, [128, 8, T], BF16)
    P.barrier()
    A.release(m_phase)

    xt = A.alloc("xt", [128, NT, D], F32); XT = P.bufs("xt", NT)
    m_phase = A.mark()
    wo = A.alloc("wo", [128, 8, D], BF16); WO = P.buf("wo")
    P.dma("pool", lambda e: e.dma_start(out=wo[:], in_=w_out_d.rearrange("(k p) c -> p k c", p=128)),
          WO, writes=[WO])
    for i in range(NT):
        P.dma("sp", lambda e, i=i: e.dma_start(out=xt[:, i, :], in_=x_d[i * 128:(i + 1) * 128, :]),
              XT[i], writes=[XT[i]])
    for i in range(NT):
        for hf in range(2):
            pp_adv(1)
            ps, PS = psum()
            for k in range(8):
                P.op("pe", lambda e, ps=ps, k=k, i=i, hf=hf: e.matmul(
                    ps[:], lhsT=yT[:, k, i * 128:(i + 1) * 128], rhs=wo[:, k, hf * 512:(hf + 1) * 512],
                    start=(k == 0), stop=(k == 7)), reads=[WO] + YT, writes=[PS])
            P.op("dve", lambda e, ps=ps, i=i, hf=hf: e.tensor_tensor(
                out=xt[:, i, hf * 512:(hf + 1) * 512], in0=xt[:, i, hf * 512:(hf + 1) * 512],
                in1=ps[:], op=ALU.add), reads=[PS, XT[i]], writes=[XT[i]])
    for _ in pp:
        pass
    P.barrier()
    A.release(m_phase)

    if stage == "mixer":
        for i in range(NT):
            P.dma("sp", lambda e, i=i: e.dma_start(out=out_d[i * 128:(i + 1) * 128, :], in_=xt[:, i, :]),
                  XT[i], reads=[XT[i]])
        return finalize(XT)

    def xt_src(i):
        return xt[:, i, :], [XT[i]]

    m_phase = A.mark()
    norm_transpose(xt_src, g_ffn_d, uT, UT)
    wq = yT
    WQ = P.buf("wq")
    wq_src = peer_wq_d.rearrange("(k p) c -> p k c", p=128)
    for hf in range(2):
        P.dma("pool", lambda e, hf=hf: e.dma_start(out=wq[:, :, hf * 1024:(hf + 1) * 1024],
                                                   in_=wq_src[:, :, hf * 1024:(hf + 1) * 1024]),
              WQ, writes=[WQ])
    kTs = A.alloc("kTs", [128, 2, 128], F32); KTS = P.buf("kTs")
    P.dma("sp", lambda e: e.dma_start(out=kTs[:, 0, :], in_=k1T_d), KTS, writes=[KTS])
    P.dma("sp", lambda e: e.dma_start(out=kTs[:, 1, :], in_=k2T_d), KTS, writes=[KTS])
    iota16 = A.alloc("iota16", [128, 16], F32); IOTA = P.buf("iota16")
    P.dma("sp", lambda e: e.dma_start(out=iota16[:], in_=iota_d), IOTA, writes=[IOTA])
    idx_all = A.alloc("idx_all", [128, NT, 128], I32); IDX = P.bufs("idx_all", NT)
    gw_all = A.alloc("gw_all", [128, NT, 128], F32)
    m_sub = A.mark()
    assert A.offs["ppb1"] == A.offs["ppb0"] + RPP * D * 2
    qTt = A.alloc_at("qTt", [128, 16, 128], F32, A.offs["pst0"]); QTT = P.buf("qTt")
    scs = [A.alloc_at("sc0", [128, 16, 128], F32, A.offs["pst1"]),
           A.alloc_at("sc1", [128, 16, 128], F32, A.offs["ppb0"])]
    SCS = P.bufs("sc", 2)
    for i in range(NT):
        for jg in range(4):
            ps, PS = psum()
            for u in range(4):
                j = jg * 4 + u
                for k in range(8):
                    P.op("pe", lambda e, ps=ps, u=u, j=j, k=k, i=i: e.matmul(
                        ps[:, u * 128:(u + 1) * 128], lhsT=wq[:, k, j * 128:(j + 1) * 128],
                        rhs=uT[:, k, i * 128:(i + 1) * 128], start=(k == 0), stop=(k == 7)),
                        reads=[WQ, UT[i]], writes=[PS])
            P.op("act", lambda e, ps=ps, jg=jg: e.copy(
                out=qTt[:, jg * 4:(jg + 1) * 4, :], in_=ps[:].rearrange("p (a b) -> p a b", a=4)),
                reads=[PS], writes=[QTT])
        sc, SC = scs[i % 2], SCS[i % 2]
        for jg in range(4):
            ps, PS = psum()
            for u in range(4):
                j = jg * 4 + u
                P.op("pe", lambda e, ps=ps, u=u, j=j: e.matmul(
                    ps[:, u * 128:(u + 1) * 128], lhsT=qTt[:, j, :], rhs=kTs[:, j % 2, :],
                    start=True, stop=True), reads=[QTT, KTS], writes=[PS])
            P.op("act", lambda e, ps=ps, jg=jg, sc=sc: e.copy(
                out=sc[:, jg * 4:(jg + 1) * 4, :], in_=ps[:].rearrange("p (a b) -> p a b", a=4)),
                reads=[PS], writes=[SC])
        if i == 0:
            dump("qTt0", qTt[:], [QTT], [128, 16, 128])
            dump("sc0", sc[:], [SC], [128, 16, 128])
        peer_topk(P, A, sc, SC, iota16, IOTA, idx_all[:, i, :], gw_all[:, i, :], IDX[i])
    dump("idx_all", idx_all[:], IDX, [128, NT, 128], I32)
    dump("gw_all", gw_all[:], IDX, [128, NT, 128])
    P.barrier()
    A.release(m_sub)
    if stage == "topk":
        return finalize([])
    NS = 12
    GE = 4
    base_off = A.offs["uT"]
    assert A.offs["yT"] == base_off + 8 * T * 2
    gs = [A.alloc_at(f"gs{j}", [128, 2 * D], BF16, base_off + j * 4096) for j in range(NS)]
    GS = P.bufs("gs", NS)
    xn32 = A.alloc("xn32", [128, D], F32); XN = P.buf("xn32")
    junk = A.alloc("junk", [128, D], BF16); JUNK = P.buf("junk")
    dots = A.alloc("dots", [128, 128], F32); DOTS = P.bufs("dots", 128)
    wvg = A.alloc("wvg", [128, 128], F32); WVG = P.bufs("wvg", 128 // GE)
    wv_t = A.alloc("wv_t", [128, 128], F32); WVT = P.bufs("wv_t", 128 // GE)
    NDG = 8
    dg = [A.alloc(f"dg{j}", [128, 128], BF16) for j in range(NDG)]; DG = P.bufs("dg", NDG)
    gi = 0
    di = 0
    fuse = stage != "ffn"
    if fuse:
        assert A.offs["wsl1"] == A.offs["wsl0"] + 2048 and A.offs["wsl2"] == A.offs["wsl0"] + 4096
        wg = A.alloc("wg", [128, 8, D], BF16); WG = P.buf("wg")
        P.dma("pool", lambda e: e.dma_start(out=wg[:], in_=w_gate_d.rearrange("(k p) c -> p k c", p=128)),
              WG, writes=[WG])
        wpp = A.alloc_at("wpp", [128, 2, D], BF16, A.offs["wsl0"]); WPP = P.buf("wpp")
        P.dma("pool", lambda e: e.dma_start(out=wpp[:], in_=w_proj_d.rearrange("(k p) c -> p k c", p=128)),
              WPP, writes=[WPP])
        uTt = A.alloc_at("uTt", [128, 8, 128], BF16, A.offs["wsl2"]); UTT = P.buf("uTt")
        xoff = base_off + NS * 4096
        gpl = A.alloc_at("gpl", [128, D], F32, xoff); GPL = P.buf("gpl")
        gfn = A.alloc_at("gfn", [128, D], F32, xoff + 4096); GFN = P.buf("gfn")
        P.dma("sp", lambda e: e.dma_start(out=gpl[:], in_=bcast_rows(g_ple_d, D)), GPL, writes=[GPL])
        P.dma("sp", lambda e: e.dma_start(out=gfn[:], in_=bcast_rows(g_final_d, D)), GFN, writes=[GFN])
        xs_p = A.alloc_at("xs_p", [128, D], BF16, xoff + 8192); XSP = P.buf("xs_p")
        sq_p = A.alloc_at("sq_p", [128, D], BF16, xoff + 10240); SQP = P.buf("sq_p")
        ss_p = A.alloc("ss_p", [128, 4], F32); SSP = P.buf("ss_p")
        p32 = A.alloc("p32", [128, 256], F32); P32 = P.buf("p32")
        pb16 = A.alloc("pb16", [128, 256], BF16); PB16 = P.buf("pb16")
        pTt_ = A.alloc("pTt_", [128, 2, 128], BF16); PTT_ = P.buf("pTt_")
        sg_ = [A.alloc_at(f"sg_{j}", [128, 512], F32, xoff + 12288 + j * 2048) for j in range(2)]
        SG_ = P.bufs("sg_", 2)
        ple_banks = [2, 3, 6, 7]
        ple_rr = [0]

        def ple_psum():
            b_ = ple_banks[ple_rr[0] % 4]
            ple_rr[0] += 1
            return psum(b_)

        def ple_gen(i):
            X = xt[:, i, :]
            P.dma("sp", lambda e: e.dma_start(out=p32[:], in_=p_d[i * 128:(i + 1) * 128, :]), P32, writes=[P32])
            P.op("act", lambda e: e.activation(out=sq_p[:], in_=X, func=AF.Square, accum_out=ss_p[:, 0:1]),
                 reads=[XT[i]], writes=[SQP, SSP])
            yield
            P.op("act", lambda e: e.activation(out=ss_p[:, 1:2], in_=ss_p[:, 0:1], func=AF.Sqrt,
                                               scale=1.0 / D, bias=EPS), reads=[SSP], writes=[SSP])
            P.op("act", lambda e: e.copy(out=pb16[:], in_=p32[:]), reads=[P32], writes=[PB16])
            yield
            P.op("dve", lambda e: e.reciprocal(out=ss_p[:, 1:2], in_=ss_p[:, 1:2]), reads=[SSP], writes=[SSP])
            yield
            P.op("dve", lambda e: e.scalar_tensor_tensor(out=xs_p[:], in0=X, scalar=ss_p[:, 1:2], in1=gpl[:],
                                                         op0=ALU.mult, op1=ALU.mult),
                 reads=[XT[i], SSP, GPL], writes=[XSP])
            yield
            ps, PS = ple_psum()
            psb = ps[:].bitcast(BF16)
            for k in range(8):
                P.op("pe", lambda e, k=k, psb=psb: e.transpose(out=psb[:, k * 128:(k + 1) * 128],
                                                               in_=xs_p[:, k * 128:(k + 1) * 128],
                                                               identity=identb[:]), reads=[XSP, IDB], writes=[PS])
            ps_p, PS_P = ple_psum()
            psbp = ps_p[:].bitcast(BF16)
            for kk in range(2):
                P.op("pe", lambda e, kk=kk, psbp=psbp: e.transpose(out=psbp[:, kk * 128:(kk + 1) * 128],
                                                                   in_=pb16[:, kk * 128:(kk + 1) * 128],
                                                                   identity=identb[:]), reads=[PB16, IDB], writes=[PS_P])
            yield
            P.op("act", lambda e, psb=psb: e.copy(out=uTt[:], in_=psb.rearrange("p (k t) -> p k t", k=8)),
                 reads=[PS], writes=[UTT])
            P.op("act", lambda e, psbp=psbp: e.copy(out=pTt_[:], in_=psbp[:, 0:256].rearrange("p (k t) -> p k t", k=2)),
                 reads=[PS_P], writes=[PTT_])
            yield
            for hf in range(2):
                ps1, PS1 = ple_psum()
                for k in range(8):
                    P.op("pe", lambda e, ps1=ps1, k=k, hf=hf: e.matmul(
                        ps1[:], lhsT=uTt[:, k, :], rhs=wg[:, k, hf * 512:(hf + 1) * 512],
                        start=(k == 0), stop=(k == 7)), reads=[WG, UTT], writes=[PS1])
                ps2, PS2 = ple_psum()
                for kk in range(2):
                    P.op("pe", lambda e, ps2=ps2, kk=kk, hf=hf: e.matmul(
                        ps2[:], lhsT=pTt_[:, kk, :], rhs=wpp[:, kk, hf * 512:(hf + 1) * 512],
                        start=(kk == 0), stop=(kk == 1)), reads=[WPP, PTT_], writes=[PS2])
                yield
                P.op("act", lambda e, ps1=ps1, hf=hf: e.activation(out=sg_[hf][:], in_=ps1[:], func=AF.Sigmoid),
                     reads=[PS1], writes=[SG_[hf]])
                yield
                P.op("dve", lambda e, ps2=ps2, hf=hf: e.tensor_tensor(out=sg_[hf][:], in0=sg_[hf][:], in1=ps2[:],
                                                                      op=ALU.mult), reads=[PS2, SG_[hf]], writes=[SG_[hf]])
                yield
                P.op("dve", lambda e, hf=hf: e.tensor_tensor(
                    out=xt[:, i, hf * 512:(hf + 1) * 512], in0=xt[:, i, hf * 512:(hf + 1) * 512],
                    in1=sg_[hf][:], op=ALU.add), reads=[SG_[hf], XT[i]], writes=[XT[i]])
                yield
            P.op("act", lambda e: e.activation(out=sq_p[:], in_=X, func=AF.Square, accum_out=ss_p[:, 2:3]),
                 reads=[XT[i]], writes=[SQP, SSP])
            yield
            P.op("act", lambda e: e.activation(out=ss_p[:, 3:4], in_=ss_p[:, 2:3], func=AF.Sqrt,
                                               scale=1.0 / D, bias=EPS), reads=[SSP], writes=[SSP])
            yield
            P.op("dve", lambda e: e.reciprocal(out=ss_p[:, 3:4], in_=ss_p[:, 3:4]), reads=[SSP], writes=[SSP])
            yield
            P.op("dve", lambda e: e.scalar_tensor_tensor(out=X, in0=X, scalar=ss_p[:, 3:4], in1=gfn[:],
                                                         op0=ALU.mult, op1=ALU.mult),
                 reads=[XT[i], SSP, GFN], writes=[XT[i]])
            P.dma("sp", lambda e: e.dma_start(out=out_d[i * 128:(i + 1) * 128, :], in_=X), XT[i], reads=[XT[i]])
            yield

    pend = None
    for i in range(NT):
        P.op("dve", lambda e, i=i: e.scalar_tensor_tensor(
            out=xn32[:], in0=xt[:, i, :], scalar=rstd_all[:, i:i + 1], in1=gB[:],
            op0=ALU.mult, op1=ALU.mult), reads=[XT[i], RSTD, GB], writes=[XN])
        bank = 4 * (i % 2)
        psy = [psum(bank), psum(bank + 1)]
        for g in range(128 // GE):
            if pend is not None:
                next(pend, None)
            sls = []
            for u in range(GE):
                j = g * GE + u
                sl = gi % NS
                gi += 1
                sls.append(sl)
                P.dma("pool", lambda e, sl=sl, i=i, j=j: e.indirect_dma_start(
                    out=gs[sl][:], out_offset=None, in_=uv_d,
                    in_offset=bass.IndirectOffsetOnAxis(ap=idx_all[:, i, j:j + 1], axis=0)),
                    GS[sl], reads=[IDX[i]], writes=[GS[sl]])
            for u in range(GE):
                j = g * GE + u
                sl = sls[u]
                P.op("dve", lambda e, sl=sl, j=j: e.scalar_tensor_tensor(
                    out=junk[:], in0=gs[sl][:, 0:D], scalar=1.0, in1=xn32[:], op0=ALU.mult, op1=ALU.mult,
                    accum_out=dots[:, j:j + 1]), reads=[GS[sl], XN], writes=[JUNK, DOTS[j]])
            c0, c1 = g * GE, (g + 1) * GE
            P.op("act", lambda e, c0=c0, c1=c1: e.activation(out=wvg[:, c0:c1], in_=dots[:, c0:c1],
                                                            func=AF.Gelu_apprx_tanh),
                 reads=DOTS[c0:c1], writes=[WVG[g]])
            P.op("dve", lambda e, i=i, c0=c0, c1=c1: e.tensor_tensor(
                out=wv_t[:, c0:c1], in0=wvg[:, c0:c1], in1=gw_all[:, i, c0:c1], op=ALU.mult),
                reads=[WVG[g], IDX[i]], writes=[WVT[g]])
            for u in range(GE):
                j = g * GE + u
                sl = sls[u]
                dsl = di % NDG
                di += 1
                P.op("act", lambda e, dsl=dsl, j=j: e.activation(
                    out=dg[dsl][:], in_=identb[:], func=AF.Copy, scale=wv_t[:, j:j + 1]),
                    reads=[IDB, WVT[g]], writes=[DG[dsl]])
                for hf in range(2):
                    ps, PS = psy[hf]
                    P.op("pe", lambda e, ps=ps, dsl=dsl, sl=sl, hf=hf, j=j: e.matmul(
                        ps[:], lhsT=dg[dsl][:], rhs=gs[sl][:, D + hf * 512:D + (hf + 1) * 512],
                        start=(j == 0), stop=(j == 127)), reads=[DG[dsl], GS[sl]], writes=[PS])
        for hf in range(2):
            ps, PS = psy[hf]
            P.op("dve", lambda e, ps=ps, i=i, hf=hf: e.tensor_tensor(
                out=xt[:, i, hf * 512:(hf + 1) * 512], in0=xt[:, i, hf * 512:(hf + 1) * 512],
                in1=ps[:], op=ALU.add), reads=[PS, XT[i]], writes=[XT[i]])
        if fuse:
            if pend is not None:
                for _ in pend:
                    pass
            pend = ple_gen(i)
    if fuse:
        for _ in pend:
            pass
        return finalize(XT)
    P.barrier()
    A.release(m_phase)
    if stage == "ffn":
        for i in range(NT):
            P.dma("sp", lambda e, i=i: e.dma_start(out=out_d[i * 128:(i + 1) * 128, :], in_=xt[:, i, :]),
                  XT[i], reads=[XT[i]])
        return finalize(XT)

    m_phase = A.mark()
    norm_transpose(xt_src, g_ple_d, uT, UT)
    wg = A.alloc("wg", [128, 8, D], BF16); WG = P.buf("wg")
    P.dma("pool", lambda e: e.dma_start(out=wg[:], in_=w_gate_d.rearrange("(k p) c -> p k c", p=128)),
          WG, writes=[WG])
    wpp = A.alloc("wpp", [128, 2, D], BF16); WPP = P.buf("wpp")
    P.dma("pool", lambda e: e.dma_start(out=wpp[:], in_=w_proj_d.rearrange("(k p) c -> p k c", p=128)),
          WPP, writes=[WPP])
    pb = [A.alloc(f"pb{j}", [128, 256], BF16) for j in range(2)]; PB = P.bufs("pb", 2)
    pTt = [A.alloc(f"pTt{j}", [128, 2, 128], BF16) for j in range(2)]; PTT = P.bufs("pTt", 2)
    sg = [A.alloc(f"sg{j}", [128, 512], F32) for j in range(2)]; SG = P.bufs("sg", 2)
    it = 0
    for i in range(NT):
        j = i % 2
        P.dma("pool", lambda e, i=i, j=j: e.dma_start(out=pb[j][:], in_=p_d[i * 128:(i + 1) * 128, :]),
              PB[j], writes=[PB[j]])
        ps, PS = psum()
        psb = ps[:].bitcast(BF16)
        for kk in range(2):
            P.op("pe", lambda e, j=j, kk=kk, psb=psb: e.transpose(
                out=psb[:, kk * 128:(kk + 1) * 128], in_=pb[j][:, kk * 128:(kk + 1) * 128],
                identity=identb[:]), reads=[PB[j], IDB], writes=[PS])
        P.op("act", lambda e, j=j, psb=psb: e.copy(
            out=pTt[j][:], in_=psb[:, 0:256].rearrange("p (k t) -> p k t", k=2)), reads=[PS], writes=[PTT[j]])
        for hf in range(2):
            jj = it % 2
            it += 1
            ps1, PS1 = psum()
            for k in range(8):
                P.op("pe", lambda e, ps1=ps1, k=k, i=i, hf=hf: e.matmul(
                    ps1[:], lhsT=uT[:, k, i * 128:(i + 1) * 128], rhs=wg[:, k, hf * 512:(hf + 1) * 512],
                    start=(k == 0), stop=(k == 7)), reads=[WG, UT[i]], writes=[PS1])
            P.op("act", lambda e, ps1=ps1, jj=jj: e.activation(out=sg[jj][:], in_=ps1[:], func=AF.Sigmoid),
                 reads=[PS1], writes=[SG[jj]])
            ps2, PS2 = psum()
            for kk in range(2):
                P.op("pe", lambda e, ps2=ps2, kk=kk, j=j, hf=hf: e.matmul(
                    ps2[:], lhsT=pTt[j][:, kk, :], rhs=wpp[:, kk, hf * 512:(hf + 1) * 512],
                    start=(kk == 0), stop=(kk == 1)), reads=[WPP, PTT[j]], writes=[PS2])
            P.op("dve", lambda e, ps2=ps2, jj=jj: e.tensor_tensor(out=sg[jj][:], in0=sg[jj][:], in1=ps2[:], op=ALU.mult),
                 reads=[PS2, SG[jj]], writes=[SG[jj]])
            P.op("dve", lambda e, jj=jj, i=i, hf=hf: e.tensor_tensor(
                out=xt[:, i, hf * 512:(hf + 1) * 512], in0=xt[:, i, hf * 512:(hf + 1) * 512],
                in1=sg[jj][:], op=ALU.add), reads=[SG[jj], XT[i]], writes=[XT[i]])
    P.barrier()
    A.release(m_phase)

    P.dma("sp", lambda e: e.dma_start(out=gB[:], in_=bcast_rows(g_final_d, D)), GB, writes=[GB])
    sqf = A.alloc("fn_sq", [128, D], BF16); SQF = P.buf("fn_sq")
    ssf = A.alloc("fn_ss", [128, 2], F32); SSF = P.buf("fn_ss")
    for i in range(NT):
        P.op("act", lambda e, i=i: e.activation(out=sqf[:], in_=xt[:, i, :], func=AF.Square,
                                                accum_out=ssf[:, 0:1]), reads=[XT[i]], writes=[SQF, SSF])
        P.op("act", lambda e: e.activation(out=ssf[:, 1:2], in_=ssf[:, 0:1], func=AF.Sqrt,
                                           scale=1.0 / D, bias=EPS), reads=[SSF], writes=[SSF])
        P.op("dve", lambda e, i=i: e.reciprocal(out=rstd_all[:, i:i + 1], in_=ssf[:, 1:2]),
             reads=[SSF], writes=[RSTD])
        P.op("dve", lambda e, i=i: e.scalar_tensor_tensor(
            out=xt[:, i, :], in0=xt[:, i, :], scalar=rstd_all[:, i:i + 1], in1=gB[:],
            op0=ALU.mult, op1=ALU.mult), reads=[XT[i], RSTD, GB], writes=[XT[i]])
        P.dma("sp", lambda e, i=i: e.dma_start(out=out_d[i * 128:(i + 1) * 128, :], in_=xt[:, i, :]),
              XT[i], reads=[XT[i]])
    return finalize(XT)


def _t5_bucket(dist):
    max_exact = 16
    n = np.maximum(dist, 0)
    nf = np.maximum(n, 1).astype(np.float32)
    large = max_exact + (np.log(nf / max_exact) / np.float32(np.log(2048 / max_exact))
                         * (32 - max_exact)).astype(np.int32)
    large = np.minimum(large, 31)
    return np.where(n < max_exact, n, large)


def _bucket_table():
    import jax, jax.numpy as jnp
    return None


def prep_shared(inp):
    import ml_dtypes
    f32 = np.float32
    sh = {}
    sh["g_mix"] = np.ascontiguousarray(inp["g_mix"][0], f32)
    sh["w_in"] = np.ascontiguousarray(inp["w_in"][0], f32)
    sh["w_out"] = np.ascontiguousarray(inp["w_out"][0], f32)
    cv = np.zeros((128, 40), f32)
    cw = inp["conv_w"][0]
    for c in range(4):
        for j in range(4):
            cv[:, c * 4 + j] = cw[j, c * 128:(c + 1) * 128]
    for n, key in enumerate(["conv_b", "b_rg", "b_ig", "lru_lambda", "g_lru_out", "g_attn_out"]):
        cv[:, 16 + 4 * n:20 + 4 * n] = inp[key][0].reshape(4, 128).T
    sh["cv"] = cv
    for name, key in (("wrg_bd", "w_rg"), ("wig_bd", "w_ig")):
        w = inp[key][0]
        bd = np.zeros((128, 4, 128), f32)
        for g in range(8):
            c, l = g // 2, g % 2
            bd[l * 64:(l + 1) * 64, c, l * 64:(l + 1) * 64] = w[g]
        sh[name] = bd
    rb = inp["rel_bias"].astype(f32)
    k = np.arange(128)[:, None]
    q = np.arange(256)[None, :]
    dist = q - k
    valid = (dist >= 0) & (dist <= 128)
    bt = np.full((128, 4, 3, 2, 256), NEG, f32)
    for bi, d in enumerate((1, 4, 16)):
        bucket = _t5_bucket(dist * d)
        for h in range(8):
            vals = rb[bucket, h]
            bt[:, h // 2, bi, h % 2, :] = np.where(valid, vals, f32(NEG))
    sh["biasT"] = bt.reshape(128, 4, 3 * 2 * 256)
    sh["ident_bf"] = np.eye(128, dtype=f32).astype(ml_dtypes.bfloat16)
    sh["g_ffn"] = np.ascontiguousarray(inp["g_ffn"][0], f32)
    sh["peer_wq"] = np.ascontiguousarray(inp["peer_wq"][0], f32)
    sh["k1T"] = np.ascontiguousarray(inp["peer_k1"][0].T, f32)
    sh["k2T"] = np.ascontiguousarray(inp["peer_k2"][0].T, f32)
    sh["iota16"] = np.tile(np.arange(16, dtype=f32)[None], (128, 1))
    sh["peer_u"] = np.ascontiguousarray(inp["peer_u"][0], f32)
    sh["peer_v"] = np.ascontiguousarray(inp["peer_v"][0], f32)
    sh["g_ple"] = np.ascontiguousarray(inp["g_ple"][0], f32)
    sh["w_ple_gate"] = np.ascontiguousarray(inp["w_ple_gate"][0], f32)
    sh["w_ple_proj"] = np.ascontiguousarray(inp["w_ple_proj"][0], f32)
    sh["g_final"] = np.ascontiguousarray(inp["g_final"], f32)
    return sh


_NC_CACHE = {}


def kernel(**inputs):
    inp = {k: np.asarray(v) for k, v in inputs.items()}
    sh = prep_shared(inp)
    if "nc" not in _NC_CACHE:
        nc = bass.Bass("TRN2", target_bir_lowering=False)
        build(nc)
        _NC_CACHE["nc"] = nc
    nc = _NC_CACHE["nc"]
    in_maps = []
    for b in range(8):
        m = dict(sh)
        m["x"] = np.ascontiguousarray(inp["x"][b], np.float32)
        m["p"] = np.ascontiguousarray(inp["p"][0, b], np.float32)
        in_maps.append(m)
    res = run_bass_kernel_spmd(nc, in_maps, core_ids=list(range(8)))
    return np.stack([np.asarray(r["out"]) for r in res.results], axis=0).astype(np.float32)
```

```python
import numpy as np
import concourse.bass as bass
import concourse.mybir as mybir
from concourse.bass_utils import run_bass_kernel_spmd

F32 = mybir.dt.float32
BF16 = mybir.dt.bfloat16
U32 = mybir.dt.uint32
I32 = mybir.dt.int32
AF = mybir.ActivationFunctionType
ALU = mybir.AluOpType
AX = mybir.AxisListType

T = 2048
D = 1024
NT = 16
EPS = 1e-6
NEG = -30000.0


class Buf:
    __slots__ = ("name", "lastw", "reads", "dsem", "dcnt")

    def __init__(self, name):
        self.name = name
        self.lastw = None
        self.reads = {}
        self.dsem = None
        self.dcnt = 0


class Prog:
    ENG = ("pe", "dve", "act", "pool", "sp")

    def __init__(self, nc):
        self.nc = nc
        self.q = {e: [] for e in self.ENG}
        self.sem = {e: nc.alloc_semaphore(f"prog_{e}") for e in self.ENG}
        self.cnt = {e: 0 for e in self.ENG}
        self.waited = {e: {} for e in self.ENG}
        self.nbuf = 0
        self.dsems = []
        self.free_dsems = []

    def buf(self, name=None):
        self.nbuf += 1
        return Buf(name or f"b{self.nbuf}")

    def bufs(self, name, n):
        return [self.buf(f"{name}{i}") for i in range(n)]

    def _tokens(self, eng, reads, writes):
        need = {}

        def add(tok):
            if tok is None:
                return
            s, v = tok
            if need.get(id(s), (None, 0))[1] < v:
                need[id(s)] = (s, v)

        for b in reads:
            add(b.lastw)
        for b in writes:
            add(b.lastw)
            for tok in b.reads.values():
                add(tok)
        own = self.sem[eng]
        waits = []
        w = self.waited[eng]
        for s, v in need.values():
            if eng == "pe" and s is own:
                continue
            if w.get(id(s), 0) < v:
                w[id(s)] = v
                waits.append((s, v))
        return waits

    def _commit(self, tok, reads, writes):
        s, v = tok
        for b in reads:
            if b.reads.get(id(s), (None, 0))[1] < v:
                b.reads[id(s)] = (s, v)
        for b in writes:
            b.lastw = tok
            b.reads = {}

    def op(self, eng, fn, reads=(), writes=()):
        waits = self._tokens(eng, reads, writes)
        self.cnt[eng] += 1
        tok = (self.sem[eng], self.cnt[eng])
        self.q[eng].append((waits, fn, tok[0], 1))
        self._commit(tok, reads, writes)
        return tok

    def dma(self, eng, fn, sb, reads=(), writes=()):
        waits = self._tokens(eng, reads, writes)
        if sb.dsem is None:
            sb.dsem = self.nc.alloc_semaphore(f"dma_{sb.name}")
            self.dsems.append(sb)
        sb.dcnt += 16
        tok = (sb.dsem, sb.dcnt)
        self.q[eng].append((waits, fn, sb.dsem, 16))
        self._commit(tok, reads, writes)
        return tok

    def share_dsem(self, bufs):
        return bufs

    def barrier(self):
        toks = [(self.sem[e], self.cnt[e]) for e in self.ENG if self.cnt[e] > 0]
        toks += [(b.dsem, b.dcnt) for b in self.dsems]
        for e in self.ENG:
            waits = []
            w = self.waited[e]
            for s, v in toks:
                if e == "pe" and s is self.sem["pe"]:
                    continue
                if w.get(id(s), 0) < v:
                    w[id(s)] = v
                    waits.append((s, v))
            if waits:
                self.q[e].append((waits, None, None, 0))

    def emit(self):
        nc = self.nc
        with nc.Block() as block:
            @block.tensor
            def _(e):
                self._emit("pe", e)

            @block.vector
            def _(e):
                self._emit("dve", e)

            @block.scalar
            def _(e):
                self._emit("act", e)

            @block.gpsimd
            def _(e):
                self._emit("pool", e)

            @block.sync
            def _(e):
                self._emit("sp", e)

    def _emit(self, name, e):
        for waits, fn, sem, inc in self.q[name]:
            for s, v in waits:
                e.wait_ge(s, v)
            if fn is not None:
                ins = fn(e)
                ins.then_inc(sem, inc)


class Alloc:
    def __init__(self, nc):
        self.nc = nc
        self.lo = (int(nc.sbuf_base) + 63) // 64 * 64
        self.hi = int(nc.sbuf_top)
        self.cur = self.lo
        self.n = 0
        self.offs = {}

    def alloc(self, name, shape, dtype):
        size = int(np.prod(shape[1:])) * mybir.dt.size(dtype)
        off = (self.cur + 63) // 64 * 64
        assert off + size <= self.hi, f"SBUF overflow allocating {name}: {off}+{size} > {self.hi}"
        self.cur = off + size
        self.n += 1
        self.offs[name] = off
        return self.nc.alloc_sbuf_tensor_at(f"{name}_{self.n}", list(shape), dtype, offset=off)

    def alloc_at(self, name, shape, dtype, off):
        self.n += 1
        return self.nc.alloc_sbuf_tensor_at(f"{name}_{self.n}", list(shape), dtype, offset=off)

    def mark(self):
        return self.cur

    def release(self, m):
        self.cur = m


def bcast_rows(ap1d, n, parts=128):
    return bass.AP(ap1d.tensor, ap1d.offset, [[0, parts], [1, n]])


def peer_topk(P, A, sc, SC, iota16, IOTA, idx_out, gw_out, OUT):
    m = A.mark()
    m16 = A.alloc("tk_m16", [128, 16, 16], F32)
    M16A = P.bufs("tk_m16a", 16); M16B = P.bufs("tk_m16b", 16)
    ix16 = A.alloc("tk_ix16", [128, 16, 16], U32)
    IXA = P.bufs("tk_ixa", 16); IXB = P.bufs("tk_ixb", 16)
    cand = A.alloc("tk_cand", [128, 8, 256], F32); SCR = P.bufs("tk_scr", 16)
    scr = cand[:].rearrange("p h (s k) -> p (h s) k", s=2)
    for j in range(16):
        P.op("dve", lambda e, j=j: e.max(out=m16[:, j, 0:8], in_=sc[:, j, :]), reads=[SC], writes=[M16A[j]])
    for j in range(16):
        P.op("dve", lambda e, j=j: e.match_replace(out=scr[:, j, :], in_to_replace=m16[:, j, 0:8],
                                                   in_values=sc[:, j, :], imm_value=-1e30),
             reads=[SC, M16A[j]], writes=[SCR[j]])
    for j in range(16):
        P.op("dve", lambda e, j=j: e.max(out=m16[:, j, 8:16], in_=scr[:, j, :]), reads=[SCR[j]], writes=[M16B[j]])
    for j in range(16):
        P.op("dve", lambda e, j=j: e.max_index(out=ix16[:, j, 0:8], in_max=m16[:, j, 0:8], in_values=sc[:, j, :]),
             reads=[SC, M16A[j]], writes=[IXA[j]])
    for j in range(16):
        P.op("dve", lambda e, j=j: e.max_index(out=ix16[:, j, 8:16], in_max=m16[:, j, 8:16], in_values=sc[:, j, :]),
             reads=[SC, M16B[j]], writes=[IXB[j]])
    M16 = M16A + M16B
    IX16 = IXA + IXB
    pm = list(m16[:].ap[0])
    t1 = bass.AP(m16[:].tensor, m16[:].offset, [pm, [32, 8], [1, 16], [0, 16]])
    t2 = bass.AP(m16[:].tensor, m16[:].offset + 16, [pm, [32, 8], [0, 16], [1, 16]])
    P.op("dve", lambda e: e.tensor_tensor(out=cand[:].rearrange("p h (a b) -> p h a b", a=16), in0=t1, in1=t2,
                                          op=ALU.add), reads=M16, writes=SCR)
    c16 = A.alloc("tk_c16", [128, 8, 16], F32); C16A = P.bufs("tk_c16a", 8); C16B = P.bufs("tk_c16b", 8)
    pos = A.alloc("tk_pos", [128, 8, 16], U32); POSA = P.bufs("tk_posa", 8); POSB = P.bufs("tk_posb", 8)
    eq = A.alloc("tk_eq", [128, 8, 256], F32); EQS = P.bufs("tk_eq", 8)
    for h in range(8):
        P.op("dve", lambda e, h=h: e.max(out=c16[:, h, 0:8], in_=cand[:, h, :]),
             reads=SCR[2 * h:2 * h + 2], writes=[C16A[h]])
    for h in range(8):
        P.op("dve", lambda e, h=h: e.match_replace(out=eq[:, h, :], in_to_replace=c16[:, h, 0:8],
                                                   in_values=cand[:, h, :], imm_value=-1e30),
             reads=SCR[2 * h:2 * h + 2] + [C16A[h]], writes=[EQS[h]])
    for h in range(8):
        P.op("dve", lambda e, h=h: e.max(out=c16[:, h, 8:16], in_=eq[:, h, :]), reads=[EQS[h]], writes=[C16B[h]])
    for h in range(8):
        P.op("dve", lambda e, h=h: e.max_index(out=pos[:, h, 0:8], in_max=c16[:, h, 0:8], in_values=cand[:, h, :]),
             reads=SCR[2 * h:2 * h + 2] + [C16A[h]], writes=[POSA[h]])
    for h in range(8):
        P.op("dve", lambda e, h=h: e.max_index(out=pos[:, h, 8:16], in_max=c16[:, h, 8:16], in_values=cand[:, h, :]),
             reads=SCR[2 * h:2 * h + 2] + [C16B[h]], writes=[POSB[h]])
    C16 = P.buf("tk_c16all"); POS = P.buf("tk_posall"); EQ = P.buf("tk_eqall")
    for Bn, lst in ((C16, C16A + C16B), (POS, POSA + POSB), (EQ, EQS)):
        for b_ in lst:
            tok = b_.lastw
            if tok is not None and (Bn.lastw is None or True):
                s_, v_ = tok
                if Bn.reads.get(id(s_), (None, 0))[1] < v_:
                    Bn.reads[id(s_)] = (s_, v_)
            for tk in b_.reads.values():
                s_, v_ = tk
                if Bn.reads.get(id(s_), (None, 0))[1] < v_:
                    Bn.reads[id(s_)] = (s_, v_)
    eq4 = eq[:].rearrange("p h (a b) -> p h a b", a=16)
    au = A.alloc("tk_au", [128, 8, 16], U32); AU = P.buf("tk_au")
    bu = A.alloc("tk_bu", [128, 8, 16], U32); BU = P.buf("tk_bu")
    bf = A.alloc("tk_bf", [128, 8, 16], F32); BFb = P.buf("tk_bf")
    idf = A.alloc("tk_idf", [128, 2, 8, 16], F32); IDF = P.bufs("tk_idf", 2)
    ssum = A.alloc("tk_ssum", [128, 8], F32); SSUM = P.buf("tk_ssum")
    P.op("dve", lambda e: e.tensor_single_scalar(out=au[:], in_=pos[:], scalar=4, op=ALU.logical_shift_right),
         reads=[], writes=[AU, POS])
    P.op("dve", lambda e: e.tensor_single_scalar(out=bu[:], in_=pos[:], scalar=15, op=ALU.bitwise_and),
         reads=[POS], writes=[BU])
    cmax = bass.AP(c16[:].tensor, c16[:].offset, [list(c16[:].ap[0]), [16, 8], [0, 16]])
    P.op("dve", lambda e: e.tensor_tensor(out=bf[:], in0=c16[:], in1=cmax, op=ALU.subtract),
         reads=[], writes=[BFb, C16])
    P.op("act", lambda e: e.activation(out=bf[:], in_=bf[:], func=AF.Exp), reads=[BFb], writes=[BFb])
    io_ = bass.AP(iota16[:].tensor, iota16[:].offset, [list(iota16[:].ap[0]), [0, 8], [0, 16], [1, 16]])
    p0 = list(ix16[:].ap[0])
    gate_ops = [
        lambda: P.op("dve", lambda e: e.tensor_reduce(out=ssum[:], in_=bf[:], axis=AX.X, op=ALU.add),
                     reads=[BFb], writes=[SSUM]),
        lambda: P.op("dve", lambda e: e.reciprocal(out=ssum[:], in_=ssum[:]), reads=[SSUM], writes=[SSUM]),
        lambda: P.op("dve", lambda e: e.tensor_tensor(
            out=gw_out.rearrange("p (h k) -> p h k", h=8), in0=bf[:],
            in1=bass.AP(ssum[:].tensor, ssum[:].offset, [list(ssum[:].ap[0]), [1, 8], [0, 16]]), op=ALU.mult),
            reads=[BFb, SSUM], writes=[OUT]),
    ]
    for s_, (vu, VU) in enumerate(((au, AU), (bu, BU))):
        vb_ = bass.AP(vu[:].tensor, vu[:].offset, [list(vu[:].ap[0]), [16, 8], [1, 16], [0, 16]])
        P.op("dve", lambda e, vb_=vb_: e.tensor_tensor(out=eq4, in0=vb_, in1=io_, op=ALU.is_equal),
             reads=[VU, IOTA], writes=[EQ])
        if gate_ops:
            gate_ops.pop(0)()
        tab = bass.AP(ix16[:].tensor, ix16[:].offset + 16 * s_, [p0, [32, 8], [0, 16], [1, 16]])
        P.op("dve", lambda e, tab=tab: e.tensor_tensor(out=eq4, in0=eq4, in1=tab, op=ALU.mult),
             reads=[EQ] + IX16, writes=[EQ])
        if gate_ops:
            gate_ops.pop(0)()
        P.op("dve", lambda e, s_=s_: e.tensor_reduce(out=idf[:, s_, :, :], in_=eq4, axis=AX.X, op=ALU.add),
             reads=[EQ], writes=[IDF[s_]])
    while gate_ops:
        gate_ops.pop(0)()
    P.op("dve", lambda e: e.scalar_tensor_tensor(out=idf[:, 0, :, :], in0=idf[:, 0, :, :], scalar=128.0,
                                                 in1=idf[:, 1, :, :], op0=ALU.mult, op1=ALU.add),
         reads=IDF, writes=[IDF[0]])
    P.op("dve", lambda e: e.tensor_copy(out=idx_out, in_=idf[:, 0, :, :].rearrange("p h k -> p (h k)")),
         reads=[IDF[0]], writes=[OUT])
    A.release(m)


def build(nc, stage="full", dumps=()):
    P = Prog(nc)
    A = Alloc(nc)
    dumps = set(dumps)
    dump_list = []

    def din(name, shape, dt=F32):
        return nc.dram_tensor(name, list(shape), dt, kind="ExternalInput").ap()

    x_d = din("x", [T, D])
    p_d = din("p", [T, 256])
    g_mix_d = din("g_mix", [D])
    w_in_d = din("w_in", [D, 2560])
    cv_d = din("cv", [128, 40])
    wrg_d = din("wrg_bd", [128, 4, 128])
    wig_d = din("wig_bd", [128, 4, 128])
    biasT_d = din("biasT", [128, 4, 3 * 2 * 256])
    w_out_d = din("w_out", [D, D])
    identb_d = din("ident_bf", [128, 128], BF16)
    g_ffn_d = din("g_ffn", [D])
    peer_wq_d = din("peer_wq", [D, 2048])
    k1T_d = din("k1T", [128, 128])
    k2T_d = din("k2T", [128, 128])
    iota_d = din("iota16", [128, 16])
    peer_u_d = din("peer_u", [16384, D])
    peer_v_d = din("peer_v", [16384, D])
    g_ple_d = din("g_ple", [D])
    w_gate_d = din("w_ple_gate", [D, D])
    w_proj_d = din("w_ple_proj", [256, D])
    g_final_d = din("g_final", [D])
    out_d = nc.dram_tensor("out", [T, D], F32, kind="ExternalOutput").ap()

    def dump(name, ap, bufs, shape, dt=F32):
        if name not in dumps:
            return
        d = nc.dram_tensor("dbg_" + name, list(shape), dt, kind="ExternalOutput").ap()
        DB = P.buf("dbg_" + name)
        P.dma("sp", lambda e: e.dma_start(out=d, in_=ap), DB, reads=list(bufs))
        dump_list.append(DB)

    psall = nc.alloc_psum_tensor("psall", [128, 8 * 512], F32)
    ps_t = [psall[:, i * 512:(i + 1) * 512] for i in range(8)]
    ps_b = [P.buf(f"psb{i}") for i in range(8)]
    ps_rr = [0]

    def psum(idx=None):
        if idx is None:
            idx = ps_rr[0]
            ps_rr[0] = (idx + 1) % 8
        return ps_t[idx], ps_b[idx]

    def psum_pair():
        idx = (ps_rr[0] + 1) // 2 * 2 % 8
        ps_rr[0] = (idx + 2) % 8
        return psall[:, idx * 512:(idx + 2) * 512].rearrange("p (a q) -> p a q", a=2), [ps_b[idx], ps_b[idx + 1]]

    identb = A.alloc("identb", [128, 128], BF16); IDB = P.buf("identb")
    P.dma("sp", lambda e: e.dma_start(out=identb[:], in_=identb_d), IDB, writes=[IDB])
    ones_f = A.alloc("ones_f", [128, 128], F32); ONF = P.buf("ones_f")
    P.op("pool", lambda e: e.memset(ones_f[:], 1.0), writes=[ONF])
    ones_b = A.alloc("ones_b", [128, 64], BF16); ONB = P.buf("ones_b")
    P.op("pool", lambda e: e.memset(ones_b[:], 1.0), writes=[ONB])
    cv = A.alloc("cv", [128, 40], F32); CV = P.buf("cv")
    P.dma("sp", lambda e: e.dma_start(out=cv[:], in_=cv_d), CV, writes=[CV])
    gB = A.alloc("gB", [128, D], F32); GB = P.buf("gB")
    rstd_all = A.alloc("rstd_all", [128, NT], F32); RSTD = P.buf("rstd_all")

    uT = A.alloc("uT", [128, 8, T], BF16); UT = P.bufs("uT", NT)
    yT = A.alloc("yT", [128, 8, T], BF16); YT = P.bufs("yT", 8)

    uv_d = nc.dram_tensor("uv16", [16384, 2 * D], BF16, kind="Internal").ap()
    RPP = 2
    pst = [A.alloc(f"pst{j}", [128, RPP, D], F32) for j in range(2)]; PST = P.bufs("pst", 2)
    psb_ = [A.alloc(f"ppb{j}", [128, RPP, D], BF16) for j in range(2)]; PSB = P.bufs("ppb", 2)
    UVD = P.buf("uv16")

    def prepass_gen():
        steps = []
        for t_, c0 in ((peer_u_d, 0), (peer_v_d, D)):
            for blk in range(16384 // (128 * RPP)):
                r0 = blk * 128 * RPP
                src = t_[r0:r0 + 128 * RPP, :].rearrange("(p r) d -> p r d", r=RPP)
                dst = uv_d[r0:r0 + 128 * RPP, c0:c0 + D].rearrange("(p r) d -> p r d", r=RPP)
                steps.append((src, dst))

        def load(n):
            j = n % 2
            src = steps[n][0]
            P.dma("sp", lambda e, j=j, src=src: e.dma_start(out=pst[j][:], in_=src), PST[j], writes=[PST[j]])

        load(0)
        for n in range(len(steps)):
            j = n % 2
            dst = steps[n][1]
            if n % 2 == 0:
                P.op("pool", lambda e, j=j: e.tensor_copy(out=psb_[j][:], in_=pst[j][:]),
                     reads=[PST[j]], writes=[PSB[j]])
            else:
                P.op("act", lambda e, j=j: e.copy(out=psb_[j][:], in_=pst[j][:]),
                     reads=[PST[j]], writes=[PSB[j]])
            if n + 1 < len(steps):
                load(n + 1)
            P.dma("sp", lambda e, j=j, dst=dst: e.dma_start(out=dst, in_=psb_[j][:]), PSB[j],
                  reads=[PSB[j]], writes=[UVD])
            yield

    pp = prepass_gen()

    def pp_adv(n):
        for _ in range(n):
            next(pp, None)

    def norm_transpose(src_tiles, gvec_d, dstT, DST, hook=None):
        m = A.mark()
        P.dma("sp", lambda e: e.dma_start(out=gB[:], in_=bcast_rows(gvec_d, D)), GB, writes=[GB])
        sq = A.alloc("nt_sq", [128, D], BF16); SQ = P.buf("nt_sq")
        ss = A.alloc("nt_ss", [128, 2 * NT], F32); SSL = P.bufs("nt_ss", NT)
        xs = [A.alloc(f"nt_xs{j}", [128, D], BF16) for j in range(2)]
        XS = P.bufs("nt_xs", 2)
        for i in range(NT):
            if hook is not None:
                hook()
            src, SRC = src_tiles(i)
            SS = SSL[i]
            P.op("act", lambda e, src=src, i=i: e.activation(out=sq[:], in_=src, func=AF.Square,
                                                              accum_out=ss[:, 2 * i:2 * i + 1]),
                 reads=SRC, writes=[SQ, SS])
            P.op("act", lambda e, i=i: e.activation(out=ss[:, 2 * i + 1:2 * i + 2], in_=ss[:, 2 * i:2 * i + 1],
                                                    func=AF.Sqrt, scale=1.0 / D, bias=EPS),
                 reads=[SS], writes=[SS])
            P.op("dve", lambda e, i=i: e.reciprocal(out=rstd_all[:, i:i + 1], in_=ss[:, 2 * i + 1:2 * i + 2]),
                 reads=[SS], writes=[RSTD])
            j = i % 2
            P.op("dve", lambda e, src=src, i=i, j=j: e.scalar_tensor_tensor(
                out=xs[j][:], in0=src, scalar=rstd_all[:, i:i + 1], in1=gB[:],
                op0=ALU.mult, op1=ALU.mult), reads=SRC + [RSTD, GB], writes=[XS[j]])
            ps, PS = psum()
            psb = ps[:].bitcast(BF16)
            for k in range(8):
                P.op("pe", lambda e, j=j, k=k, psb=psb: e.transpose(
                    out=psb[:, k * 128:(k + 1) * 128], in_=xs[j][:, k * 128:(k + 1) * 128],
                    identity=identb[:]), reads=[XS[j], IDB], writes=[PS])
            P.op("act", lambda e, i=i, psb=psb: e.copy(
                out=dstT[:, :, i * 128:(i + 1) * 128],
                in_=psb.rearrange("p (k t) -> p k t", k=8)), reads=[PS], writes=[DST[i]])
        P.barrier()
        A.release(m)

    wsl = [A.alloc(f"wsl{j}", [128, 8, 128], BF16) for j in range(3)]
    WSL = P.bufs("wsl", 3)
    wsl_rr = [0]

    def load_wchunk(w_d, col0):
        j = wsl_rr[0]
        wsl_rr[0] = (j + 1) % 3
        src = w_d.rearrange("(k p) c -> p k c", p=128)[:, :, col0:col0 + 128]
        P.dma("pool", lambda e: e.dma_start(out=wsl[j][:], in_=src), WSL[j], writes=[WSL[j]])
        return wsl[j], WSL[j]

    def proj_chunk(w_d, col0, srcT, SRC, evac):
        w, W = load_wchunk(w_d, col0)
        for tq in range(4):
            ps, PS = psum()
            for k in range(8):
                P.op("pe", lambda e, ps=ps, k=k, tq=tq, w=w: e.matmul(
                    ps[:], lhsT=w[:, k, :], rhs=srcT[:, k, tq * 512:(tq + 1) * 512],
                    start=(k == 0), stop=(k == 7)),
                    reads=[W] + SRC[tq * 4:(tq + 1) * 4], writes=[PS])
            evac(tq, ps, PS)

    def finalize(out_bufs):
        final = []
        w = P.waited["sp"]
        for b in list(out_bufs) + dump_list:
            if b.dsem is not None and w.get(id(b.dsem), 0) < b.dcnt:
                w[id(b.dsem)] = b.dcnt
                final.append((b.dsem, b.dcnt))
        P.q["sp"].append((final, None, None, 0))
        P.emit()
        return nc

    m_phase = A.mark()
    xin = [A.alloc(f"xin{j}", [128, D], F32) for j in range(2)]
    XIN = P.bufs("xin", 2)

    def x_src(i):
        j = i % 2
        P.dma("sp", lambda e: e.dma_start(out=xin[j][:], in_=x_d[i * 128:(i + 1) * 128, :]),
              XIN[j], writes=[XIN[j]])
        return xin[j][:], [XIN[j]]

    norm_transpose(x_src, g_mix_d, uT, UT, hook=lambda: pp_adv(1))
    dump("uT", uT[:], UT, [128, 8, T], BF16)
    P.barrier()
    A.release(m_phase)
    if stage == "p1":
        return finalize([])

    m_phase = A.mark()
    y32 = A.alloc("y32", [128, 4, T], F32); Y32 = P.bufs("y32", 4)
    wrg = A.alloc("wrg", [128, 4, 128], BF16); WRG = P.buf("wrg")
    wig = A.alloc("wig", [128, 4, 128], BF16); WIG = P.buf("wig")
    P.dma("pool", lambda e: e.dma_start(out=wrg[:], in_=wrg_d), WRG, writes=[WRG])
    P.dma("pool", lambda e: e.dma_start(out=wig[:], in_=wig_d), WIG, writes=[WIG])
    nsp = A.alloc("nsp", [128, 4], F32); NSP = P.buf("nsp")
    P.op("act", lambda e: e.activation(out=nsp[:], in_=cv[:, 28:32], func=AF.Exp, scale=-1.0),
         reads=[CV], writes=[NSP])
    P.op("act", lambda e: e.activation(out=nsp[:], in_=nsp[:], func=AF.Ln, scale=1.0, bias=1.0),
         reads=[NSP], writes=[NSP])
    P.op("dve", lambda e: e.tensor_scalar(out=nsp[:], in0=nsp[:], scalar1=-8.0, scalar2=None,
                                          op0=ALU.mult), reads=[NSP], writes=[NSP])

    def big(name, dt=F32):
        return A.alloc(name, [128, T], dt), P.buf(name)

    xl, XL = big("xl"); gt, GT = big("gt"); xc, XC = big("xc"); xcb, XCB = big("xcb", BF16)
    ra, RA = big("ra"); ig, IG = big("ig"); tmp, TMP = big("tmp")

    for c in range(4):
        def ev_xl(tq, ps, PS):
            P.op("act", lambda e, ps=ps, tq=tq: e.copy(out=xl[:, tq * 512:(tq + 1) * 512], in_=ps[:]),
                 reads=[PS], writes=[XL])

        def ev_gt(tq, ps, PS):
            P.op("act", lambda e, ps=ps, tq=tq: e.copy(out=gt[:, tq * 512:(tq + 1) * 512], in_=ps[:]),
                 reads=[PS], writes=[GT])

        proj_chunk(w_in_d, c * 128, uT, UT, ev_xl)
        pp_adv(2)
        proj_chunk(w_in_d, 512 + c * 128, uT, UT, ev_gt)
        pp_adv(2)
        P.op("dve", lambda e, c=c: e.tensor_scalar(
            out=xc[:], in0=xl[:], scalar1=cv[:, c * 4 + 3:c * 4 + 4], scalar2=cv[:, 16 + c:17 + c],
            op0=ALU.mult, op1=ALU.add), reads=[XL, CV], writes=[XC])
        for s in (1, 2, 3):
            P.op("dve", lambda e, c=c, s=s: e.scalar_tensor_tensor(
                out=xc[:, s:], in0=xl[:, 0:T - s], scalar=cv[:, c * 4 + 3 - s:c * 4 + 4 - s],
                in1=xc[:, s:], op0=ALU.mult, op1=ALU.add), reads=[XL, CV, XC], writes=[XC])
        P.op("act", lambda e: e.copy(out=xcb[:], in_=xc[:]), reads=[XC], writes=[XCB])
        for tq in range(4):
            ps, PS = psum()
            P.op("pe", lambda e, ps=ps, tq=tq, c=c: e.matmul(
                ps[:], lhsT=wrg[:, c, :], rhs=xcb[:, tq * 512:(tq + 1) * 512], start=True, stop=True),
                reads=[WRG, XCB], writes=[PS])
            P.op("act", lambda e, ps=ps, tq=tq, c=c: e.activation(
                out=ra[:, tq * 512:(tq + 1) * 512], in_=ps[:], func=AF.Sigmoid,
                bias=cv[:, 20 + c:21 + c], scale=1.0), reads=[PS, CV], writes=[RA])
        for tq in range(4):
            ps, PS = psum()
            P.op("pe", lambda e, ps=ps, tq=tq, c=c: e.matmul(
                ps[:], lhsT=wig[:, c, :], rhs=xcb[:, tq * 512:(tq + 1) * 512], start=True, stop=True),
                reads=[WIG, XCB], writes=[PS])
            P.op("act", lambda e, ps=ps, tq=tq, c=c: e.activation(
                out=ig[:, tq * 512:(tq + 1) * 512], in_=ps[:], func=AF.Sigmoid,
                bias=cv[:, 24 + c:25 + c], scale=1.0), reads=[PS, CV], writes=[IG])
        P.op("act", lambda e, c=c: e.activation(out=ra[:], in_=ra[:], func=AF.Exp,
                                                scale=nsp[:, c:c + 1]), reads=[RA, NSP], writes=[RA])
        P.op("dve", lambda e: e.tensor_tensor(out=tmp[:], in0=ra[:], in1=ra[:], op=ALU.mult),
             reads=[RA], writes=[TMP])
        P.op("act", lambda e: e.activation(out=tmp[:], in_=tmp[:], func=AF.Sqrt, scale=-1.0, bias=1.0),
             reads=[TMP], writes=[TMP])
        P.op("dve", lambda e: e.tensor_tensor(out=ig[:], in0=ig[:], in1=xc[:], op=ALU.mult),
             reads=[IG, XC], writes=[IG])
        P.op("dve", lambda e: e.tensor_tensor(out=ig[:], in0=ig[:], in1=tmp[:], op=ALU.mult),
             reads=[IG, TMP], writes=[IG])
        P.op("dve", lambda e: e.tensor_tensor_scan(out=tmp[:], data0=ra[:], data1=ig[:], initial=0.0,
                                                   op0=ALU.mult, op1=ALU.add),
             reads=[RA, IG], writes=[TMP])
        P.op("act", lambda e: e.activation(out=gt[:], in_=gt[:], func=AF.Gelu_apprx_tanh),
             reads=[GT], writes=[GT])
        P.op("dve", lambda e, c=c: e.tensor_tensor(out=y32[:, c, :], in0=tmp[:], in1=gt[:], op=ALU.mult),
             reads=[TMP, GT], writes=[Y32[c]])

    def group_rmsnorm(gcol0, ybase, y32, Y32, tmp, TMP, ra, RA):
        pss = [psum(i) for i in range(4)]
        for c in range(4):
            P.op("act", lambda e, c=c: e.activation(out=tmp[:], in_=y32[:, c, :], func=AF.Square),
                 reads=[Y32[c]], writes=[TMP])
            for tq in range(4):
                ps, PS = pss[tq]
                P.op("pe", lambda e, ps=ps, tq=tq, c=c: e.matmul(
                    ps[:], lhsT=ones_f[:], rhs=tmp[:, tq * 512:(tq + 1) * 512],
                    start=(c == 0), stop=(c == 3)), reads=[ONF, TMP], writes=[PS])
        for tq in range(4):
            ps, PS = pss[tq]
            P.op("act", lambda e, ps=ps, tq=tq: e.activation(
                out=ra[:, tq * 512:(tq + 1) * 512], in_=ps[:], func=AF.Sqrt, scale=1.0 / 512, bias=EPS),
                reads=[PS], writes=[RA])
        P.op("dve", lambda e: e.reciprocal(out=ra[:], in_=ra[:]), reads=[RA], writes=[RA])
        for c in range(4):
            P.op("dve", lambda e, c=c: e.scalar_tensor_tensor(
                out=yT[:, ybase + c, :], in0=y32[:, c, :], scalar=cv[:, gcol0 + c:gcol0 + c + 1],
                in1=ra[:], op0=ALU.mult, op1=ALU.mult), reads=[Y32[c], CV, RA], writes=[YT[ybase + c]])

    dump("y_lru", y32[:], Y32, [128, 4, T])
    if stage == "p2a":
        return finalize([])
    group_rmsnorm(32, 0, y32, Y32, tmp, TMP, ra, RA)
    if stage == "p2":
        dump("yT", yT[:], YT, [128, 8, T], BF16)
    P.barrier()
    A.release(m_phase)
    if stage == "p2":
        return finalize([])

    m_phase = A.mark()
    y3 = A.alloc("y32a", [128, 4, T], F32); Y3 = P.bufs("y32a", 4)
    ra3, RA3 = big("ra2"); tmp3, TMP3 = big("tmp2")
    qT, QT = big("qT", BF16); kT, KT = big("kT", BF16)
    dacc, DACC = big("dacc")
    bias_sb = A.alloc("bias_sb", [128, 3, 2, 256], F32); BIAS = P.buf("bias_sb")
    vbs = [A.alloc(f"vb{j}", [128, 16, 128], BF16) for j in range(2)]; VBS = P.bufs("vb", 2)
    tS = [A.alloc(f"tS{j}", [128, 2, 256], F32) for j in range(3)]; TS = P.bufs("tS", 3)
    pT = [A.alloc(f"pT{j}", [128, 2, 256], BF16) for j in range(3)]; PT = P.bufs("pT", 3)
    it = 0
    for c in range(4):
        OACC = Y3[c]
        P.dma("sp", lambda e, c=c: e.dma_start(
            out=bias_sb[:].rearrange("p a b q -> p (a b q)"), in_=biasT_d[:, c, :]), BIAS, writes=[BIAS])

        def ev_q(tq, ps, PS):
            P.op("act", lambda e, ps=ps, tq=tq: e.activation(
                out=qT[:, tq * 512:(tq + 1) * 512], in_=ps[:], func=AF.Copy, scale=0.125),
                reads=[PS], writes=[QT])

        def ev_k(tq, ps, PS):
            P.op("act", lambda e, ps=ps, tq=tq: e.copy(out=kT[:, tq * 512:(tq + 1) * 512], in_=ps[:]),
                 reads=[PS], writes=[KT])

        proj_chunk(w_in_d, 1024 + c * 128, uT, UT, ev_q)
        proj_chunk(w_in_d, 1536 + c * 128, uT, UT, ev_k)
        wv, WV = load_wchunk(w_in_d, 2048 + c * 128)
        P.op("pool", lambda e, c=c: e.memset(y3[:, c, :], 0.0), writes=[OACC])
        P.op("pool", lambda e: e.memset(dacc[:], 0.0), writes=[DACC])
        iters = []
        for bi, d in enumerate((1, 4, 16)):
            nb = 16 // d
            first = True
            for r in range(d):
                for m in range(nb):
                    iters.append((bi, d, nb, r, m, first))
                    first = False

        def stage_S(n):
            bi, d, nb, r, m, first = iters[n]
            vbb, VBB = vbs[bi % 2], VBS[bi % 2]
            if first:
                for g4 in range(4):
                    ps, PS = psum()
                    for u4 in range(4):
                        ti = g4 * 4 + u4
                        rr, mm = ti // nb, ti % nb
                        tt0 = rr + d * 128 * mm
                        for k in range(8):
                            P.op("pe", lambda e, ps=ps, u4=u4, k=k, tt0=tt0, d=d, wv=wv: e.matmul(
                                ps[:, u4 * 128:(u4 + 1) * 128],
                                lhsT=uT[:, k, tt0:tt0 + d * 127 + 1:d], rhs=wv[:, k, :],
                                start=(k == 0), stop=(k == 7)), reads=[WV] + UT, writes=[PS])
                    P.op("act", lambda e, ps=ps, g4=g4, vbb=vbb: e.copy(
                        out=vbb[:, g4 * 4:(g4 + 1) * 4, :], in_=ps[:].rearrange("p (a b) -> p a b", a=4)),
                        reads=[PS], writes=[VBB])
            nq = 256 if m + 1 < nb else 128
            t0 = r + d * 128 * m
            kend = t0 + d * 127 + 1
            qend = t0 + d * (nq - 1) + 1
            j = n % 3
            if n % 3 == 0:
                pp_adv(1)
            ps, PSP = psum_pair()
            for hh in range(2):
                p0 = hh * 64
                P.op("pe", lambda e, ps=ps, hh=hh, p0=p0, t0=t0, kend=kend, qend=qend, d=d, nq=nq: e.matmul(
                    ps[:, hh, 0:nq], lhsT=kT[p0:p0 + 64, t0:kend:d],
                    rhs=qT[p0:p0 + 64, t0:qend:d], start=True, stop=True),
                    reads=[KT, QT], writes=[PSP[hh]])
            P.op("dve", lambda e, ps=ps, j=j, bi=bi, nq=nq: e.tensor_tensor(
                out=tS[j][:, :, 0:nq], in0=ps[:, :, 0:nq],
                in1=bias_sb[:, bi, :, 0:nq], op=ALU.add), reads=PSP + [BIAS], writes=[TS[j]])
            P.op("act", lambda e, j=j, nq=nq: e.activation(
                out=pT[j][:, :, 0:nq], in_=tS[j][:, :, 0:nq], func=AF.Exp),
                reads=[TS[j]], writes=[PT[j]])

        def stage_PV(n):
            bi, d, nb, r, m, first = iters[n]
            vbb, VBB = vbs[bi % 2], VBS[bi % 2]
            nq = 256 if m + 1 < nb else 128
            t0 = r + d * 128 * m
            qend = t0 + d * (nq - 1) + 1
            j = n % 3
            ps2, PS2 = psum()
            ti = r * nb + m
            for hh in range(2):
                p0 = hh * 64
                P.op("pe", lambda e, ps2=ps2, hh=hh, p0=p0, ti=ti, j=j, nq=nq, vbb=vbb: e.matmul(
                    ps2[p0:p0 + 64, 0:nq], lhsT=vbb[:, ti, hh * 64:(hh + 1) * 64],
                    rhs=pT[j][:, hh, 0:nq], start=True, stop=True),
                    reads=[VBB, PT[j]], writes=[PS2])
                P.op("pe", lambda e, ps2=ps2, hh=hh, p0=p0, j=j, nq=nq: e.matmul(
                    ps2[p0:p0 + 64, 256:256 + nq], lhsT=ones_b[:, 0:64],
                    rhs=pT[j][:, hh, 0:nq], start=True, stop=True),
                    reads=[ONB, PT[j]], writes=[PS2])
            P.op("dve", lambda e, ps2=ps2, c=c, t0=t0, qend=qend, d=d, nq=nq: e.tensor_tensor(
                out=y3[:, c, t0:qend:d], in0=y3[:, c, t0:qend:d], in1=ps2[:, 0:nq], op=ALU.add),
                reads=[PS2, OACC], writes=[OACC])
            P.op("dve", lambda e, ps2=ps2, t0=t0, qend=qend, d=d, nq=nq: e.tensor_tensor(
                out=dacc[:, t0:qend:d], in0=dacc[:, t0:qend:d], in1=ps2[:, 256:256 + nq], op=ALU.add),
                reads=[PS2, DACC], writes=[DACC])

        for n in range(len(iters) + 1):
            if n < len(iters):
                stage_S(n)
            if n >= 1:
                stage_PV(n - 1)
        P.op("dve", lambda e: e.reciprocal(out=dacc[:], in_=dacc[:]), reads=[DACC], writes=[DACC])
        P.op("dve", lambda e, c=c: e.tensor_tensor(out=y3[:, c, :], in0=y3[:, c, :], in1=dacc[:], op=ALU.mult),
             reads=[OACC, DACC], writes=[OACC])
    dump("y_att", y3[:], Y3, [128, 4, T])
    if stage == "p3a":
        return finalize([])
    group_rmsnorm(36, 4, y3, Y3, tmp3, TMP3, ra3, RA3)
    if stage != "p2":
        dump("yT", yT[:], YT, [128, 8, T], BF16)
    P.barrier()
    A.release(m_phase)

    xt = A.alloc("xt", [128, NT, D], F32); XT = P.bufs("xt", NT)
    m_phase = A.mark()
    wo = A.alloc("wo", [128, 8, D], BF16); WO = P.buf("wo")
    P.dma("pool", lambda e: e.dma_start(out=wo[:], in_=w_out_d.rearrange("(k p) c -> p k c", p=128)),
          WO, writes=[WO])
    for i in range(NT):
        P.dma("sp", lambda e, i=i: e.dma_start(out=xt[:, i, :], in_=x_d[i * 128:(i + 1) * 128, :]),
              XT[i], writes=[XT[i]])
    for i in range(NT):
        for hf in range(2):
            pp_adv(1)
            ps, PS = psum()
            for k in range(8):
                P.op("pe", lambda e, ps=ps, k=k, i=i, hf=hf: e.matmul(
                    ps[:], lhsT=yT[:, k, i * 128:(i + 1) * 128], rhs=wo[:, k, hf * 512:(hf + 1) * 512],
                    start=(k == 0), stop=(k == 7)), reads=[WO] + YT, writes=[PS])
            P.op("dve", lambda e, ps=ps, i=i, hf=hf: e.tensor_tensor(
                out=xt[:, i, hf * 512:(hf + 1) * 512], in0=xt[:, i, hf * 512:(hf + 1) * 512],
                in1=ps[:], op=ALU.add), reads=[PS, XT[i]], writes=[XT[i]])
    for _ in pp:
        pass
    P.barrier()
    A.release(m_phase)

    if stage == "mixer":
        for i in range(NT):
            P.dma("sp", lambda e, i=i: e.dma_start(out=out_d[i * 128:(i + 1) * 128, :], in_=xt[:, i, :]),
                  XT[i], reads=[XT[i]])
        return finalize(XT)

    def xt_src(i):
        return xt[:, i, :], [XT[i]]

    m_phase = A.mark()
    norm_transpose(xt_src, g_ffn_d, uT, UT)
    wq = yT
    WQ = P.buf("wq")
    wq_src = peer_wq_d.rearrange("(k p) c -> p k c", p=128)
    for hf in range(2):
        P.dma("pool", lambda e, hf=hf: e.dma_start(out=wq[:, :, hf * 1024:(hf + 1) * 1024],
                                                   in_=wq_src[:, :, hf * 1024:(hf + 1) * 1024]),
              WQ, writes=[WQ])
    kTs = A.alloc("kTs", [128, 2, 128], F32); KTS = P.buf("kTs")
    P.dma("sp", lambda e: e.dma_start(out=kTs[:, 0, :], in_=k1T_d), KTS, writes=[KTS])
    P.dma("sp", lambda e: e.dma_start(out=kTs[:, 1, :], in_=k2T_d), KTS, writes=[KTS])
    iota16 = A.alloc("iota16", [128, 16], F32); IOTA = P.buf("iota16")
    P.dma("sp", lambda e: e.dma_start(out=iota16[:], in_=iota_d), IOTA, writes=[IOTA])
    idx_all = A.alloc("idx_all", [128, NT, 128], I32); IDX = P.bufs("idx_all", NT)
    gw_all = A.alloc("gw_all", [128, NT, 128], F32)
    m_sub = A.mark()
    assert A.offs["ppb1"] == A.offs["ppb0"] + RPP * D * 2
    qTt = A.alloc_at("qTt", [128, 16, 128], F32, A.offs["pst0"]); QTT = P.buf("qTt")
    scs = [A.alloc_at("sc0", [128, 16, 128], F32, A.offs["pst1"]),
           A.alloc_at("sc1", [128, 16, 128], F32, A.offs["ppb0"])]
    SCS = P.bufs("sc", 2)
    for i in range(NT):
        for jg in range(4):
            ps, PS = psum()
            for u in range(4):
                j = jg * 4 + u
                for k in range(8):
                    P.op("pe", lambda e, ps=ps, u=u, j=j, k=k, i=i: e.matmul(
                        ps[:, u * 128:(u + 1) * 128], lhsT=wq[:, k, j * 128:(j + 1) * 128],
                        rhs=uT[:, k, i * 128:(i + 1) * 128], start=(k == 0), stop=(k == 7)),
                        reads=[WQ, UT[i]], writes=[PS])
            P.op("act", lambda e, ps=ps, jg=jg: e.copy(
                out=qTt[:, jg * 4:(jg + 1) * 4, :], in_=ps[:].rearrange("p (a b) -> p a b", a=4)),
                reads=[PS], writes=[QTT])
        sc, SC = scs[i % 2], SCS[i % 2]
        for jg in range(4):
            ps, PS = psum()
            for u in range(4):
                j = jg * 4 + u
                P.op("pe", lambda e, ps=ps, u=u, j=j: e.matmul(
                    ps[:, u * 128:(u + 1) * 128], lhsT=qTt[:, j, :], rhs=kTs[:, j % 2, :],
                    start=True, stop=True), reads=[QTT, KTS], writes=[PS])
            P.op("act", lambda e, ps=ps, jg=jg, sc=sc: e.copy(
                out=sc[:, jg * 4:(jg + 1) * 4, :], in_=ps[:].rearrange("p (a b) -> p a b", a=4)),
                reads=[PS], writes=[SC])
        if i == 0:
            dump("qTt0", qTt[:], [QTT], [128, 16, 128])
            dump("sc0", sc[:], [SC], [128, 16, 128])
        peer_topk(P, A, sc, SC, iota16, IOTA, idx_all[:, i, :], gw_all[:, i, :], IDX[i])
    dump("idx_all", idx_all[:], IDX, [128, NT, 128], I32)
    dump("gw_all", gw_all[:], IDX, [128, NT, 128])
    P.barrier()
    A.release(m_sub)
    if stage == "topk":
        return finalize([])
    NS = 16
    GE = 4
    base_off = A.offs["uT"]
    assert A.offs["yT"] == base_off + 8 * T * 2
    gs = [A.alloc_at(f"gs{j}", [128, 2 * D], BF16, base_off + j * 4096) for j in range(NS)]
    GS = P.bufs("gs", NS)
    xn32 = A.alloc("xn32", [128, D], F32); XN = P.buf("xn32")
    junk = A.alloc("junk", [128, D], BF16); JUNK = P.buf("junk")
    dots = A.alloc("dots", [128, 128], F32); DOTS = P.bufs("dots", 128)
    wvg = A.alloc("wvg", [128, 128], F32); WVG = P.bufs("wvg", 128 // GE)
    wv_t = A.alloc("wv_t", [128, 128], F32); WVT = P.bufs("wv_t", 128 // GE)
    NDG = 8
    dg = [A.alloc(f"dg{j}", [128, 128], BF16) for j in range(NDG)]; DG = P.bufs("dg", NDG)
    gi = 0
    di = 0
    fuse = stage != "ffn"
    if fuse:
        assert A.offs["wsl1"] == A.offs["wsl0"] + 2048 and A.offs["wsl2"] == A.offs["wsl0"] + 4096
        wg = A.alloc("wg", [128, 8, D], BF16); WG = P.buf("wg")
        P.dma("pool", lambda e: e.dma_start(out=wg[:], in_=w_gate_d.rearrange("(k p) c -> p k c", p=128)),
              WG, writes=[WG])
        wpp = A.alloc_at("wpp", [128, 2, D], BF16, A.offs["wsl0"]); WPP = P.buf("wpp")
        P.dma("pool", lambda e: e.dma_start(out=wpp[:], in_=w_proj_d.rearrange("(k p) c -> p k c", p=128)),
              WPP, writes=[WPP])
        uTt = A.alloc_at("uTt", [128, 8, 128], BF16, A.offs["wsl2"]); UTT = P.buf("uTt")
        xo0, xo1 = A.offs["pst0"], A.offs["pst1"]
        gpl = A.alloc_at("gpl", [128, D], F32, xo0); GPL = P.buf("gpl")
        gfn = A.alloc_at("gfn", [128, D], F32, xo0 + 4096); GFN = P.buf("gfn")
        P.dma("sp", lambda e: e.dma_start(out=gpl[:], in_=bcast_rows(g_ple_d, D)), GPL, writes=[GPL])
        P.dma("sp", lambda e: e.dma_start(out=gfn[:], in_=bcast_rows(g_final_d, D)), GFN, writes=[GFN])
        xs_p = A.alloc_at("xs_p", [128, D], BF16, xo1); XSP = P.buf("xs_p")
        sq_p = A.alloc_at("sq_p", [128, D], BF16, xo1 + 2048); SQP = P.buf("sq_p")
        ss_p = A.alloc("ss_p", [128, 4], F32); SSP = P.buf("ss_p")
        p32 = A.alloc("p32", [128, 256], F32); P32 = P.buf("p32")
        pb16 = A.alloc("pb16", [128, 256], BF16); PB16 = P.buf("pb16")
        pTt_ = A.alloc("pTt_", [128, 2, 128], BF16); PTT_ = P.buf("pTt_")
        sg_ = [A.alloc_at(f"sg_{j}", [128, 512], F32, xo1 + 4096 + j * 2048) for j in range(2)]
        SG_ = P.bufs("sg_", 2)
        ple_banks = [2, 3, 6, 7]
        ple_rr = [0]

        def ple_psum():
            b_ = ple_banks[ple_rr[0] % 4]
            ple_rr[0] += 1
            return psum(b_)

        def ple_gen(i):
            X = xt[:, i, :]
            P.dma("sp", lambda e: e.dma_start(out=p32[:], in_=p_d[i * 128:(i + 1) * 128, :]), P32, writes=[P32])
            P.op("act", lambda e: e.activation(out=sq_p[:], in_=X, func=AF.Square, accum_out=ss_p[:, 0:1]),
                 reads=[XT[i]], writes=[SQP, SSP])
            yield
            P.op("act", lambda e: e.activation(out=ss_p[:, 1:2], in_=ss_p[:, 0:1], func=AF.Sqrt,
                                               scale=1.0 / D, bias=EPS), reads=[SSP], writes=[SSP])
            P.op("act", lambda e: e.copy(out=pb16[:], in_=p32[:]), reads=[P32], writes=[PB16])
            yield
            P.op("dve", lambda e: e.reciprocal(out=ss_p[:, 1:2], in_=ss_p[:, 1:2]), reads=[SSP], writes=[SSP])
            yield
            P.op("dve", lambda e: e.scalar_tensor_tensor(out=xs_p[:], in0=X, scalar=ss_p[:, 1:2], in1=gpl[:],
                                                         op0=ALU.mult, op1=ALU.mult),
                 reads=[XT[i], SSP, GPL], writes=[XSP])
            yield
            ps, PS = ple_psum()
            psb = ps[:].bitcast(BF16)
            for k in range(8):
                P.op("pe", lambda e, k=k, psb=psb: e.transpose(out=psb[:, k * 128:(k + 1) * 128],
                                                               in_=xs_p[:, k * 128:(k + 1) * 128],
                                                               identity=identb[:]), reads=[XSP, IDB], writes=[PS])
            ps_p, PS_P = ple_psum()
            psbp = ps_p[:].bitcast(BF16)
            for kk in range(2):
                P.op("pe", lambda e, kk=kk, psbp=psbp: e.transpose(out=psbp[:, kk * 128:(kk + 1) * 128],
                                                                   in_=pb16[:, kk * 128:(kk + 1) * 128],
                                                                   identity=identb[:]), reads=[PB16, IDB], writes=[PS_P])
            yield
            P.op("act", lambda e, psb=psb: e.copy(out=uTt[:], in_=psb.rearrange("p (k t) -> p k t", k=8)),
                 reads=[PS], writes=[UTT])
            P.op("act", lambda e, psbp=psbp: e.copy(out=pTt_[:], in_=psbp[:, 0:256].rearrange("p (k t) -> p k t", k=2)),
                 reads=[PS_P], writes=[PTT_])
            yield
            for hf in range(2):
                ps1, PS1 = ple_psum()
                for k in range(8):
                    P.op("pe", lambda e, ps1=ps1, k=k, hf=hf: e.matmul(
                        ps1[:], lhsT=uTt[:, k, :], rhs=wg[:, k, hf * 512:(hf + 1) * 512],
                        start=(k == 0), stop=(k == 7)), reads=[WG, UTT], writes=[PS1])
                ps2, PS2 = ple_psum()
                for kk in range(2):
                    P.op("pe", lambda e, ps2=ps2, kk=kk, hf=hf: e.matmul(
                        ps2[:], lhsT=pTt_[:, kk, :], rhs=wpp[:, kk, hf * 512:(hf + 1) * 512],
                        start=(kk == 0), stop=(kk == 1)), reads=[WPP, PTT_], writes=[PS2])
                yield
                P.op("act", lambda e, ps1=ps1, hf=hf: e.activation(out=sg_[hf][:], in_=ps1[:], func=AF.Sigmoid),
                     reads=[PS1], writes=[SG_[hf]])
                yield
                P.op("dve", lambda e, ps2=ps2, hf=hf: e.tensor_tensor(out=sg_[hf][:], in0=sg_[hf][:], in1=ps2[:],
                                                                      op=ALU.mult), reads=[PS2, SG_[hf]], writes=[SG_[hf]])
                yield
                P.op("dve", lambda e, hf=hf: e.tensor_tensor(
                    out=xt[:, i, hf * 512:(hf + 1) * 512], in0=xt[:, i, hf * 512:(hf + 1) * 512],
                    in1=sg_[hf][:], op=ALU.add), reads=[SG_[hf], XT[i]], writes=[XT[i]])
                yield
            P.op("act", lambda e: e.activation(out=sq_p[:], in_=X, func=AF.Square, accum_out=ss_p[:, 2:3]),
                 reads=[XT[i]], writes=[SQP, SSP])
            yield
            P.op("act", lambda e: e.activation(out=ss_p[:, 3:4], in_=ss_p[:, 2:3], func=AF.Sqrt,
                                               scale=1.0 / D, bias=EPS), reads=[SSP], writes=[SSP])
            yield
            P.op("dve", lambda e: e.reciprocal(out=ss_p[:, 3:4], in_=ss_p[:, 3:4]), reads=[SSP], writes=[SSP])
            yield
            P.op("dve", lambda e: e.scalar_tensor_tensor(out=X, in0=X, scalar=ss_p[:, 3:4], in1=gfn[:],
                                                         op0=ALU.mult, op1=ALU.mult),
                 reads=[XT[i], SSP, GFN], writes=[XT[i]])
            P.dma("sp", lambda e: e.dma_start(out=out_d[i * 128:(i + 1) * 128, :], in_=X), XT[i], reads=[XT[i]])
            yield

    pend = None
    for i in range(NT):
        P.op("dve", lambda e, i=i: e.scalar_tensor_tensor(
            out=xn32[:], in0=xt[:, i, :], scalar=rstd_all[:, i:i + 1], in1=gB[:],
            op0=ALU.mult, op1=ALU.mult), reads=[XT[i], RSTD, GB], writes=[XN])
        bank = 4 * (i % 2)
        psy = [psum(bank), psum(bank + 1)]
        for g in range(128 // GE):
            if pend is not None:
                next(pend, None)
            sls = []
            for u in range(GE):
                j = g * GE + u
                sl = gi % NS
                gi += 1
                sls.append(sl)
                P.dma("pool", lambda e, sl=sl, i=i, j=j: e.indirect_dma_start(
                    out=gs[sl][:], out_offset=None, in_=uv_d,
                    in_offset=bass.IndirectOffsetOnAxis(ap=idx_all[:, i, j:j + 1], axis=0)),
                    GS[sl], reads=[IDX[i]], writes=[GS[sl]])
            for u in range(GE):
                j = g * GE + u
                sl = sls[u]
                P.op("dve", lambda e, sl=sl, j=j: e.scalar_tensor_tensor(
                    out=junk[:], in0=gs[sl][:, 0:D], scalar=1.0, in1=xn32[:], op0=ALU.mult, op1=ALU.mult,
                    accum_out=dots[:, j:j + 1]), reads=[GS[sl], XN], writes=[JUNK, DOTS[j]])
            c0, c1 = g * GE, (g + 1) * GE
            P.op("act", lambda e, c0=c0, c1=c1: e.activation(out=wvg[:, c0:c1], in_=dots[:, c0:c1],
                                                            func=AF.Gelu_apprx_tanh),
                 reads=DOTS[c0:c1], writes=[WVG[g]])
            P.op("dve", lambda e, i=i, c0=c0, c1=c1: e.tensor_tensor(
                out=wv_t[:, c0:c1], in0=wvg[:, c0:c1], in1=gw_all[:, i, c0:c1], op=ALU.mult),
                reads=[WVG[g], IDX[i]], writes=[WVT[g]])
            for u in range(GE):
                j = g * GE + u
                sl = sls[u]
                dsl = di % NDG
                di += 1
                P.op("act", lambda e, dsl=dsl, j=j: e.activation(
                    out=dg[dsl][:], in_=identb[:], func=AF.Copy, scale=wv_t[:, j:j + 1]),
                    reads=[IDB, WVT[g]], writes=[DG[dsl]])
                for hf in range(2):
                    ps, PS = psy[hf]
                    P.op("pe", lambda e, ps=ps, dsl=dsl, sl=sl, hf=hf, j=j: e.matmul(
                        ps[:], lhsT=dg[dsl][:], rhs=gs[sl][:, D + hf * 512:D + (hf + 1) * 512],
                        start=(j == 0), stop=(j == 127)), reads=[DG[dsl], GS[sl]], writes=[PS])
        for hf in range(2):
            ps, PS = psy[hf]
            P.op("dve", lambda e, ps=ps, i=i, hf=hf: e.tensor_tensor(
                out=xt[:, i, hf * 512:(hf + 1) * 512], in0=xt[:, i, hf * 512:(hf + 1) * 512],
                in1=ps[:], op=ALU.add), reads=[PS, XT[i]], writes=[XT[i]])
        if fuse:
            if pend is not None:
                for _ in pend:
                    pass
            pend = ple_gen(i)
    if fuse:
        for _ in pend:
            pass
        return finalize(XT)
    P.barrier()
    A.release(m_phase)
    if stage == "ffn":
        for i in range(NT):
            P.dma("sp", lambda e, i=i: e.dma_start(out=out_d[i * 128:(i + 1) * 128, :], in_=xt[:, i, :]),
                  XT[i], reads=[XT[i]])
        return finalize(XT)

    m_phase = A.mark()
    norm_transpose(xt_src, g_ple_d, uT, UT)
    wg = A.alloc("wg", [128, 8, D], BF16); WG = P.buf("wg")
    P.dma("pool", lambda e: e.dma_start(out=wg[:], in_=w_gate_d.rearrange("(k p) c -> p k c", p=128)),
          WG, writes=[WG])
    wpp = A.alloc("wpp", [128, 2, D], BF16); WPP = P.buf("wpp")
    P.dma("pool", lambda e: e.dma_start(out=wpp[:], in_=w_proj_d.rearrange("(k p) c -> p k c", p=128)),
          WPP, writes=[WPP])
    pb = [A.alloc(f"pb{j}", [128, 256], BF16) for j in range(2)]; PB = P.bufs("pb", 2)
    pTt = [A.alloc(f"pTt{j}", [128, 2, 128], BF16) for j in range(2)]; PTT = P.bufs("pTt", 2)
    sg = [A.alloc(f"sg{j}", [128, 512], F32) for j in range(2)]; SG = P.bufs("sg", 2)
    it = 0
    for i in range(NT):
        j = i % 2
        P.dma("pool", lambda e, i=i, j=j: e.dma_start(out=pb[j][:], in_=p_d[i * 128:(i + 1) * 128, :]),
              PB[j], writes=[PB[j]])
        ps, PS = psum()
        psb = ps[:].bitcast(BF16)
        for kk in range(2):
            P.op("pe", lambda e, j=j, kk=kk, psb=psb: e.transpose(
                out=psb[:, kk * 128:(kk + 1) * 128], in_=pb[j][:, kk * 128:(kk + 1) * 128],
                identity=identb[:]), reads=[PB[j], IDB], writes=[PS])
        P.op("act", lambda e, j=j, psb=psb: e.copy(
            out=pTt[j][:], in_=psb[:, 0:256].rearrange("p (k t) -> p k t", k=2)), reads=[PS], writes=[PTT[j]])
        for hf in range(2):
            jj = it % 2
            it += 1
            ps1, PS1 = psum()
            for k in range(8):
                P.op("pe", lambda e, ps1=ps1, k=k, i=i, hf=hf: e.matmul(
                    ps1[:], lhsT=uT[:, k, i * 128:(i + 1) * 128], rhs=wg[:, k, hf * 512:(hf + 1) * 512],
                    start=(k == 0), stop=(k == 7)), reads=[WG, UT[i]], writes=[PS1])
            P.op("act", lambda e, ps1=ps1, jj=jj: e.activation(out=sg[jj][:], in_=ps1[:], func=AF.Sigmoid),
                 reads=[PS1], writes=[SG[jj]])
            ps2, PS2 = psum()
            for kk in range(2):
                P.op("pe", lambda e, ps2=ps2, kk=kk, j=j, hf=hf: e.matmul(
                    ps2[:], lhsT=pTt[j][:, kk, :], rhs=wpp[:, kk, hf * 512:(hf + 1) * 512],
                    start=(kk == 0), stop=(kk == 1)), reads=[WPP, PTT[j]], writes=[PS2])
            P.op("dve", lambda e, ps2=ps2, jj=jj: e.tensor_tensor(out=sg[jj][:], in0=sg[jj][:], in1=ps2[:], op=ALU.mult),
                 reads=[PS2, SG[jj]], writes=[SG[jj]])
            P.op("dve", lambda e, jj=jj, i=i, hf=hf: e.tensor_tensor(
                out=xt[:, i, hf * 512:(hf + 1) * 512], in0=xt[:, i, hf * 512:(hf + 1) * 512],
                in1=sg[jj][:], op=ALU.add), reads=[SG[jj], XT[i]], writes=[XT[i]])
    P.barrier()
    A.release(m_phase)

    P.dma("sp", lambda e: e.dma_start(out=gB[:], in_=bcast_rows(g_final_d, D)), GB, writes=[GB])
    sqf = A.alloc("fn_sq", [128, D], BF16); SQF = P.buf("fn_sq")
    ssf = A.alloc("fn_ss", [128, 2], F32); SSF = P.buf("fn_ss")
    for i in range(NT):
        P.op("act", lambda e, i=i: e.activation(out=sqf[:], in_=xt[:, i, :], func=AF.Square,
                                                accum_out=ssf[:, 0:1]), reads=[XT[i]], writes=[SQF, SSF])
        P.op("act", lambda e: e.activation(out=ssf[:, 1:2], in_=ssf[:, 0:1], func=AF.Sqrt,
                                           scale=1.0 / D, bias=EPS), reads=[SSF], writes=[SSF])
        P.op("dve", lambda e, i=i: e.reciprocal(out=rstd_all[:, i:i + 1], in_=ssf[:, 1:2]),
             reads=[SSF], writes=[RSTD])
        P.op("dve", lambda e, i=i: e.scalar_tensor_tensor(
            out=xt[:, i, :], in0=xt[:, i, :], scalar=rstd_all[:, i:i + 1], in1=gB[:],
            op0=ALU.mult, op1=ALU.mult), reads=[XT[i], RSTD, GB], writes=[XT[i]])
        P.dma("sp", lambda e, i=i: e.dma_start(out=out_d[i * 128:(i + 1) * 128, :], in_=xt[:, i, :]),
              XT[i], reads=[XT[i]])
    return finalize(XT)


def _t5_bucket(dist):
    max_exact = 16
    n = np.maximum(dist, 0)
    nf = np.maximum(n, 1).astype(np.float32)
    large = max_exact + (np.log(nf / max_exact) / np.float32(np.log(2048 / max_exact))
                         * (32 - max_exact)).astype(np.int32)
    large = np.minimum(large, 31)
    return np.where(n < max_exact, n, large)


def _bucket_table():
    import jax, jax.numpy as jnp
    return None


def prep_shared(inp):
    import ml_dtypes
    f32 = np.float32
    sh = {}
    sh["g_mix"] = np.ascontiguousarray(inp["g_mix"][0], f32)
    sh["w_in"] = np.ascontiguousarray(inp["w_in"][0], f32)
    sh["w_out"] = np.ascontiguousarray(inp["w_out"][0], f32)
    cv = np.zeros((128, 40), f32)
    cw = inp["conv_w"][0]
    for c in range(4):
        for j in range(4):
            cv[:, c * 4 + j] = cw[j, c * 128:(c + 1) * 128]
    for n, key in enumerate(["conv_b", "b_rg", "b_ig", "lru_lambda", "g_lru_out", "g_attn_out"]):
        cv[:, 16 + 4 * n:20 + 4 * n] = inp[key][0].reshape(4, 128).T
    sh["cv"] = cv
    for name, key in (("wrg_bd", "w_rg"), ("wig_bd", "w_ig")):
        w = inp[key][0]
        bd = np.zeros((128, 4, 128), f32)
        for g in range(8):
            c, l = g // 2, g % 2
            bd[l * 64:(l + 1) * 64, c, l * 64:(l + 1) * 64] = w[g]
        sh[name] = bd
    rb = inp["rel_bias"].astype(f32)
    k = np.arange(128)[:, None]
    q = np.arange(256)[None, :]
    dist = q - k
    valid = (dist >= 0) & (dist <= 128)
    bt = np.full((128, 4, 3, 2, 256), NEG, f32)
    for bi, d in enumerate((1, 4, 16)):
        bucket = _t5_bucket(dist * d)
        for h in range(8):
            vals = rb[bucket, h]
            bt[:, h // 2, bi, h % 2, :] = np.where(valid, vals, f32(NEG))
    sh["biasT"] = bt.reshape(128, 4, 3 * 2 * 256)
    sh["ident_bf"] = np.eye(128, dtype=f32).astype(ml_dtypes.bfloat16)
    sh["g_ffn"] = np.ascontiguousarray(inp["g_ffn"][0], f32)
    sh["peer_wq"] = np.ascontiguousarray(inp["peer_wq"][0], f32)
    sh["k1T"] = np.ascontiguousarray(inp["peer_k1"][0].T, f32)
    sh["k2T"] = np.ascontiguousarray(inp["peer_k2"][0].T, f32)
    sh["iota16"] = np.tile(np.arange(16, dtype=f32)[None], (128, 1))
    sh["peer_u"] = np.ascontiguousarray(inp["peer_u"][0], f32)
    sh["peer_v"] = np.ascontiguousarray(inp["peer_v"][0], f32)
    sh["g_ple"] = np.ascontiguousarray(inp["g_ple"][0], f32)
    sh["w_ple_gate"] = np.ascontiguousarray(inp["w_ple_gate"][0], f32)
    sh["w_ple_proj"] = np.ascontiguousarray(inp["w_ple_proj"][0], f32)
    sh["g_final"] = np.ascontiguousarray(inp["g_final"], f32)
    return sh


_NC_CACHE = {}


def kernel(**inputs):
    inp = {k: np.asarray(v) for k, v in inputs.items()}
    sh = prep_shared(inp)
    if "nc" not in _NC_CACHE:
        nc = bass.Bass("TRN2", target_bir_lowering=False)
        build(nc)
        _NC_CACHE["nc"] = nc
    nc = _NC_CACHE["nc"]
    in_maps = []
    for b in range(8):
        m = dict(sh)
        m["x"] = np.ascontiguousarray(inp["x"][b], np.float32)
        m["p"] = np.ascontiguousarray(inp["p"][0, b], np.float32)
        in_maps.append(m)
    res = run_bass_kernel_spmd(nc, in_maps, core_ids=list(range(8)))
    return np.stack([np.asarray(r["out"]) for r in res.results], axis=0).astype(np.float32)
```

```python
import numpy as np
import concourse.bass as bass
import concourse.mybir as mybir
from concourse.bass_utils import run_bass_kernel_spmd

F32 = mybir.dt.float32
BF16 = mybir.dt.bfloat16
U32 = mybir.dt.uint32
I32 = mybir.dt.int32
AF = mybir.ActivationFunctionType
ALU = mybir.AluOpType
AX = mybir.AxisListType

T = 2048
D = 1024
NT = 16
EPS = 1e-6
NEG = -30000.0


class Buf:
    __slots__ = ("name", "lastw", "reads", "dsem", "dcnt")

    def __init__(self, name):
        self.name = name
        self.lastw = None
        self.reads = {}
        self.dsem = None
        self.dcnt = 0


class Prog:
    ENG = ("pe", "dve", "act", "pool", "sp")

    def __init__(self, nc):
        self.nc = nc
        self.q = {e: [] for e in self.ENG}
        self.sem = {e: nc.alloc_semaphore(f"prog_{e}") for e in self.ENG}
        self.cnt = {e: 0 for e in self.ENG}
        self.waited = {e: {} for e in self.ENG}
        self.nbuf = 0
        self.dsems = []
        self.free_dsems = []

    def buf(self, name=None):
        self.nbuf += 1
        return Buf(name or f"b{self.nbuf}")

    def bufs(self, name, n):
        return [self.buf(f"{name}{i}") for i in range(n)]

    def _tokens(self, eng, reads, writes):
        need = {}

        def add(tok):
            if tok is None:
                return
            s, v = tok
            if need.get(id(s), (None, 0))[1] < v:
                need[id(s)] = (s, v)

        for b in reads:
            add(b.lastw)
        for b in writes:
            add(b.lastw)
            for tok in b.reads.values():
                add(tok)
        own = self.sem[eng]
        waits = []
        w = self.waited[eng]
        for s, v in need.values():
            if eng == "pe" and s is own:
                continue
            if w.get(id(s), 0) < v:
                w[id(s)] = v
                waits.append((s, v))
        return waits

    def _commit(self, tok, reads, writes):
        s, v = tok
        for b in reads:
            if b.reads.get(id(s), (None, 0))[1] < v:
                b.reads[id(s)] = (s, v)
        for b in writes:
            b.lastw = tok
            b.reads = {}

    def op(self, eng, fn, reads=(), writes=()):
        waits = self._tokens(eng, reads, writes)
        self.cnt[eng] += 1
        tok = (self.sem[eng], self.cnt[eng])
        self.q[eng].append((waits, fn, tok[0], 1))
        self._commit(tok, reads, writes)
        return tok

    def dma(self, eng, fn, sb, reads=(), writes=()):
        waits = self._tokens(eng, reads, writes)
        if sb.dsem is None:
            sb.dsem = self.nc.alloc_semaphore(f"dma_{sb.name}")
            self.dsems.append(sb)
        sb.dcnt += 16
        tok = (sb.dsem, sb.dcnt)
        self.q[eng].append((waits, fn, sb.dsem, 16))
        self._commit(tok, reads, writes)
        return tok

    def share_dsem(self, bufs):
        return bufs

    def barrier(self):
        toks = [(self.sem[e], self.cnt[e]) for e in self.ENG if self.cnt[e] > 0]
        toks += [(b.dsem, b.dcnt) for b in self.dsems]
        for e in self.ENG:
            waits = []
            w = self.waited[e]
            for s, v in toks:
                if e == "pe" and s is self.sem["pe"]:
                    continue
                if w.get(id(s), 0) < v:
                    w[id(s)] = v
                    waits.append((s, v))
            if waits:
                self.q[e].append((waits, None, None, 0))

    def emit(self):
        nc = self.nc
        with nc.Block() as block:
            @block.tensor
            def _(e):
                self._emit("pe", e)

            @block.vector
            def _(e):
                self._emit("dve", e)

            @block.scalar
            def _(e):
                self._emit("act", e)

            @block.gpsimd
            def _(e):
                self._emit("pool", e)

            @block.sync
            def _(e):
                self._emit("sp", e)

    def _emit(self, name, e):
        for waits, fn, sem, inc in self.q[name]:
            for s, v in waits:
                e.wait_ge(s, v)
            if fn is not None:
                ins = fn(e)
                ins.then_inc(sem, inc)


class Alloc:
    def __init__(self, nc):
        self.nc = nc
        self.lo = (int(nc.sbuf_base) + 63) // 64 * 64
        self.hi = int(nc.sbuf_top)
        self.cur = self.lo
        self.n = 0
        self.offs = {}
        self.extra = []

    def alloc(self, name, shape, dtype):
        size = int(np.prod(shape[1:])) * mybir.dt.size(dtype)
        for ex in self.extra:
            eo = (ex[0] + 63) // 64 * 64
            if size >= 1024 and eo + size <= ex[0] + ex[1]:
                ex[1] -= (eo + size - ex[0])
                ex[0] = eo + size
                self.n += 1
                self.offs[name] = eo
                return self.nc.alloc_sbuf_tensor_at(f"{name}_{self.n}", list(shape), dtype, offset=eo)
        off = (self.cur + 63) // 64 * 64
        assert off + size <= self.hi, f"SBUF overflow allocating {name}: {off}+{size} > {self.hi}"
        self.cur = off + size
        self.n += 1
        self.offs[name] = off
        return self.nc.alloc_sbuf_tensor_at(f"{name}_{self.n}", list(shape), dtype, offset=off)

    def alloc_at(self, name, shape, dtype, off):
        self.n += 1
        return self.nc.alloc_sbuf_tensor_at(f"{name}_{self.n}", list(shape), dtype, offset=off)

    def mark(self):
        return (self.cur, [list(e) for e in self.extra])

    def release(self, m):
        self.cur = m[0]
        self.extra = [list(e) for e in m[1]]


def bcast_rows(ap1d, n, parts=128):
    return bass.AP(ap1d.tensor, ap1d.offset, [[0, parts], [1, n]])


def peer_topk(P, A, sc, SC, iota16, IOTA, idx_out, gw_out, OUT):
    m = A.mark()
    m16 = A.alloc("tk_m16", [128, 16, 16], F32)
    M16A = P.bufs("tk_m16a", 16); M16B = P.bufs("tk_m16b", 16)
    ix16 = A.alloc("tk_ix16", [128, 16, 16], U32)
    IXA = P.bufs("tk_ixa", 16); IXB = P.bufs("tk_ixb", 16)
    cand = A.alloc("tk_cand", [128, 8, 256], F32); SCR = P.bufs("tk_scr", 16)
    scr = cand[:].rearrange("p h (s k) -> p (h s) k", s=2)
    for j in range(16):
        P.op("dve", lambda e, j=j: e.max(out=m16[:, j, 0:8], in_=sc[:, j, :]), reads=[SC], writes=[M16A[j]])
    for j in range(16):
        P.op("dve", lambda e, j=j: e.match_replace(out=scr[:, j, :], in_to_replace=m16[:, j, 0:8],
                                                   in_values=sc[:, j, :], imm_value=-1e30),
             reads=[SC, M16A[j]], writes=[SCR[j]])
    for j in range(16):
        P.op("dve", lambda e, j=j: e.max(out=m16[:, j, 8:16], in_=scr[:, j, :]), reads=[SCR[j]], writes=[M16B[j]])
    for j in range(16):
        P.op("dve", lambda e, j=j: e.max_index(out=ix16[:, j, 0:8], in_max=m16[:, j, 0:8], in_values=sc[:, j, :]),
             reads=[SC, M16A[j]], writes=[IXA[j]])
    for j in range(16):
        P.op("dve", lambda e, j=j: e.max_index(out=ix16[:, j, 8:16], in_max=m16[:, j, 8:16], in_values=sc[:, j, :]),
             reads=[SC, M16B[j]], writes=[IXB[j]])
    M16 = M16A + M16B
    IX16 = IXA + IXB
    pm = list(m16[:].ap[0])
    t1 = bass.AP(m16[:].tensor, m16[:].offset, [pm, [32, 8], [1, 16], [0, 16]])
    t2 = bass.AP(m16[:].tensor, m16[:].offset + 16, [pm, [32, 8], [0, 16], [1, 16]])
    P.op("dve", lambda e: e.tensor_tensor(out=cand[:].rearrange("p h (a b) -> p h a b", a=16), in0=t1, in1=t2,
                                          op=ALU.add), reads=M16, writes=SCR)
    c16 = A.alloc("tk_c16", [128, 8, 16], F32); C16A = P.bufs("tk_c16a", 8); C16B = P.bufs("tk_c16b", 8)
    pos = A.alloc("tk_pos", [128, 8, 16], U32); POSA = P.bufs("tk_posa", 8); POSB = P.bufs("tk_posb", 8)
    eq = A.alloc("tk_eq", [128, 8, 256], F32); EQS = P.bufs("tk_eq", 8)
    for h in range(8):
        P.op("dve", lambda e, h=h: e.max(out=c16[:, h, 0:8], in_=cand[:, h, :]),
             reads=SCR[2 * h:2 * h + 2], writes=[C16A[h]])
    for h in range(8):
        P.op("dve", lambda e, h=h: e.match_replace(out=eq[:, h, :], in_to_replace=c16[:, h, 0:8],
                                                   in_values=cand[:, h, :], imm_value=-1e30),
             reads=SCR[2 * h:2 * h + 2] + [C16A[h]], writes=[EQS[h]])
    for h in range(8):
        P.op("dve", lambda e, h=h: e.max(out=c16[:, h, 8:16], in_=eq[:, h, :]), reads=[EQS[h]], writes=[C16B[h]])
    for h in range(8):
        P.op("dve", lambda e, h=h: e.max_index(out=pos[:, h, 0:8], in_max=c16[:, h, 0:8], in_values=cand[:, h, :]),
             reads=SCR[2 * h:2 * h + 2] + [C16A[h]], writes=[POSA[h]])
    for h in range(8):
        P.op("dve", lambda e, h=h: e.max_index(out=pos[:, h, 8:16], in_max=c16[:, h, 8:16], in_values=cand[:, h, :]),
             reads=SCR[2 * h:2 * h + 2] + [C16B[h]], writes=[POSB[h]])
    C16 = P.buf("tk_c16all"); POS = P.buf("tk_posall"); EQ = P.buf("tk_eqall")
    for Bn, lst in ((C16, C16A + C16B), (POS, POSA + POSB), (EQ, EQS)):
        for b_ in lst:
            tok = b_.lastw
            if tok is not None and (Bn.lastw is None or True):
                s_, v_ = tok
                if Bn.reads.get(id(s_), (None, 0))[1] < v_:
                    Bn.reads[id(s_)] = (s_, v_)
            for tk in b_.reads.values():
                s_, v_ = tk
                if Bn.reads.get(id(s_), (None, 0))[1] < v_:
                    Bn.reads[id(s_)] = (s_, v_)
    eq4 = eq[:].rearrange("p h (a b) -> p h a b", a=16)
    au = A.alloc("tk_au", [128, 8, 16], U32); AU = P.buf("tk_au")
    bu = A.alloc("tk_bu", [128, 8, 16], U32); BU = P.buf("tk_bu")
    bf = A.alloc("tk_bf", [128, 8, 16], F32); BFb = P.buf("tk_bf")
    idf = A.alloc("tk_idf", [128, 2, 8, 16], F32); IDF = P.bufs("tk_idf", 2)
    ssum = A.alloc("tk_ssum", [128, 8], F32); SSUM = P.buf("tk_ssum")
    P.op("dve", lambda e: e.tensor_single_scalar(out=au[:], in_=pos[:], scalar=4, op=ALU.logical_shift_right),
         reads=[], writes=[AU, POS])
    P.op("dve", lambda e: e.tensor_single_scalar(out=bu[:], in_=pos[:], scalar=15, op=ALU.bitwise_and),
         reads=[POS], writes=[BU])
    cmax = bass.AP(c16[:].tensor, c16[:].offset, [list(c16[:].ap[0]), [16, 8], [0, 16]])
    P.op("dve", lambda e: e.tensor_tensor(out=bf[:], in0=c16[:], in1=cmax, op=ALU.subtract),
         reads=[], writes=[BFb, C16])
    P.op("act", lambda e: e.activation(out=bf[:], in_=bf[:], func=AF.Exp), reads=[BFb], writes=[BFb])
    io_ = bass.AP(iota16[:].tensor, iota16[:].offset, [list(iota16[:].ap[0]), [0, 8], [0, 16], [1, 16]])
    p0 = list(ix16[:].ap[0])
    gate_ops = [
        lambda: P.op("dve", lambda e: e.tensor_reduce(out=ssum[:], in_=bf[:], axis=AX.X, op=ALU.add),
                     reads=[BFb], writes=[SSUM]),
        lambda: P.op("dve", lambda e: e.reciprocal(out=ssum[:], in_=ssum[:]), reads=[SSUM], writes=[SSUM]),
        lambda: P.op("dve", lambda e: e.tensor_tensor(
            out=gw_out.rearrange("p (h k) -> p h k", h=8), in0=bf[:],
            in1=bass.AP(ssum[:].tensor, ssum[:].offset, [list(ssum[:].ap[0]), [1, 8], [0, 16]]), op=ALU.mult),
            reads=[BFb, SSUM], writes=[OUT]),
    ]
    for s_, (vu, VU) in enumerate(((au, AU), (bu, BU))):
        vb_ = bass.AP(vu[:].tensor, vu[:].offset, [list(vu[:].ap[0]), [16, 8], [1, 16], [0, 16]])
        P.op("dve", lambda e, vb_=vb_: e.tensor_tensor(out=eq4, in0=vb_, in1=io_, op=ALU.is_equal),
             reads=[VU, IOTA], writes=[EQ])
        if gate_ops:
            gate_ops.pop(0)()
        tab = bass.AP(ix16[:].tensor, ix16[:].offset + 16 * s_, [p0, [32, 8], [0, 16], [1, 16]])
        P.op("dve", lambda e, tab=tab: e.tensor_tensor(out=eq4, in0=eq4, in1=tab, op=ALU.mult),
             reads=[EQ] + IX16, writes=[EQ])
        if gate_ops:
            gate_ops.pop(0)()
        P.op("dve", lambda e, s_=s_: e.tensor_reduce(out=idf[:, s_, :, :], in_=eq4, axis=AX.X, op=ALU.add),
             reads=[EQ], writes=[IDF[s_]])
    while gate_ops:
        gate_ops.pop(0)()
    P.op("dve", lambda e: e.scalar_tensor_tensor(out=idf[:, 0, :, :], in0=idf[:, 0, :, :], scalar=128.0,
                                                 in1=idf[:, 1, :, :], op0=ALU.mult, op1=ALU.add),
         reads=IDF, writes=[IDF[0]])
    P.op("dve", lambda e: e.tensor_copy(out=idx_out, in_=idf[:, 0, :, :].rearrange("p h k -> p (h k)")),
         reads=[IDF[0]], writes=[OUT])
    A.release(m)


def build(nc, stage="full", dumps=()):
    P = Prog(nc)
    A = Alloc(nc)
    dumps = set(dumps)
    dump_list = []

    def din(name, shape, dt=F32):
        return nc.dram_tensor(name, list(shape), dt, kind="ExternalInput").ap()

    x_d = din("x", [T, D])
    p_d = din("p", [T, 256])
    g_mix_d = din("g_mix", [D])
    w_in_d = din("w_in", [D, 2560])
    cv_d = din("cv", [128, 40])
    wrg_d = din("wrg_bd", [128, 4, 128])
    wig_d = din("wig_bd", [128, 4, 128])
    biasT_d = din("biasT", [128, 4, 3 * 2 * 256])
    w_out_d = din("w_out", [D, D])
    identb_d = din("ident_bf", [128, 128], BF16)
    g_ffn_d = din("g_ffn", [D])
    peer_wq_d = din("peer_wq", [D, 2048])
    k1T_d = din("k1T", [128, 128])
    k2T_d = din("k2T", [128, 128])
    iota_d = din("iota16", [128, 16])
    peer_u_d = din("peer_u", [16384, D])
    peer_v_d = din("peer_v", [16384, D])
    g_ple_d = din("g_ple", [D])
    w_gate_d = din("w_ple_gate", [D, D])
    w_proj_d = din("w_ple_proj", [256, D])
    g_final_d = din("g_final", [D])
    out_d = nc.dram_tensor("out", [T, D], F32, kind="ExternalOutput").ap()

    def dump(name, ap, bufs, shape, dt=F32):
        if name not in dumps:
            return
        d = nc.dram_tensor("dbg_" + name, list(shape), dt, kind="ExternalOutput").ap()
        DB = P.buf("dbg_" + name)
        P.dma("sp", lambda e: e.dma_start(out=d, in_=ap), DB, reads=list(bufs))
        dump_list.append(DB)

    psall = nc.alloc_psum_tensor("psall", [128, 8 * 512], F32)
    ps_t = [psall[:, i * 512:(i + 1) * 512] for i in range(8)]
    ps_b = [P.buf(f"psb{i}") for i in range(8)]
    ps_rr = [0]

    def psum(idx=None):
        if idx is None:
            idx = ps_rr[0]
            ps_rr[0] = (idx + 1) % 8
        return ps_t[idx], ps_b[idx]

    def psum_pair():
        idx = (ps_rr[0] + 1) // 2 * 2 % 8
        ps_rr[0] = (idx + 2) % 8
        return psall[:, idx * 512:(idx + 2) * 512].rearrange("p (a q) -> p a q", a=2), [ps_b[idx], ps_b[idx + 1]]

    identb = A.alloc("identb", [128, 128], BF16); IDB = P.buf("identb")
    P.dma("sp", lambda e: e.dma_start(out=identb[:], in_=identb_d), IDB, writes=[IDB])
    ones_f = A.alloc("ones_f", [128, 128], F32); ONF = P.buf("ones_f")
    P.op("pool", lambda e: e.memset(ones_f[:], 1.0), writes=[ONF])
    ones_b = A.alloc("ones_b", [128, 64], BF16); ONB = P.buf("ones_b")
    P.op("pool", lambda e: e.memset(ones_b[:], 1.0), writes=[ONB])
    cv = A.alloc("cv", [128, 40], F32); CV = P.buf("cv")
    P.dma("sp", lambda e: e.dma_start(out=cv[:], in_=cv_d), CV, writes=[CV])
    gB = A.alloc("gB", [128, D], F32); GB = P.buf("gB")
    rstd_all = A.alloc("rstd_all", [128, NT], F32); RSTD = P.buf("rstd_all")

    uT = A.alloc("uT", [128, 8, T], BF16); UT = P.bufs("uT", NT)
    yT = A.alloc("yT", [128, 8, T], BF16); YT = P.bufs("yT", 8)

    uv_d = nc.dram_tensor("uv16", [16384, 2 * D], BF16, kind="Internal").ap()
    RPP = 2
    NPS = 3
    pst = [A.alloc(f"pst{j}", [128, RPP, D], F32) for j in range(NPS)]; PST = P.bufs("pst", NPS)
    psb_ = [A.alloc(f"ppb{j}", [128, RPP, D], BF16) for j in range(NPS)]; PSB = P.bufs("ppb", NPS)
    UVD = P.buf("uv16")

    def prepass_gen():
        steps = []
        for t_, c0 in ((peer_u_d, 0), (peer_v_d, D)):
            for blk in range(16384 // (128 * RPP)):
                r0 = blk * 128 * RPP
                src = t_[r0:r0 + 128 * RPP, :].rearrange("(p r) d -> p r d", r=RPP)
                dst = uv_d[r0:r0 + 128 * RPP, c0:c0 + D].rearrange("(p r) d -> p r d", r=RPP)
                steps.append((src, dst))

        def load(n):
            j = n % NPS
            src = steps[n][0]
            P.dma("sp", lambda e, j=j, src=src: e.dma_start(out=pst[j][:], in_=src), PST[j], writes=[PST[j]])

        load(0)
        load(1)
        for n in range(len(steps)):
            j = n % NPS
            dst = steps[n][1]
            if n % 2 == 0:
                P.op("pool", lambda e, j=j: e.tensor_copy(out=psb_[j][:], in_=pst[j][:]),
                     reads=[PST[j]], writes=[PSB[j]])
            else:
                P.op("act", lambda e, j=j: e.copy(out=psb_[j][:], in_=pst[j][:]),
                     reads=[PST[j]], writes=[PSB[j]])
            if n + 2 < len(steps):
                load(n + 2)
            P.dma("sp", lambda e, j=j, dst=dst: e.dma_start(out=dst, in_=psb_[j][:]), PSB[j],
                  reads=[PSB[j]], writes=[UVD])
            yield

    pp = prepass_gen()

    def pp_adv(n):
        for _ in range(n):
            next(pp, None)

    def norm_transpose(src_tiles, gvec_d, dstT, DST, hook=None, end_hook=None):
        m = A.mark()
        P.dma("sp", lambda e: e.dma_start(out=gB[:], in_=bcast_rows(gvec_d, D)), GB, writes=[GB])
        sq = A.alloc("nt_sq", [128, D], BF16); SQ = P.buf("nt_sq")
        ss = A.alloc("nt_ss", [128, 2 * NT], F32); SSL = P.bufs("nt_ss", NT)
        xs = [A.alloc(f"nt_xs{j}", [128, D], BF16) for j in range(2)]
        XS = P.bufs("nt_xs", 2)
        pend = {}

        def stage1(i):
            if hook is not None:
                hook(i)
            src, SRC = src_tiles(i)
            SS = SSL[i]
            P.op("dve", lambda e, src=src, i=i: e.scalar_tensor_tensor(
                out=sq[:], in0=src, scalar=1.0, in1=src, op0=ALU.mult, op1=ALU.mult,
                accum_out=ss[:, 2 * i:2 * i + 1]), reads=SRC, writes=[SQ, SS])
            P.op("act", lambda e, i=i: e.activation(out=ss[:, 2 * i + 1:2 * i + 2], in_=ss[:, 2 * i:2 * i + 1],
                                                    func=AF.Sqrt, scale=1.0 / D, bias=EPS),
                 reads=[SS], writes=[SS])
            P.op("dve", lambda e, i=i: e.reciprocal(out=rstd_all[:, i:i + 1], in_=ss[:, 2 * i + 1:2 * i + 2]),
                 reads=[SS], writes=[RSTD])
            j = i % 2
            P.op("dve", lambda e, src=src, i=i, j=j: e.scalar_tensor_tensor(
                out=xs[j][:], in0=src, scalar=rstd_all[:, i:i + 1], in1=gB[:],
                op0=ALU.mult, op1=ALU.mult), reads=SRC + [RSTD, GB], writes=[XS[j]])
            ps, PS = psum()
            psb = ps[:].bitcast(BF16)
            for k in range(8):
                P.op("pe", lambda e, j=j, k=k, psb=psb: e.transpose(
                    out=psb[:, k * 128:(k + 1) * 128], in_=xs[j][:, k * 128:(k + 1) * 128],
                    identity=identb[:]), reads=[XS[j], IDB], writes=[PS])
            pend[i] = (psb, PS)

        def stage2(i):
            psb, PS = pend.pop(i)
            P.op("act", lambda e, i=i, psb=psb: e.copy(
                out=dstT[:, :, i * 128:(i + 1) * 128],
                in_=psb.rearrange("p (k t) -> p k t", k=8)), reads=[PS], writes=[DST[i]])

        for i in range(NT + 1):
            if i < NT:
                stage1(i)
            if i >= 1:
                stage2(i - 1)
        if end_hook is not None:
            end_hook()
        P.barrier()
        A.release(m)

    wsl = [A.alloc(f"wsl{j}", [128, 8, 128], BF16) for j in range(3)]
    WSL = P.bufs("wsl", 3)
    wsl_rr = [0]

    def load_wchunk(w_d, col0):
        j = wsl_rr[0]
        wsl_rr[0] = (j + 1) % 3
        src = w_d.rearrange("(k p) c -> p k c", p=128)[:, :, col0:col0 + 128]
        P.dma("pool", lambda e: e.dma_start(out=wsl[j][:], in_=src), WSL[j], writes=[WSL[j]])
        return wsl[j], WSL[j]

    def proj_chunk(w_d, col0, srcT, SRC, evac):
        w, W = load_wchunk(w_d, col0)
        for tq in range(4):
            ps, PS = psum()
            for k in range(8):
                P.op("pe", lambda e, ps=ps, k=k, tq=tq, w=w: e.matmul(
                    ps[:], lhsT=w[:, k, :], rhs=srcT[:, k, tq * 512:(tq + 1) * 512],
                    start=(k == 0), stop=(k == 7)),
                    reads=[W] + SRC[tq * 4:(tq + 1) * 4], writes=[PS])
            evac(tq, ps, PS)

    def finalize(out_bufs):
        final = []
        w = P.waited["sp"]
        for b in list(out_bufs) + dump_list:
            if b.dsem is not None and w.get(id(b.dsem), 0) < b.dcnt:
                w[id(b.dsem)] = b.dcnt
                final.append((b.dsem, b.dcnt))
        P.q["sp"].append((final, None, None, 0))
        P.emit()
        return nc

    m_phase = A.mark()
    xin = [A.alloc(f"xin{j}", [128, D], F32) for j in range(2)]
    XIN = P.bufs("xin", 2)

    def x_src(i):
        j = i % 2
        P.dma("sp", lambda e: e.dma_start(out=xin[j][:], in_=x_d[i * 128:(i + 1) * 128, :]),
              XIN[j], writes=[XIN[j]])
        return xin[j][:], [XIN[j]]

    norm_transpose(x_src, g_mix_d, uT, UT, hook=lambda i: pp_adv(i % 2))
    dump("uT", uT[:], UT, [128, 8, T], BF16)
    P.barrier()
    A.release(m_phase)
    if stage == "p1":
        return finalize([])

    m_phase = A.mark()
    y32 = A.alloc("y32", [128, 4, T], F32); Y32 = P.bufs("y32", 4)
    wrg = A.alloc("wrg", [128, 4, 128], BF16); WRG = P.buf("wrg")
    wig = A.alloc("wig", [128, 4, 128], BF16); WIG = P.buf("wig")
    P.dma("pool", lambda e: e.dma_start(out=wrg[:], in_=wrg_d), WRG, writes=[WRG])
    P.dma("pool", lambda e: e.dma_start(out=wig[:], in_=wig_d), WIG, writes=[WIG])
    nsp = A.alloc("nsp", [128, 4], F32); NSP = P.buf("nsp")
    P.op("act", lambda e: e.activation(out=nsp[:], in_=cv[:, 28:32], func=AF.Exp, scale=-1.0),
         reads=[CV], writes=[NSP])
    P.op("act", lambda e: e.activation(out=nsp[:], in_=nsp[:], func=AF.Ln, scale=1.0, bias=1.0),
         reads=[NSP], writes=[NSP])
    P.op("dve", lambda e: e.tensor_scalar(out=nsp[:], in0=nsp[:], scalar1=-8.0, scalar2=None,
                                          op0=ALU.mult), reads=[NSP], writes=[NSP])

    def big(name, dt=F32):
        return A.alloc(name, [128, T], dt), P.buf(name)

    xl, XL = big("xl"); gt, GT = big("gt"); xc, XC = big("xc"); xcb, XCB = big("xcb", BF16)
    ra, RA = big("ra"); ig, IG = big("ig"); tmp, TMP = big("tmp")

    for c in range(4):
        def ev_xl(tq, ps, PS):
            P.op("act", lambda e, ps=ps, tq=tq: e.copy(out=xl[:, tq * 512:(tq + 1) * 512], in_=ps[:]),
                 reads=[PS], writes=[XL])

        def ev_gt(tq, ps, PS):
            P.op("act", lambda e, ps=ps, tq=tq: e.copy(out=gt[:, tq * 512:(tq + 1) * 512], in_=ps[:]),
                 reads=[PS], writes=[GT])

        proj_chunk(w_in_d, c * 128, uT, UT, ev_xl)
        pp_adv(2)
        proj_chunk(w_in_d, 512 + c * 128, uT, UT, ev_gt)
        pp_adv(2)
        P.op("dve", lambda e, c=c: e.tensor_scalar(
            out=xc[:], in0=xl[:], scalar1=cv[:, c * 4 + 3:c * 4 + 4], scalar2=cv[:, 16 + c:17 + c],
            op0=ALU.mult, op1=ALU.add), reads=[XL, CV], writes=[XC])
        for s in (1, 2, 3):
            P.op("dve", lambda e, c=c, s=s: e.scalar_tensor_tensor(
                out=xc[:, s:], in0=xl[:, 0:T - s], scalar=cv[:, c * 4 + 3 - s:c * 4 + 4 - s],
                in1=xc[:, s:], op0=ALU.mult, op1=ALU.add), reads=[XL, CV, XC], writes=[XC])
        P.op("act", lambda e: e.copy(out=xcb[:], in_=xc[:]), reads=[XC], writes=[XCB])
        for tq in range(4):
            ps, PS = psum()
            P.op("pe", lambda e, ps=ps, tq=tq, c=c: e.matmul(
                ps[:], lhsT=wrg[:, c, :], rhs=xcb[:, tq * 512:(tq + 1) * 512], start=True, stop=True),
                reads=[WRG, XCB], writes=[PS])
            P.op("act", lambda e, ps=ps, tq=tq, c=c: e.activation(
                out=ra[:, tq * 512:(tq + 1) * 512], in_=ps[:], func=AF.Sigmoid,
                bias=cv[:, 20 + c:21 + c], scale=1.0), reads=[PS, CV], writes=[RA])
        for tq in range(4):
            ps, PS = psum()
            P.op("pe", lambda e, ps=ps, tq=tq, c=c: e.matmul(
                ps[:], lhsT=wig[:, c, :], rhs=xcb[:, tq * 512:(tq + 1) * 512], start=True, stop=True),
                reads=[WIG, XCB], writes=[PS])
            P.op("act", lambda e, ps=ps, tq=tq, c=c: e.activation(
                out=ig[:, tq * 512:(tq + 1) * 512], in_=ps[:], func=AF.Sigmoid,
                bias=cv[:, 24 + c:25 + c], scale=1.0), reads=[PS, CV], writes=[IG])
        P.op("act", lambda e, c=c: e.activation(out=ra[:], in_=ra[:], func=AF.Exp,
                                                scale=nsp[:, c:c + 1]), reads=[RA, NSP], writes=[RA])
        P.op("dve", lambda e: e.tensor_tensor(out=tmp[:], in0=ra[:], in1=ra[:], op=ALU.mult),
             reads=[RA], writes=[TMP])
        P.op("act", lambda e: e.activation(out=tmp[:], in_=tmp[:], func=AF.Sqrt, scale=-1.0, bias=1.0),
             reads=[TMP], writes=[TMP])
        P.op("dve", lambda e: e.tensor_tensor(out=ig[:], in0=ig[:], in1=xc[:], op=ALU.mult),
             reads=[IG, XC], writes=[IG])
        P.op("dve", lambda e: e.tensor_tensor(out=ig[:], in0=ig[:], in1=tmp[:], op=ALU.mult),
             reads=[IG, TMP], writes=[IG])
        P.op("dve", lambda e: e.tensor_tensor_scan(out=tmp[:], data0=ra[:], data1=ig[:], initial=0.0,
                                                   op0=ALU.mult, op1=ALU.add),
             reads=[RA, IG], writes=[TMP])
        P.op("act", lambda e: e.activation(out=gt[:], in_=gt[:], func=AF.Gelu_apprx_tanh),
             reads=[GT], writes=[GT])
        P.op("dve", lambda e, c=c: e.tensor_tensor(out=y32[:, c, :], in0=tmp[:], in1=gt[:], op=ALU.mult),
             reads=[TMP, GT], writes=[Y32[c]])

    def group_rmsnorm(gcol0, ybase, y32, Y32, tmp, TMP, ra, RA):
        pss = [psum(i) for i in range(4)]
        for c in range(4):
            P.op("act", lambda e, c=c: e.activation(out=tmp[:], in_=y32[:, c, :], func=AF.Square),
                 reads=[Y32[c]], writes=[TMP])
            for tq in range(4):
                ps, PS = pss[tq]
                P.op("pe", lambda e, ps=ps, tq=tq, c=c: e.matmul(
                    ps[:], lhsT=ones_f[:], rhs=tmp[:, tq * 512:(tq + 1) * 512],
                    start=(c == 0), stop=(c == 3)), reads=[ONF, TMP], writes=[PS])
        for tq in range(4):
            ps, PS = pss[tq]
            P.op("act", lambda e, ps=ps, tq=tq: e.activation(
                out=ra[:, tq * 512:(tq + 1) * 512], in_=ps[:], func=AF.Sqrt, scale=1.0 / 512, bias=EPS),
                reads=[PS], writes=[RA])
        P.op("dve", lambda e: e.reciprocal(out=ra[:], in_=ra[:]), reads=[RA], writes=[RA])
        for c in range(4):
            P.op("dve", lambda e, c=c: e.scalar_tensor_tensor(
                out=yT[:, ybase + c, :], in0=y32[:, c, :], scalar=cv[:, gcol0 + c:gcol0 + c + 1],
                in1=ra[:], op0=ALU.mult, op1=ALU.mult), reads=[Y32[c], CV, RA], writes=[YT[ybase + c]])

    dump("y_lru", y32[:], Y32, [128, 4, T])
    if stage == "p2a":
        return finalize([])
    group_rmsnorm(32, 0, y32, Y32, tmp, TMP, ra, RA)
    if stage == "p2":
        dump("yT", yT[:], YT, [128, 8, T], BF16)
    P.barrier()
    A.release(m_phase)
    if stage == "p2":
        return finalize([])

    m_phase = A.mark()
    y3 = A.alloc("y32a", [128, 4, T], F32); Y3 = P.bufs("y32a", 4)
    qT, QT = big("qT", BF16); kT, KT = big("kT", BF16)
    dacc, DACC = big("dacc")
    bias_sb = A.alloc("bias_sb", [128, 3, 2, 256], F32); BIAS = P.buf("bias_sb")
    vbs = [A.alloc(f"vb{j}", [128, 16, 128], BF16) for j in range(2)]; VBS = P.bufs("vb", 2)
    NSL = 4
    tS = [A.alloc(f"tS{j}", [128, 2, 256], F32) for j in range(NSL)]; TS = P.bufs("tS", NSL)
    pT = [A.alloc(f"pT{j}", [128, 2, 256], BF16) for j in range(NSL)]; PT = P.bufs("pT", NSL)
    oacc = A.alloc("oacc", [128, 2, T], F32); OAC = P.buf("oacc")
    ra3 = A.alloc_at("ra2", [128, T], F32, A.offs["oacc"]); RA3 = OAC
    tmp3 = A.alloc_at("tmp2", [128, T], F32, A.offs["oacc"] + T * 4); TMP3 = OAC
    it = 0
    for c in range(4):
        OACC = Y3[c]
        P.dma("sp", lambda e, c=c: e.dma_start(
            out=bias_sb[:].rearrange("p a b q -> p (a b q)"), in_=biasT_d[:, c, :]), BIAS, writes=[BIAS])

        def ev_q(tq, ps, PS):
            P.op("act", lambda e, ps=ps, tq=tq: e.activation(
                out=qT[:, tq * 512:(tq + 1) * 512], in_=ps[:], func=AF.Copy, scale=0.125),
                reads=[PS], writes=[QT])

        def ev_k(tq, ps, PS):
            P.op("act", lambda e, ps=ps, tq=tq: e.copy(out=kT[:, tq * 512:(tq + 1) * 512], in_=ps[:]),
                 reads=[PS], writes=[KT])

        proj_chunk(w_in_d, 1024 + c * 128, uT, UT, ev_q)
        proj_chunk(w_in_d, 1536 + c * 128, uT, UT, ev_k)
        wv, WV = load_wchunk(w_in_d, 2048 + c * 128)
        P.op("pool", lambda e: e.memset(oacc[:], 0.0), writes=[OAC])
        iters = []
        for bi, d in enumerate((1, 4, 16)):
            nb = 16 // d
            first = True
            for r in range(d):
                for m in range(nb):
                    iters.append((bi, d, nb, r, m, first))
                    first = False

        def stage_S(n):
            bi, d, nb, r, m, first = iters[n]
            vbb, VBB = vbs[bi % 2], VBS[bi % 2]
            if first:
                for g4 in range(4):
                    ps, PS = psum()
                    for u4 in range(4):
                        ti = g4 * 4 + u4
                        rr, mm = ti // nb, ti % nb
                        tt0 = rr + d * 128 * mm
                        for k in range(8):
                            P.op("pe", lambda e, ps=ps, u4=u4, k=k, tt0=tt0, d=d, wv=wv: e.matmul(
                                ps[:, u4 * 128:(u4 + 1) * 128],
                                lhsT=uT[:, k, tt0:tt0 + d * 127 + 1:d], rhs=wv[:, k, :],
                                start=(k == 0), stop=(k == 7)), reads=[WV] + UT, writes=[PS])
                    P.op("act", lambda e, ps=ps, g4=g4, vbb=vbb: e.copy(
                        out=vbb[:, g4 * 4:(g4 + 1) * 4, :], in_=ps[:].rearrange("p (a b) -> p a b", a=4)),
                        reads=[PS], writes=[VBB])
            nq = 256 if m + 1 < nb else 128
            t0 = r + d * 128 * m
            kend = t0 + d * 127 + 1
            qend = t0 + d * (nq - 1) + 1
            j = n % NSL
            if n % 3 == 0:
                pp_adv(1)
            ps, PSP = psum_pair()
            for hh in range(2):
                p0 = hh * 64
                P.op("pe", lambda e, ps=ps, hh=hh, p0=p0, t0=t0, kend=kend, qend=qend, d=d, nq=nq: e.matmul(
                    ps[:, hh, 0:nq], lhsT=kT[p0:p0 + 64, t0:kend:d],
                    rhs=qT[p0:p0 + 64, t0:qend:d], start=True, stop=True),
                    reads=[KT, QT], writes=[PSP[hh]])
            P.op("dve", lambda e, ps=ps, j=j, bi=bi, nq=nq: e.tensor_tensor(
                out=tS[j][:, :, 0:nq], in0=ps[:, :, 0:nq],
                in1=bias_sb[:, bi, :, 0:nq], op=ALU.add), reads=PSP + [BIAS], writes=[TS[j]])
            P.op("act", lambda e, j=j, nq=nq: e.activation(
                out=pT[j][:, :, 0:nq], in_=tS[j][:, :, 0:nq], func=AF.Exp),
                reads=[TS[j]], writes=[PT[j]])

        def stage_PV(n):
            bi, d, nb, r, m, first = iters[n]
            vbb, VBB = vbs[bi % 2], VBS[bi % 2]
            nq = 256 if m + 1 < nb else 128
            t0 = r + d * 128 * m
            qend = t0 + d * (nq - 1) + 1
            j = n % NSL
            ps2, PS2 = psum()
            ti = r * nb + m
            for hh in range(2):
                p0 = hh * 64
                P.op("pe", lambda e, ps2=ps2, hh=hh, p0=p0, ti=ti, j=j, nq=nq, vbb=vbb: e.matmul(
                    ps2[p0:p0 + 64, 0:nq], lhsT=vbb[:, ti, hh * 64:(hh + 1) * 64],
                    rhs=pT[j][:, hh, 0:nq], start=True, stop=True),
                    reads=[VBB, PT[j]], writes=[PS2])
                P.op("pe", lambda e, ps2=ps2, hh=hh, p0=p0, j=j, nq=nq: e.matmul(
                    ps2[p0:p0 + 64, 256:256 + nq], lhsT=ones_b[:, 0:64],
                    rhs=pT[j][:, hh, 0:nq], start=True, stop=True),
                    reads=[ONB, PT[j]], writes=[PS2])
            P.op("dve", lambda e, ps2=ps2, t0=t0, qend=qend, d=d, nq=nq: e.tensor_tensor(
                out=oacc[:, :, t0:qend:d], in0=oacc[:, :, t0:qend:d],
                in1=ps2[:].rearrange("p (a q) -> p a q", a=2)[:, :, 0:nq], op=ALU.add),
                reads=[PS2, OAC], writes=[OAC])

        for n in range(len(iters) + 2):
            if n < len(iters):
                stage_S(n)
            if n >= 2:
                stage_PV(n - 2)
        P.op("dve", lambda e: e.reciprocal(out=dacc[:], in_=oacc[:, 1, :]), reads=[OAC], writes=[DACC])
        P.op("dve", lambda e, c=c: e.tensor_tensor(out=y3[:, c, :], in0=oacc[:, 0, :], in1=dacc[:], op=ALU.mult),
             reads=[OAC, DACC], writes=[OACC])
    dump("y_att", y3[:], Y3, [128, 4, T])
    if stage == "p3a":
        return finalize([])
    group_rmsnorm(36, 4, y3, Y3, tmp3, TMP3, ra3, RA3)
    if stage != "p2":
        dump("yT", yT[:], YT, [128, 8, T], BF16)
    P.barrier()
    A.release(m_phase)

    xt = A.alloc("xt", [128, NT, D], F32); XT = P.bufs("xt", NT)
    m_phase = A.mark()
    wo = A.alloc("wo", [128, 8, D], BF16); WO = P.buf("wo")
    P.dma("pool", lambda e: e.dma_start(out=wo[:], in_=w_out_d.rearrange("(k p) c -> p k c", p=128)),
          WO, writes=[WO])
    for i in range(NT):
        P.dma("sp", lambda e, i=i: e.dma_start(out=xt[:, i, :], in_=x_d[i * 128:(i + 1) * 128, :]),
              XT[i], writes=[XT[i]])
    for i in range(NT):
        for hf in range(2):
            pp_adv(1)
            ps, PS = psum()
            for k in range(8):
                P.op("pe", lambda e, ps=ps, k=k, i=i, hf=hf: e.matmul(
                    ps[:], lhsT=yT[:, k, i * 128:(i + 1) * 128], rhs=wo[:, k, hf * 512:(hf + 1) * 512],
                    start=(k == 0), stop=(k == 7)), reads=[WO] + YT, writes=[PS])
            P.op("dve", lambda e, ps=ps, i=i, hf=hf: e.tensor_tensor(
                out=xt[:, i, hf * 512:(hf + 1) * 512], in0=xt[:, i, hf * 512:(hf + 1) * 512],
                in1=ps[:], op=ALU.add), reads=[PS, XT[i]], writes=[XT[i]])
    if stage == "mixer":
        for _ in pp:
            pass
    P.barrier()
    A.release(m_phase)

    if stage == "mixer":
        for i in range(NT):
            P.dma("sp", lambda e, i=i: e.dma_start(out=out_d[i * 128:(i + 1) * 128, :], in_=xt[:, i, :]),
                  XT[i], reads=[XT[i]])
        return finalize(XT)

    def xt_src(i):
        return xt[:, i, :], [XT[i]]

    m_phase = A.mark()
    def pp_drain():
        for _ in pp:
            pass

    norm_transpose(xt_src, g_ffn_d, uT, UT, hook=lambda i: pp_adv(2), end_hook=pp_drain)
    wq = yT
    WQ = P.buf("wq")
    wq_src = peer_wq_d.rearrange("(k p) c -> p k c", p=128)
    for hf in range(2):
        P.dma("pool", lambda e, hf=hf: e.dma_start(out=wq[:, :, hf * 1024:(hf + 1) * 1024],
                                                   in_=wq_src[:, :, hf * 1024:(hf + 1) * 1024]),
              WQ, writes=[WQ])
    kTs = A.alloc("kTs", [128, 2, 128], F32); KTS = P.buf("kTs")
    P.dma("sp", lambda e: e.dma_start(out=kTs[:, 0, :], in_=k1T_d), KTS, writes=[KTS])
    P.dma("sp", lambda e: e.dma_start(out=kTs[:, 1, :], in_=k2T_d), KTS, writes=[KTS])
    iota16 = A.alloc("iota16", [128, 16], F32); IOTA = P.buf("iota16")
    P.dma("sp", lambda e: e.dma_start(out=iota16[:], in_=iota_d), IOTA, writes=[IOTA])
    idx_all = A.alloc("idx_all", [128, NT, 128], I32); IDX = P.bufs("idx_all", NT)
    gw_all = A.alloc("gw_all", [128, NT, 128], F32)
    m_sub = A.mark()
    assert A.offs["ppb1"] == A.offs["ppb0"] + RPP * D * 2 and A.offs["ppb2"] == A.offs["ppb0"] + 2 * RPP * D * 2
    qTt = A.alloc_at("qTt", [128, 16, 128], F32, A.offs["pst0"]); QTT = P.buf("qTt")
    scs = [A.alloc_at("sc0", [128, 16, 128], F32, A.offs["pst1"]),
           A.alloc_at("sc1", [128, 16, 128], F32, A.offs["pst2"])]
    SCS = P.bufs("sc", 2)
    A.extra = [[A.offs["ppb0"], 3 * RPP * D * 2]]
    for i in range(NT):
        for jg in range(4):
            ps, PS = psum()
            for u in range(4):
                j = jg * 4 + u
                for k in range(8):
                    P.op("pe", lambda e, ps=ps, u=u, j=j, k=k, i=i: e.matmul(
                        ps[:, u * 128:(u + 1) * 128], lhsT=wq[:, k, j * 128:(j + 1) * 128],
                        rhs=uT[:, k, i * 128:(i + 1) * 128], start=(k == 0), stop=(k == 7)),
                        reads=[WQ, UT[i]], writes=[PS])
            P.op("act", lambda e, ps=ps, jg=jg: e.copy(
                out=qTt[:, jg * 4:(jg + 1) * 4, :], in_=ps[:].rearrange("p (a b) -> p a b", a=4)),
                reads=[PS], writes=[QTT])
        sc, SC = scs[i % 2], SCS[i % 2]
        for jg in range(4):
            ps, PS = psum()
            for u in range(4):
                j = jg * 4 + u
                P.op("pe", lambda e, ps=ps, u=u, j=j: e.matmul(
                    ps[:, u * 128:(u + 1) * 128], lhsT=qTt[:, j, :], rhs=kTs[:, j % 2, :],
                    start=True, stop=True), reads=[QTT, KTS], writes=[PS])
            P.op("act", lambda e, ps=ps, jg=jg, sc=sc: e.copy(
                out=sc[:, jg * 4:(jg + 1) * 4, :], in_=ps[:].rearrange("p (a b) -> p a b", a=4)),
                reads=[PS], writes=[SC])
        if i == 0:
            dump("qTt0", qTt[:], [QTT], [128, 16, 128])
            dump("sc0", sc[:], [SC], [128, 16, 128])
        peer_topk(P, A, sc, SC, iota16, IOTA, idx_all[:, i, :], gw_all[:, i, :], IDX[i])
    dump("idx_all", idx_all[:], IDX, [128, NT, 128], I32)
    dump("gw_all", gw_all[:], IDX, [128, NT, 128])
    P.barrier()
    A.release(m_sub)
    A.extra = []
    if stage == "topk":
        return finalize([])
    NS = 16
    GE = 4
    base_off = A.offs["uT"]
    assert A.offs["yT"] == base_off + 8 * T * 2
    gs = [A.alloc_at(f"gs{j}", [128, 2 * D], BF16, base_off + j * 4096) for j in range(NS)]
    GS = P.bufs("gs", NS)
    xn32 = A.alloc("xn32", [128, D], F32); XN = P.buf("xn32")
    junk = A.alloc("junk", [128, D], BF16); JUNK = P.buf("junk")
    dots = A.alloc("dots", [128, 128], F32); DOTS = P.bufs("dots", 128)
    wvg = A.alloc("wvg", [128, 128], F32); WVG = P.bufs("wvg", 128 // GE)
    wv_t = A.alloc("wv_t", [128, 128], F32); WVT = P.bufs("wv_t", 128 // GE)
    NDG = 8
    dg = [A.alloc(f"dg{j}", [128, 128], BF16) for j in range(NDG)]; DG = P.bufs("dg", NDG)
    gi = 0
    di = 0
    fuse = stage != "ffn"
    if fuse:
        assert A.offs["wsl1"] == A.offs["wsl0"] + 2048 and A.offs["wsl2"] == A.offs["wsl0"] + 4096
        assert A.offs["ppb0"] == A.offs["pst2"] + RPP * D * 4
        wg = A.alloc_at("wg", [128, 8, D], BF16, A.offs["pst2"]); WG = P.buf("wg")
        P.dma("pool", lambda e: e.dma_start(out=wg[:], in_=w_gate_d.rearrange("(k p) c -> p k c", p=128)),
              WG, writes=[WG])
        wpp = A.alloc_at("wpp", [128, 2, D], BF16, A.offs["wsl0"]); WPP = P.buf("wpp")
        P.dma("pool", lambda e: e.dma_start(out=wpp[:], in_=w_proj_d.rearrange("(k p) c -> p k c", p=128)),
              WPP, writes=[WPP])
        uTt = A.alloc_at("uTt", [128, 8, 128], BF16, A.offs["wsl2"]); UTT = P.buf("uTt")
        xo0, xo1 = A.offs["pst0"], A.offs["pst1"]
        gpl = A.alloc_at("gpl", [128, D], F32, xo0); GPL = P.buf("gpl")
        gfn = A.alloc_at("gfn", [128, D], F32, xo0 + 4096); GFN = P.buf("gfn")
        P.dma("sp", lambda e: e.dma_start(out=gpl[:], in_=bcast_rows(g_ple_d, D)), GPL, writes=[GPL])
        P.dma("sp", lambda e: e.dma_start(out=gfn[:], in_=bcast_rows(g_final_d, D)), GFN, writes=[GFN])
        xs_p = A.alloc_at("xs_p", [128, D], BF16, xo1); XSP = P.buf("xs_p")
        sq_p = A.alloc_at("sq_p", [128, D], BF16, xo1 + 2048); SQP = P.buf("sq_p")
        ss_p = A.alloc("ss_p", [128, 4], F32); SSP = P.buf("ss_p")
        p32 = A.alloc("p32", [128, 256], F32); P32 = P.buf("p32")
        pb16 = A.alloc("pb16", [128, 256], BF16); PB16 = P.buf("pb16")
        pTt_ = A.alloc("pTt_", [128, 2, 128], BF16); PTT_ = P.buf("pTt_")
        sg_ = [A.alloc_at(f"sg_{j}", [128, 512], F32, xo1 + 4096 + j * 2048) for j in range(2)]
        SG_ = P.bufs("sg_", 2)
        ple_banks = [2, 3, 6, 7]
        ple_rr = [0]

        def ple_psum():
            b_ = ple_banks[ple_rr[0] % 4]
            ple_rr[0] += 1
            return psum(b_)

        def ple_gen(i):
            X = xt[:, i, :]
            P.dma("sp", lambda e: e.dma_start(out=p32[:], in_=p_d[i * 128:(i + 1) * 128, :]), P32, writes=[P32])
            P.op("act", lambda e: e.activation(out=sq_p[:], in_=X, func=AF.Square, accum_out=ss_p[:, 0:1]),
                 reads=[XT[i]], writes=[SQP, SSP])
            yield
            P.op("act", lambda e: e.activation(out=ss_p[:, 1:2], in_=ss_p[:, 0:1], func=AF.Sqrt,
                                               scale=1.0 / D, bias=EPS), reads=[SSP], writes=[SSP])
            P.op("act", lambda e: e.copy(out=pb16[:], in_=p32[:]), reads=[P32], writes=[PB16])
            yield
            P.op("dve", lambda e: e.reciprocal(out=ss_p[:, 1:2], in_=ss_p[:, 1:2]), reads=[SSP], writes=[SSP])
            yield
            P.op("dve", lambda e: e.scalar_tensor_tensor(out=xs_p[:], in0=X, scalar=ss_p[:, 1:2], in1=gpl[:],
                                                         op0=ALU.mult, op1=ALU.mult),
                 reads=[XT[i], SSP, GPL], writes=[XSP])
            yield
            ps, PS = ple_psum()
            psb = ps[:].bitcast(BF16)
            for k in range(8):
                P.op("pe", lambda e, k=k, psb=psb: e.transpose(out=psb[:, k * 128:(k + 1) * 128],
                                                               in_=xs_p[:, k * 128:(k + 1) * 128],
                                                               identity=identb[:]), reads=[XSP, IDB], writes=[PS])
            ps_p, PS_P = ple_psum()
            psbp = ps_p[:].bitcast(BF16)
            for kk in range(2):
                P.op("pe", lambda e, kk=kk, psbp=psbp: e.transpose(out=psbp[:, kk * 128:(kk + 1) * 128],
                                                                   in_=pb16[:, kk * 128:(kk + 1) * 128],
                                                                   identity=identb[:]), reads=[PB16, IDB], writes=[PS_P])
            yield
            P.op("act", lambda e, psb=psb: e.copy(out=uTt[:], in_=psb.rearrange("p (k t) -> p k t", k=8)),
                 reads=[PS], writes=[UTT])
            P.op("act", lambda e, psbp=psbp: e.copy(out=pTt_[:], in_=psbp[:, 0:256].rearrange("p (k t) -> p k t", k=2)),
                 reads=[PS_P], writes=[PTT_])
            yield
            for hf in range(2):
                ps1, PS1 = ple_psum()
                for k in range(8):
                    P.op("pe", lambda e, ps1=ps1, k=k, hf=hf: e.matmul(
                        ps1[:], lhsT=uTt[:, k, :], rhs=wg[:, k, hf * 512:(hf + 1) * 512],
                        start=(k == 0), stop=(k == 7)), reads=[WG, UTT], writes=[PS1])
                ps2, PS2 = ple_psum()
                for kk in range(2):
                    P.op("pe", lambda e, ps2=ps2, kk=kk, hf=hf: e.matmul(
                        ps2[:], lhsT=pTt_[:, kk, :], rhs=wpp[:, kk, hf * 512:(hf + 1) * 512],
                        start=(kk == 0), stop=(kk == 1)), reads=[WPP, PTT_], writes=[PS2])
                yield
                P.op("act", lambda e, ps1=ps1, hf=hf: e.activation(out=sg_[hf][:], in_=ps1[:], func=AF.Sigmoid),
                     reads=[PS1], writes=[SG_[hf]])
                yield
                P.op("dve", lambda e, ps2=ps2, hf=hf: e.tensor_tensor(out=sg_[hf][:], in0=sg_[hf][:], in1=ps2[:],
                                                                      op=ALU.mult), reads=[PS2, SG_[hf]], writes=[SG_[hf]])
                yield
                P.op("dve", lambda e, hf=hf: e.tensor_tensor(
                    out=xt[:, i, hf * 512:(hf + 1) * 512], in0=xt[:, i, hf * 512:(hf + 1) * 512],
                    in1=sg_[hf][:], op=ALU.add), reads=[SG_[hf], XT[i]], writes=[XT[i]])
                yield
            P.op("act", lambda e: e.activation(out=sq_p[:], in_=X, func=AF.Square, accum_out=ss_p[:, 2:3]),
                 reads=[XT[i]], writes=[SQP, SSP])
            yield
            P.op("act", lambda e: e.activation(out=ss_p[:, 3:4], in_=ss_p[:, 2:3], func=AF.Sqrt,
                                               scale=1.0 / D, bias=EPS), reads=[SSP], writes=[SSP])
            yield
            P.op("dve", lambda e: e.reciprocal(out=ss_p[:, 3:4], in_=ss_p[:, 3:4]), reads=[SSP], writes=[SSP])
            yield
            P.op("dve", lambda e: e.scalar_tensor_tensor(out=X, in0=X, scalar=ss_p[:, 3:4], in1=gfn[:],
                                                         op0=ALU.mult, op1=ALU.mult),
                 reads=[XT[i], SSP, GFN], writes=[XT[i]])
            P.dma("sp", lambda e: e.dma_start(out=out_d[i * 128:(i + 1) * 128, :], in_=X), XT[i], reads=[XT[i]])
            yield

    pend = None
    for i in range(NT):
        P.op("dve", lambda e, i=i: e.scalar_tensor_tensor(
            out=xn32[:], in0=xt[:, i, :], scalar=rstd_all[:, i:i + 1], in1=gB[:],
            op0=ALU.mult, op1=ALU.mult), reads=[XT[i], RSTD, GB], writes=[XN])
        bank = 4 * (i % 2)
        psy = [psum(bank), psum(bank + 1)]
        for g in range(128 // GE):
            if pend is not None:
                next(pend, None)
            sls = []
            for u in range(GE):
                j = g * GE + u
                sl = gi % NS
                gi += 1
                sls.append(sl)
                P.dma("pool", lambda e, sl=sl, i=i, j=j: e.indirect_dma_start(
                    out=gs[sl][:], out_offset=None, in_=uv_d,
                    in_offset=bass.IndirectOffsetOnAxis(ap=idx_all[:, i, j:j + 1], axis=0)),
                    GS[sl], reads=[IDX[i]], writes=[GS[sl]])
            for u in range(GE):
                j = g * GE + u
                sl = sls[u]
                P.op("dve", lambda e, sl=sl, j=j: e.scalar_tensor_tensor(
                    out=junk[:], in0=gs[sl][:, 0:D], scalar=1.0, in1=xn32[:], op0=ALU.mult, op1=ALU.mult,
                    accum_out=dots[:, j:j + 1]), reads=[GS[sl], XN], writes=[JUNK, DOTS[j]])
            c0, c1 = g * GE, (g + 1) * GE
            P.op("act", lambda e, c0=c0, c1=c1: e.activation(out=wvg[:, c0:c1], in_=dots[:, c0:c1],
                                                            func=AF.Gelu_apprx_tanh),
                 reads=DOTS[c0:c1], writes=[WVG[g]])
            P.op("dve", lambda e, i=i, c0=c0, c1=c1: e.tensor_tensor(
                out=wv_t[:, c0:c1], in0=wvg[:, c0:c1], in1=gw_all[:, i, c0:c1], op=ALU.mult),
                reads=[WVG[g], IDX[i]], writes=[WVT[g]])
            for u in range(GE):
                j = g * GE + u
                sl = sls[u]
                dsl = di % NDG
                di += 1
                P.op("act", lambda e, dsl=dsl, j=j: e.activation(
                    out=dg[dsl][:], in_=identb[:], func=AF.Copy, scale=wv_t[:, j:j + 1]),
                    reads=[IDB, WVT[g]], writes=[DG[dsl]])
                for hf in range(2):
                    ps, PS = psy[hf]
                    P.op("pe", lambda e, ps=ps, dsl=dsl, sl=sl, hf=hf, j=j: e.matmul(
                        ps[:], lhsT=dg[dsl][:], rhs=gs[sl][:, D + hf * 512:D + (hf + 1) * 512],
                        start=(j == 0), stop=(j == 127)), reads=[DG[dsl], GS[sl]], writes=[PS])
        for hf in range(2):
            ps, PS = psy[hf]
            P.op("dve", lambda e, ps=ps, i=i, hf=hf: e.tensor_tensor(
                out=xt[:, i, hf * 512:(hf + 1) * 512], in0=xt[:, i, hf * 512:(hf + 1) * 512],
                in1=ps[:], op=ALU.add), reads=[PS, XT[i]], writes=[XT[i]])
        if fuse:
            if pend is not None:
                for _ in pend:
                    pass
            pend = ple_gen(i)
    if fuse:
        for _ in pend:
            pass
        return finalize(XT)
    P.barrier()
    A.release(m_phase)
    if stage == "ffn":
        for i in range(NT):
            P.dma("sp", lambda e, i=i: e.dma_start(out=out_d[i * 128:(i + 1) * 128, :], in_=xt[:, i, :]),
                  XT[i], reads=[XT[i]])
        return finalize(XT)

    m_phase = A.mark()
    norm_transpose(xt_src, g_ple_d, uT, UT)
    wg = A.alloc("wg", [128, 8, D], BF16); WG = P.buf("wg")
    P.dma("pool", lambda e: e.dma_start(out=wg[:], in_=w_gate_d.rearrange("(k p) c -> p k c", p=128)),
          WG, writes=[WG])
    wpp = A.alloc("wpp", [128, 2, D], BF16); WPP = P.buf("wpp")
    P.dma("pool", lambda e: e.dma_start(out=wpp[:], in_=w_proj_d.rearrange("(k p) c -> p k c", p=128)),
          WPP, writes=[WPP])
    pb = [A.alloc(f"pb{j}", [128, 256], BF16) for j in range(2)]; PB = P.bufs("pb", 2)
    pTt = [A.alloc(f"pTt{j}", [128, 2, 128], BF16) for j in range(2)]; PTT = P.bufs("pTt", 2)
    sg = [A.alloc(f"sg{j}", [128, 512], F32) for j in range(2)]; SG = P.bufs("sg", 2)
    it = 0
    for i in range(NT):
        j = i % 2
        P.dma("pool", lambda e, i=i, j=j: e.dma_start(out=pb[j][:], in_=p_d[i * 128:(i + 1) * 128, :]),
              PB[j], writes=[PB[j]])
        ps, PS = psum()
        psb = ps[:].bitcast(BF16)
        for kk in range(2):
            P.op("pe", lambda e, j=j, kk=kk, psb=psb: e.transpose(
                out=psb[:, kk * 128:(kk + 1) * 128], in_=pb[j][:, kk * 128:(kk + 1) * 128],
                identity=identb[:]), reads=[PB[j], IDB], writes=[PS])
        P.op("act", lambda e, j=j, psb=psb: e.copy(
            out=pTt[j][:], in_=psb[:, 0:256].rearrange("p (k t) -> p k t", k=2)), reads=[PS], writes=[PTT[j]])
        for hf in range(2):
            jj = it % 2
            it += 1
            ps1, PS1 = psum()
            for k in range(8):
                P.op("pe", lambda e, ps1=ps1, k=k, i=i, hf=hf: e.matmul(
                    ps1[:], lhsT=uT[:, k, i * 128:(i + 1) * 128], rhs=wg[:, k, hf * 512:(hf + 1) * 512],
                    start=(k == 0), stop=(k == 7)), reads=[WG, UT[i]], writes=[PS1])
            P.op("act", lambda e, ps1=ps1, jj=jj: e.activation(out=sg[jj][:], in_=ps1[:], func=AF.Sigmoid),
                 reads=[PS1], writes=[SG[jj]])
            ps2, PS2 = psum()
            for kk in range(2):
                P.op("pe", lambda e, ps2=ps2, kk=kk, j=j, hf=hf: e.matmul(
                    ps2[:], lhsT=pTt[j][:, kk, :], rhs=wpp[:, kk, hf * 512:(hf + 1) * 512],
                    start=(kk == 0), stop=(kk == 1)), reads=[WPP, PTT[j]], writes=[PS2])
            P.op("dve", lambda e, ps2=ps2, jj=jj: e.tensor_tensor(out=sg[jj][:], in0=sg[jj][:], in1=ps2[:], op=ALU.mult),
                 reads=[PS2, SG[jj]], writes=[SG[jj]])
            P.op("dve", lambda e, jj=jj, i=i, hf=hf: e.tensor_tensor(
                out=xt[:, i, hf * 512:(hf + 1) * 512], in0=xt[:, i, hf * 512:(hf + 1) * 512],
                in1=sg[jj][:], op=ALU.add), reads=[SG[jj], XT[i]], writes=[XT[i]])
    P.barrier()
    A.release(m_phase)

    P.dma("sp", lambda e: e.dma_start(out=gB[:], in_=bcast_rows(g_final_d, D)), GB, writes=[GB])
    sqf = A.alloc("fn_sq", [128, D], BF16); SQF = P.buf("fn_sq")
    ssf = A.alloc("fn_ss", [128, 2], F32); SSF = P.buf("fn_ss")
    for i in range(NT):
        P.op("act", lambda e, i=i: e.activation(out=sqf[:], in_=xt[:, i, :], func=AF.Square,
                                                accum_out=ssf[:, 0:1]), reads=[XT[i]], writes=[SQF, SSF])
        P.op("act", lambda e: e.activation(out=ssf[:, 1:2], in_=ssf[:, 0:1], func=AF.Sqrt,
                                           scale=1.0 / D, bias=EPS), reads=[SSF], writes=[SSF])
        P.op("dve", lambda e, i=i: e.reciprocal(out=rstd_all[:, i:i + 1], in_=ssf[:, 1:2]),
             reads=[SSF], writes=[RSTD])
        P.op("dve", lambda e, i=i: e.scalar_tensor_tensor(
            out=xt[:, i, :], in0=xt[:, i, :], scalar=rstd_all[:, i:i + 1], in1=gB[:],
            op0=ALU.mult, op1=ALU.mult), reads=[XT[i], RSTD, GB], writes=[XT[i]])
        P.dma("sp", lambda e, i=i: e.dma_start(out=out_d[i * 128:(i + 1) * 128, :], in_=xt[:, i, :]),
              XT[i], reads=[XT[i]])
    return finalize(XT)


def _t5_bucket(dist):
    max_exact = 16
    n = np.maximum(dist, 0)
    nf = np.maximum(n, 1).astype(np.float32)
    large = max_exact + (np.log(nf / max_exact) / np.float32(np.log(2048 / max_exact))
                         * (32 - max_exact)).astype(np.int32)
    large = np.minimum(large, 31)
    return np.where(n < max_exact, n, large)


def _bucket_table():
    import jax, jax.numpy as jnp
    return None


def prep_shared(inp):
    import ml_dtypes
    f32 = np.float32
    sh = {}
    sh["g_mix"] = np.ascontiguousarray(inp["g_mix"][0], f32)
    sh["w_in"] = np.ascontiguousarray(inp["w_in"][0], f32)
    sh["w_out"] = np.ascontiguousarray(inp["w_out"][0], f32)
    cv = np.zeros((128, 40), f32)
    cw = inp["conv_w"][0]
    for c in range(4):
        for j in range(4):
            cv[:, c * 4 + j] = cw[j, c * 128:(c + 1) * 128]
    for n, key in enumerate(["conv_b", "b_rg", "b_ig", "lru_lambda", "g_lru_out", "g_attn_out"]):
        cv[:, 16 + 4 * n:20 + 4 * n] = inp[key][0].reshape(4, 128).T
    sh["cv"] = cv
    for name, key in (("wrg_bd", "w_rg"), ("wig_bd", "w_ig")):
        w = inp[key][0]
        bd = np.zeros((128, 4, 128), f32)
        for g in range(8):
            c, l = g // 2, g % 2
            bd[l * 64:(l + 1) * 64, c, l * 64:(l + 1) * 64] = w[g]
        sh[name] = bd
    rb = inp["rel_bias"].astype(f32)
    k = np.arange(128)[:, None]
    q = np.arange(256)[None, :]
    dist = q - k
    valid = (dist >= 0) & (dist <= 128)
    bt = np.full((128, 4, 3, 2, 256), NEG, f32)
    for bi, d in enumerate((1, 4, 16)):
        bucket = _t5_bucket(dist * d)
        for h in range(8):
            vals = rb[bucket, h]
            bt[:, h // 2, bi, h % 2, :] = np.where(valid, vals, f32(NEG))
    sh["biasT"] = bt.reshape(128, 4, 3 * 2 * 256)
    sh["ident_bf"] = np.eye(128, dtype=f32).astype(ml_dtypes.bfloat16)
    sh["g_ffn"] = np.ascontiguousarray(inp["g_ffn"][0], f32)
    sh["peer_wq"] = np.ascontiguousarray(inp["peer_wq"][0], f32)
    sh["k1T"] = np.ascontiguousarray(inp["peer_k1"][0].T, f32)
    sh["k2T"] = np.ascontiguousarray(inp["peer_k2"][0].T, f32)
    sh["iota16"] = np.tile(np.arange(16, dtype=f32)[None], (128, 1))
    sh["peer_u"] = np.ascontiguousarray(inp["peer_u"][0], f32)
    sh["peer_v"] = np.ascontiguousarray(inp["peer_v"][0], f32)
    sh["g_ple"] = np.ascontiguousarray(inp["g_ple"][0], f32)
    sh["w_ple_gate"] = np.ascontiguousarray(inp["w_ple_gate"][0], f32)
    sh["w_ple_proj"] = np.ascontiguousarray(inp["w_ple_proj"][0], f32)
    sh["g_final"] = np.ascontiguousarray(inp["g_final"], f32)
    return sh


_NC_CACHE = {}


def kernel(**inputs):
    inp = {k: np.asarray(v) for k, v in inputs.items()}
    sh = prep_shared(inp)
    if "nc" not in _NC_CACHE:
        nc = bass.Bass("TRN2", target_bir_lowering=False)
        build(nc)
        _NC_CACHE["nc"] = nc
    nc = _NC_CACHE["nc"]
    in_maps = []
    for b in range(8):
        m = dict(sh)
        m["x"] = np.ascontiguousarray(inp["x"][b], np.float32)
        m["p"] = np.ascontiguousarray(inp["p"][0, b], np.float32)
        in_maps.append(m)
    res = run_bass_kernel_spmd(nc, in_maps, core_ids=list(range(8)))
    return np.stack([np.asarray(r["out"]) for r in res.results], axis=0).astype(np.float32)
```
